# Optimizing a Trainium2 kernel written in Bass

```python
import math
import jax
import jax.numpy as jnp
from jax import lax
import numpy as np

D_MODEL = 2048
BATCH = 2
SEQ = 4096
DEPTH = 1

CTX_LEN = 256
GRID_W = 64
N_DIR = 2
S5_WIDTH = D_MODEL // 2
S5_GROUP = 16
S5_GROUPS = S5_WIDTH // S5_GROUP
S5_STATE = 64
S5_DT_MIN = 1e-3
S5_DT_MAX = 1e-1
RW_WIDTH = D_MODEL - S5_WIDTH
RW_HEAD = 64
RW_HEADS = RW_WIDTH // RW_HEAD
RW_DECAY_LORA = 64
RW_AAA_LORA = 64
RW_GATE_LORA = 160
RW_GN_EPS = 64e-5
RW_COLS = 3 * RW_WIDTH + N_DIR * RW_DECAY_LORA + N_DIR * RW_AAA_LORA + RW_GATE_LORA
D_MIX = S5_WIDTH + RW_WIDTH
IN_COLS = S5_WIDTH + RW_COLS
N_EXPERTS = 32
TOP_K = 4
D_EXPERT = D_MODEL
SWIGLU_ALPHA = 1.702
SWIGLU_LIMIT = 7.0
MOE_BLOCK = 128
NORM_EPS = 1e-5

kernel_name = 'hybrid_s5_rwkv7_moe_prefix_block'


def rmsnorm(x, g):
    xf = x.astype(jnp.float32)
    y = xf * lax.rsqrt(jnp.mean(xf * xf, axis=-1, keepdims=True) + NORM_EPS)
    return (y * g.astype(jnp.float32)).astype(x.dtype)


def adaln_params(cond, w, b):
    m = jnp.matmul(jax.nn.silu(cond), w) + b
    return jnp.split(m[..., None, :], 6, axis=-1)


def _complex(re, im):
    return lax.complex(re.astype(jnp.float32), im.astype(jnp.float32))


def diag_linear_scan(lam_bar, bu, h0, reverse):
    first = -1 if reverse else 0
    bu = bu.at[:, first].add(lam_bar * h0)
    a = jnp.broadcast_to(lam_bar, bu.shape)

    def combine(left, right):
        a1, b1 = left
        a2, b2 = right
        return a1 * a2, a2 * b1 + b2

    _, h = lax.associative_scan(combine, (a, bu), reverse=reverse, axis=1)
    return h


def s5_mixer(u_ctx, u_lat, a_re, a_im, log_dt, b_re, b_im, c_re, c_im, d_skip, glu_w, glu_b,
             ctx_out):
    def groups(u):
        return u.astype(jnp.float32).reshape(u.shape[0], u.shape[1], S5_GROUPS, S5_GROUP)

    uc, ul = groups(u_ctx), groups(u_lat)
    ucc, ulc = uc.astype(jnp.complex64), ul.astype(jnp.complex64)
    y_ctx = jnp.zeros_like(uc)
    y_lat = jnp.zeros_like(ul)
    h_zero = jnp.zeros((uc.shape[0], S5_GROUPS, S5_STATE), jnp.complex64)
    for d in range(N_DIR):
        rev = d == 1
        lam = _complex(a_re[d], a_im[d])
        dt = jnp.exp(log_dt[d].astype(jnp.float32))[:, None]
        lam_bar = jnp.exp(lam * dt)
        b_bar = ((lam_bar - 1.0) / lam)[..., None] * _complex(b_re[d], b_im[d])
        c_mat = _complex(c_re[d], c_im[d])
        h_ctx = diag_linear_scan(lam_bar, jnp.einsum('blgh,gph->blgp', ucc, b_bar), h_zero, rev)
        h0 = h_ctx[:, 0] if rev else h_ctx[:, -1]
        h_lat = diag_linear_scan(lam_bar, jnp.einsum('blgh,gph->blgp', ulc, b_bar), h0, rev)
        y_lat = y_lat + jnp.real(jnp.einsum('blgp,ghp->blgh', h_lat, c_mat))
        if ctx_out:
            y_ctx = y_ctx + jnp.real(jnp.einsum('blgp,ghp->blgh', h_ctx, c_mat))

    d_vec = d_skip.astype(jnp.float32).reshape(S5_GROUPS, S5_GROUP)
    w_g = glu_w.astype(jnp.float32)
    b_g = glu_b.astype(jnp.float32).reshape(S5_GROUPS, S5_GROUP)

    def glu(y, u):
        y = jax.nn.gelu(y + d_vec * u)
        gate = jnp.einsum('blgh,ghk->blgk', y, w_g) + b_g
        out = y * jax.nn.sigmoid(gate)
        return out.reshape(out.shape[0], out.shape[1], S5_WIDTH)

    out_ctx = glu(y_ctx, uc) if ctx_out else None
    return out_ctx, glu(y_lat, ul)


def heads(t):
    return t.reshape(t.shape[:-1] + (RW_HEADS, RW_HEAD))


def neighbour_diff(g, axis, offset):
    n = g.shape[axis]
    idx = jnp.arange(n) + offset
    valid = ((idx >= 0) & (idx < n)).reshape([n if i == axis else 1 for i in range(g.ndim)])
    return jnp.where(valid, jnp.roll(g, -offset, axis=axis) - g, jnp.zeros((), g.dtype))


def centred_shift(z, mu, rows, width):
    bsz, length, cz = z.shape
    g = z.reshape(bsz, rows, width, cz)
    out = g
    for j, (axis, off) in enumerate(((2, -1), (2, 1), (1, -1), (1, 1))):
        out = out + mu[j] * neighbour_diff(g, axis, off)
    return out.reshape(bsz, length, cz)


def rwkv_features(z, rows, width, mu, w0, w2, a0, a2, g2, k_k, k_a):
    bsz, length, _ = z.shape
    z = centred_shift(z, mu, rows, width).astype(jnp.float32)
    i1, i2, i3 = RW_WIDTH, 2 * RW_WIDTH, 3 * RW_WIDTH
    i4 = i3 + N_DIR * RW_DECAY_LORA
    i5 = i4 + N_DIR * RW_AAA_LORA
    r, k, v = z[..., :i1], z[..., i1:i2], z[..., i2:i3]
    xw = z[..., i3:i4].reshape(bsz, length, N_DIR, RW_DECAY_LORA)
    xa = z[..., i4:i5].reshape(bsz, length, N_DIR, RW_AAA_LORA)
    xg = z[..., i5:]
    dec = w0.astype(jnp.float32) + jnp.einsum('blnr,nrc->blnc', jnp.tanh(xw), w2.astype(jnp.float32))
    w = jnp.exp(-math.exp(-0.5) * jax.nn.sigmoid(dec))
    a = jax.nn.sigmoid(a0.astype(jnp.float32)
                       + jnp.einsum('blnr,nrc->blnc', xa, a2.astype(jnp.float32)))
    g = jnp.matmul(jax.nn.sigmoid(xg), g2.astype(jnp.float32))
    kk = heads(k * k_k.astype(jnp.float32))
    kh = kk / jnp.maximum(jnp.linalg.norm(kk, axis=-1, keepdims=True), 1e-12)
    kt = k[:, :, None, :] * (1.0 + (a - 1.0) * k_a.astype(jnp.float32))
    return r, v, g, w, a, kh, kt


def rwkv_scan(w, kh, a, kt, v, r, s0, reverse):
    xs = tuple(jnp.moveaxis(t, 1, 0) for t in (w, kh, a * kh, kt, v, r))

    def step(s, inp):
        w_t, kh_t, akh_t, kt_t, v_t, r_t = inp
        s_k = jnp.einsum('bhvk,bhk->bhv', s, kh_t)
        s = (s * w_t[:, :, None, :] - s_k[..., None] * akh_t[:, :, None, :]
             + v_t[..., None] * kt_t[:, :, None, :])
        return s, jnp.einsum('bhvk,bhk->bhv', s, r_t)

    s_fin, y = lax.scan(step, s0, xs, reverse=reverse)
    return s_fin, jnp.moveaxis(y, 0, 1)


def rwkv_readout(y, feats, r_k, ln_w, ln_b):
    r, v, g, _, _, _, kt = feats
    mean = jnp.mean(y, axis=-1, keepdims=True)
    var = jnp.mean(jnp.square(y - mean), axis=-1, keepdims=True)
    yn = ((y - mean) * lax.rsqrt(var + RW_GN_EPS) * heads(ln_w.astype(jnp.float32))
          + heads(ln_b.astype(jnp.float32)))
    bonus = jnp.sum(heads(r)[:, :, None] * heads(kt) * r_k.astype(jnp.float32),
                    axis=(2, 4))[..., None] * heads(v)
    out = yn + bonus
    return out.reshape(out.shape[0], out.shape[1], RW_WIDTH) * g


def rwkv_mixer(z_ctx, z_lat, lat_rows, mu, w0, w2, a0, a2, g2, k_k, k_a, r_k, ln_w, ln_b, ctx_out):
    fc = rwkv_features(z_ctx, 1, z_ctx.shape[1], mu, w0, w2, a0, a2, g2, k_k, k_a)
    fl = rwkv_features(z_lat, lat_rows, GRID_W, mu, w0, w2, a0, a2, g2, k_k, k_a)
    s0 = jnp.zeros((z_lat.shape[0], RW_HEADS, RW_HEAD, RW_HEAD), jnp.float32)

    def dir_inputs(f, d):
        r, v, _, w, a, kh, kt = f
        return heads(w[:, :, d]), kh, heads(a[:, :, d]), heads(kt[:, :, d]), heads(v), heads(r)

    y_ctx = 0.0
    y_lat = 0.0
    for d in range(N_DIR):
        rev = d == 1
        s_ctx, yc = rwkv_scan(*dir_inputs(fc, d), s0, rev)
        _, yl = rwkv_scan(*dir_inputs(fl, d), s_ctx, rev)
        y_ctx = y_ctx + yc
        y_lat = y_lat + yl
    out_ctx = rwkv_readout(y_ctx, fc, r_k, ln_w, ln_b) if ctx_out else None
    return out_ctx, rwkv_readout(y_lat, fl, r_k, ln_w, ln_b)


def moe_ffn(h, router_w, router_b, w_gu, b_gu, w_dn, b_dn):
    n_tok, d_model = h.shape
    logits = (jnp.matmul(h, router_w) + router_b).astype(jnp.float32)
    top_val, top_idx = lax.top_k(logits, TOP_K)
    gates = jax.nn.softmax(top_val, axis=-1)
    n_assign = n_tok * TOP_K
    flat_e = top_idx.reshape(-1)
    flat_tok = jnp.repeat(jnp.arange(n_tok, dtype=jnp.int32), TOP_K)
    flat_g = gates.reshape(-1)
    order = jnp.argsort(flat_e)
    sorted_e = flat_e[order]
    counts = jnp.bincount(flat_e, length=N_EXPERTS)
    start = jnp.cumsum(counts) - counts
    padded = (counts + MOE_BLOCK - 1) // MOE_BLOCK * MOE_BLOCK
    padded_end = jnp.cumsum(padded)
    padded_start = padded_end - padded
    dest = padded_start[sorted_e] + jnp.arange(n_assign) - start[sorted_e]
    n_blocks = -(-(n_assign + N_EXPERTS * (MOE_BLOCK - 1)) // MOE_BLOCK)
    n_slots = n_blocks * MOE_BLOCK
    slot_tok = jnp.zeros((n_slots,), jnp.int32).at[dest].set(flat_tok[order])
    slot_gate = jnp.zeros((n_slots,), jnp.float32).at[dest].set(flat_g[order])
    block_e = jnp.minimum(
        jnp.searchsorted(padded_end, jnp.arange(n_blocks) * MOE_BLOCK, side='right'),
        N_EXPERTS - 1)

    def expert_block(args):
        tok, e = args
        xb = h[tok]
        gu = jnp.matmul(xb, w_gu[e]) + b_gu[e]
        glu, lin = jnp.split(gu, 2, axis=-1)
        glu = jnp.minimum(glu, SWIGLU_LIMIT)
        lin = jnp.clip(lin, -SWIGLU_LIMIT, SWIGLU_LIMIT)
        act = (lin + 1.0) * glu * jax.nn.sigmoid(SWIGLU_ALPHA * glu)
        return jnp.matmul(act, w_dn[e]) + b_dn[e]

    y = lax.map(expert_block, (slot_tok.reshape(n_blocks, MOE_BLOCK), block_e))
    y = y.reshape(n_slots, d_model) * slot_gate[:, None].astype(y.dtype)
    return jax.ops.segment_sum(y, slot_tok, num_segments=n_tok)


def setup_inputs(seed: int = 0) -> dict:
    key = jax.random.key(seed)
    keys = iter(jax.random.split(key, 48))

    def nrm(shape, scale):
        return scale * jax.random.normal(next(keys), shape, jnp.float32)

    def unif(shape, lo, hi):
        return jax.random.uniform(next(keys), shape, jnp.float32, lo, hi)

    nl, g, p, h = DEPTH, S5_GROUPS, S5_STATE, S5_GROUP
    return {
        'x': nrm((BATCH, SEQ, D_MODEL), 1.0),
        'c': nrm((BATCH, D_MODEL), 1.0),
        'ctx': nrm((BATCH, CTX_LEN, D_MODEL), 1.0),
        'c_ctx': nrm((D_MODEL,), 1.0),
        'mod_w': nrm((nl, D_MODEL, 6 * D_MODEL), D_MODEL ** -0.5),
        'mod_b': nrm((nl, 6 * D_MODEL), 0.02),
        'norm1_g': 1.0 + nrm((nl, D_MODEL), 0.02),
        'w_in': nrm((nl, D_MODEL, IN_COLS), D_MODEL ** -0.5),
        's5_a_re': -0.5 + nrm((nl, N_DIR, g, p), 0.01),
        's5_a_im': jnp.pi * jnp.arange(p, dtype=jnp.float32) + nrm((nl, N_DIR, g, p), 0.01),
        's5_log_dt': unif((nl, N_DIR, g), math.log(S5_DT_MIN), math.log(S5_DT_MAX)),
        's5_b_re': nrm((nl, N_DIR, g, p, h), (2 * h) ** -0.5),
        's5_b_im': nrm((nl, N_DIR, g, p, h), (2 * h) ** -0.5),
        's5_c_re': nrm((nl, N_DIR, g, h, p), p ** -0.5),
        's5_c_im': nrm((nl, N_DIR, g, h, p), p ** -0.5),
        's5_d': nrm((nl, S5_WIDTH), 1.0),
        's5_glu_w': nrm((nl, g, h, h), h ** -0.5),
        's5_glu_b': nrm((nl, S5_WIDTH), 0.02),
        'rw_mu': unif((nl, 4, RW_COLS), 0.0, 0.25),
        'rw_w0': unif((nl, N_DIR, RW_WIDTH), -6.0, 1.0),
        'rw_w2': nrm((nl, N_DIR, RW_DECAY_LORA, RW_WIDTH), 0.1 * RW_DECAY_LORA ** -0.5),
        'rw_a0': nrm((nl, N_DIR, RW_WIDTH), 0.5),
        'rw_a2': nrm((nl, N_DIR, RW_AAA_LORA, RW_WIDTH), 0.1 * RW_AAA_LORA ** -0.5),
        'rw_g2': nrm((nl, RW_GATE_LORA, RW_WIDTH), RW_GATE_LORA ** -0.5),
        'rw_k_k': 0.85 + nrm((nl, RW_WIDTH), 0.05),
        'rw_k_a': 1.0 + nrm((nl, RW_WIDTH), 0.05),
        'rw_r_k': nrm((nl, RW_HEADS, RW_HEAD), 0.1),
        'rw_ln_w': 1.0 + nrm((nl, RW_WIDTH), 0.02),
        'rw_ln_b': nrm((nl, RW_WIDTH), 0.02),
        'w_out': nrm((nl, D_MIX, D_MODEL), D_MIX ** -0.5),
        'norm2_g': 1.0 + nrm((nl, D_MODEL), 0.02),
        'router_w': nrm((nl, D_MODEL, N_EXPERTS), D_MODEL ** -0.5),
        'router_b': nrm((nl, N_EXPERTS), 0.01),
        'exp_w_gu': nrm((nl, N_EXPERTS, D_MODEL, 2 * D_EXPERT), D_MODEL ** -0.5),
        'exp_b_gu': nrm((nl, N_EXPERTS, 2 * D_EXPERT), 0.01),
        'exp_w_dn': nrm((nl, N_EXPERTS, D_EXPERT, D_MODEL), D_EXPERT ** -0.5),
        'exp_b_dn': nrm((nl, N_EXPERTS, D_MODEL), 0.01),
        'final_g': 1.0 + nrm((D_MODEL,), 0.02),
    }


def reference(x, c, ctx, c_ctx, mod_w, mod_b, norm1_g, w_in, s5_a_re, s5_a_im, s5_log_dt,
              s5_b_re, s5_b_im, s5_c_re, s5_c_im, s5_d, s5_glu_w, s5_glu_b, rw_mu, rw_w0, rw_w2,
              rw_a0, rw_a2, rw_g2, rw_k_k, rw_k_a, rw_r_k, rw_ln_w, rw_ln_b, w_out, norm2_g,
              router_w, router_b, exp_w_gu, exp_b_gu, exp_w_dn, exp_b_dn, final_g):
    bsz, n_lat, d_model = x.shape
    n_ctx = ctx.shape[1]
    rows = n_lat // GRID_W
    for i in range(DEPTH):
        last = i == DEPTH - 1
        sh1, sc1, gt1, sh2, sc2, gt2 = adaln_params(c, mod_w[i], mod_b[i])
        csh1, csc1, cgt1, csh2, csc2, cgt2 = adaln_params(c_ctx, mod_w[i], mod_b[i])

        p_lat = jnp.matmul(rmsnorm(x, norm1_g[i]) * (1.0 + sc1) + sh1, w_in[i])
        p_ctx = jnp.matmul(rmsnorm(ctx, norm1_g[i]) * (1.0 + csc1) + csh1, w_in[i])
        y5_ctx, y5_lat = s5_mixer(p_ctx[..., :S5_WIDTH], p_lat[..., :S5_WIDTH],
                                  s5_a_re[i], s5_a_im[i], s5_log_dt[i], s5_b_re[i], s5_b_im[i],
                                  s5_c_re[i], s5_c_im[i], s5_d[i], s5_glu_w[i], s5_glu_b[i],
                                  not last)
        yr_ctx, yr_lat = rwkv_mixer(p_ctx[..., S5_WIDTH:], p_lat[..., S5_WIDTH:], rows,
                                    rw_mu[i], rw_w0[i], rw_w2[i], rw_a0[i], rw_a2[i], rw_g2[i],
                                    rw_k_k[i], rw_k_a[i], rw_r_k[i], rw_ln_w[i], rw_ln_b[i],
                                    not last)
        mix_lat = jnp.matmul(jnp.concatenate([y5_lat, yr_lat], axis=-1).astype(x.dtype), w_out[i])
        x = x + gt1 * mix_lat
        if not last:
            mix_ctx = jnp.matmul(jnp.concatenate([y5_ctx, yr_ctx], axis=-1).astype(ctx.dtype),
                                 w_out[i])
            ctx = ctx + cgt1 * mix_ctx

        f_lat = (rmsnorm(x, norm2_g[i]) * (1.0 + sc2) + sh2).reshape(bsz * n_lat, d_model)
        moe_args = (router_w[i], router_b[i], exp_w_gu[i], exp_b_gu[i], exp_w_dn[i], exp_b_dn[i])
        if last:
            x = x + gt2 * moe_ffn(f_lat, *moe_args).reshape(bsz, n_lat, d_model)
        else:
            f_ctx = (rmsnorm(ctx, norm2_g[i]) * (1.0 + csc2) + csh2).reshape(bsz * n_ctx, d_model)
            f = moe_ffn(jnp.concatenate([f_lat, f_ctx], axis=0), *moe_args)
            x = x + gt2 * f[:bsz * n_lat].reshape(bsz, n_lat, d_model)
            ctx = ctx + cgt2 * f[bsz * n_lat:].reshape(bsz, n_ctx, d_model)
    return rmsnorm(x, final_g)
```

```python
import math
from contextlib import ExitStack

import numpy as np
import concourse.bass as bass
import concourse.mybir as mybir
from concourse.bass_utils import run_bass_kernel_spmd

F32 = mybir.dt.float32
BF16 = mybir.dt.bfloat16
ALU = mybir.AluOpType
AF = mybir.ActivationFunctionType
AX = mybir.AxisListType

D = 2048
KC = D // 128
S5W = 1024
RWW = 1024
RWC = 3 * RWW + 128 + 128 + 160
INC = S5W + RWC
NEXP = 32
CS = 64
NSUB = 128 // CS
ARENA = 52800


class Cfg:
    def __init__(self, nrow=64, ctx=256, dexp=2048, stages=99):
        self.nrow = nrow
        self.seq = nrow * 64
        self.ctx = ctx
        self.ntok = self.seq + ctx
        self.npos = self.seq + 2 * ctx
        self.dexp = dexp
        self.stages = stages


ENGS = ("sp", "act", "dve", "pool", "pe")
NDSEM = 6
SELF_SYNC = {"pool", "dve", "act"}


class Op:
    __slots__ = ("eng", "fn", "deps", "sig", "sem", "val", "dma", "prev_dma")

    def __init__(self, eng, fn, dma):
        self.eng, self.fn, self.dma = eng, fn, dma
        self.deps = []
        self.sig = dma
        self.sem = None
        self.val = 0
        self.prev_dma = None


class Prog:
    def __init__(self, nc, es):
        self.nc = nc
        self.ops = {e: [] for e in ENGS}
        self.tab = {}
        self.csem = {e: es.enter_context(nc.semaphore("c_" + e)) for e in ENGS}
        self.dsem = {e: [es.enter_context(nc.semaphore(f"d_{e}{i}")) for i in range(NDSEM)]
                     for e in ("sp", "act", "pool")}
        self.dcnt = {e: [0] * NDSEM for e in self.dsem}
        self.dlast = {e: [None] * NDSEM for e in self.dsem}
        self.drr = {e: 0 for e in self.dsem}

    def _entries(self, key):
        name, sub = key
        t = self.tab.setdefault(name, {})
        if sub is None:
            return list(t.values())
        out = []
        if sub in t:
            out.append(t[sub])
        if None in t:
            out.append(t[None])
        return out

    def add(self, eng, fn, reads=(), writes=(), dma=False):
        op = Op(eng, fn, dma)
        deps = []
        for k in reads:
            for ent in self._entries(k):
                if ent[0] is not None:
                    deps.append(ent[0])
        for k in writes:
            for ent in self._entries(k):
                if ent[0] is not None:
                    deps.append(ent[0])
                deps.extend(ent[1].values())
        if dma:
            i = self.drr[eng]
            self.drr[eng] = (i + 1) % NDSEM
            op.sem = self.dsem[eng][i]
            self.dcnt[eng][i] += 1
            op.val = 16 * self.dcnt[eng][i]
            op.prev_dma = self.dlast[eng][i]
            self.dlast[eng][i] = op
            if op.prev_dma is not None:
                deps.append(op.prev_dma)
        seen = set()
        for d in deps:
            if d is op or id(d) in seen:
                continue
            seen.add(id(d))
            if (not d.dma) and d.eng == eng and eng not in SELF_SYNC:
                continue
            d.sig = True
            op.deps.append(d)
        for k in writes:
            name, sub = k
            t = self.tab.setdefault(name, {})
            if sub is None:
                t.clear()
            t[sub] = [op, {}]
        for k in reads:
            name, sub = k
            t = self.tab.setdefault(name, {})
            if sub not in t:
                t[sub] = [None, {}]
            t[sub][1][eng if not dma else (eng, id(op))] = op
        self.ops[eng].append(op)
        return op

    def barrier(self):
        lasts = []
        for e in ENGS:
            for o in reversed(self.ops[e]):
                if o.fn is not None and not o.dma:
                    lasts.append(o)
                    break
        for e in self.dsem:
            for d in self.dlast[e]:
                if d is not None:
                    lasts.append(d)
        for e in ENGS:
            op = Op(e, None, False)
            for d in lasts:
                if (not d.dma) and d.eng == e:
                    continue
                d.sig = True
                op.deps.append(d)
            self.ops[e].append(op)
        self.tab = {}

    def emit(self):
        nc = self.nc
        for e in ENGS:
            n = 0
            for op in self.ops[e]:
                if not op.dma and op.sig:
                    n += 1
                    op.sem = self.csem[e]
                    op.val = n
        with nc.Block() as block:
            def run(e, eng):
                seen = {}
                for op in self.ops[e]:
                    need = {}
                    for d in op.deps:
                        k = id(d.sem)
                        if d.val > seen.get(k, 0) and d.val > need.get(k, (None, 0))[1]:
                            need[k] = (d.sem, d.val)
                    for k, (s, v) in need.items():
                        eng.wait_ge(s, v)
                        seen[k] = v
                    if op.fn is not None:
                        ins = op.fn(eng)
                        if op.sig:
                            ins.then_inc(op.sem, 16 if op.dma else 1)

            @block.sync
            def _(eng):
                run("sp", eng)

            @block.scalar
            def _(eng):
                run("act", eng)

            @block.vector
            def _(eng):
                run("dve", eng)

            @block.gpsimd
            def _(eng):
                run("pool", eng)

            @block.tensor
            def _(eng):
                run("pe", eng)


class B:
    __slots__ = ("ap", "key")

    def __init__(self, ap, key):
        self.ap, self.key = ap, key


class Ctx:
    def __init__(self, nc, es, cfg):
        self.nc, self.cfg = nc, cfg
        self.p = Prog(nc, es)
        self.arena = es.enter_context(nc.sbuf_tensor("arena", [128, ARENA], F32))
        self.psum = es.enter_context(nc.psum_tensor("psum", [128, 4096], F32))
        self.top = 0
        self.uid = 0
        self.pers = 0

    def alloc(self, name, n, dt=F32):
        words = n if dt == F32 else (n + 1) // 2
        words = (words + 7) // 8 * 8
        assert self.top + words <= ARENA, (name, self.top, words)
        ap = self.arena[:, self.top:self.top + words]
        if dt != F32:
            ap = ap.bitcast(dt)[:, 0:n]
        else:
            ap = ap[:, 0:n]
        self.top += words
        self.uid += 1
        return B(ap, (f"{name}#{self.uid}", None))

    def bank(self, i, name=None):
        return B(self.psum[:, i * 512:(i + 1) * 512], (f"bank{i}", None))

    def stage_end(self):
        self.p.barrier()
        self.top = self.pers

    def op(self, eng, fn, reads, writes, dma=False):
        return self.p.add(eng, fn, [b.key for b in reads], [b.key for b in writes], dma)

    def dma(self, q, out, in_, outb, inb, **kw):
        return self.op(q, lambda e: e.dma_start(out=out, in_=in_, **kw), [inb], [outb], dma=True)

    def mm(self, out, lhsT, rhs, outb, inbs, start=True, stop=True):
        return self.op("pe", lambda e: e.matmul(out, lhsT, rhs, start=start, stop=stop), inbs,
                       [outb] if start else [outb])

    def tr(self, out, in_, ident, outb, inbs):
        return self.op("pe", lambda e: e.transpose(out, in_, ident), inbs, [outb])

    def act(self, out, in_, func, outb, inbs, bias=None, scale=None, accum=None, accb=None, eng="act"):
        kw = {}
        if bias is not None:
            kw["bias"] = bias
        if scale is not None:
            kw["scale"] = scale
        if accum is not None:
            kw["accum_out"] = accum
        return self.op(eng, lambda e: e.activation(out=out, in_=in_, func=func, **kw), inbs,
                       [outb] + ([accb] if accb is not None else []))

    def ts(self, eng, out, in0, s1, s2, op0, op1, outb, inbs):
        if op1 is None:
            return self.op(eng, lambda e: e.tensor_scalar(out=out, in0=in0, scalar1=s1, scalar2=None, op0=op0),
                           inbs, [outb])
        return self.op(eng, lambda e: e.tensor_scalar(out=out, in0=in0, scalar1=s1, scalar2=s2, op0=op0, op1=op1),
                       inbs, [outb])

    def tt(self, eng, out, in0, in1, op, outb, inbs):
        return self.op(eng, lambda e: e.tensor_tensor(out=out, in0=in0, in1=in1, op=op), inbs, [outb])

    def stt(self, eng, out, in0, scalar, in1, op0, op1, outb, inbs):
        return self.op(eng, lambda e: e.scalar_tensor_tensor(out=out, in0=in0, scalar=scalar, in1=in1,
                                                             op0=op0, op1=op1), inbs, [outb])

    def copy(self, eng, out, in_, outb, inbs):
        if eng == "act":
            return self.op(eng, lambda e: e.copy(out=out, in_=in_), inbs, [outb])
        return self.op(eng, lambda e: e.tensor_copy(out=out, in_=in_), inbs, [outb])

    def memset(self, eng, ap, val, outb):
        return self.op(eng, lambda e: e.memset(ap, val), [], [outb])


def dram_in(nc, name, shape):
    return nc.dram_tensor(name, list(shape), F32, kind="ExternalInput")


def build(cfg):
    nc = bass.Bass("TRN2", target_bir_lowering=False)
    es = ExitStack()
    T = {}
    shapes = input_shapes(cfg)
    for k, s in shapes.items():
        T[k] = dram_in(nc, k, s)
    out = nc.dram_tensor("out", [cfg.seq, D], F32, kind="ExternalOutput")
    with es:
        c = Ctx(nc, es, cfg)
        c.T = T
        c.out = out
        c.Dk = {k: B(None, ("dram_" + k, None)) for k in T}
        c.Dk["out"] = B(None, ("dram_out", None))
        c.P = nc.dram_tensor("P_s", [INC, cfg.npos], F32, kind="Internal")
        c.Pk = B(None, ("P_s", None))
        stage_consts(c)
        stage_adaln(c)
        nch = (RWC + 127) // 128
        c.ZS = nc.dram_tensor("ZS_s", [nch * 128, cfg.npos], F32, kind="Internal")
        c.ZSk = B(None, ("ZS_s", None))
        c.YM = nc.dram_tensor("YM_s", [D, cfg.seq], BF16, kind="Internal")
        c.YMk = B(None, ("YM_s", None))
        if cfg.stages >= 1:
            stage_inproj(c)
        if cfg.stages >= 2:
            stage_shift(c)
        if cfg.stages >= 3:
            stage_s5(c)
        c.LO = nc.dram_tensor("LO_s", [5, 8, 128, cfg.npos], F32, kind="Internal")
        c.LOk = B(None, ("LO_s", None))
        c.RW = nc.dram_tensor("RW_s", [2, 8, 128, 6, cfg.npos], F32, kind="Internal")
        c.RWk = B(None, ("RW_s", None))
        c.YD = nc.dram_tensor("YD_s", [2, cfg.seq, 1024], F32, kind="Internal")
        c.YDk = B(None, ("YD_s", None))
        if cfg.stages >= 4:
            stage_rw_lora(c)
        if cfg.stages >= 5:
            stage_rw(c)
        c.X1 = nc.dram_tensor("X1_s", [cfg.seq, D], F32, kind="Internal")
        c.X1k = B(None, ("X1_s", None))
        c.FT = nc.dram_tensor("FT_s", [D, cfg.seq], BF16, kind="Internal")
        c.FTk = B(None, ("FT_s", None))
        c.GT = nc.dram_tensor("GT_s", [32, cfg.seq], F32, kind="Internal")
        c.GTk = B(None, ("GT_s", None))
        if cfg.stages >= 6:
            stage_outproj(c)
        if cfg.stages >= 7:
            stage_moe(c)
        c.p.barrier()
        c.p.emit()
    return nc


def input_shapes(cfg):
    return {
        "x": (cfg.seq, D), "ctx": (cfg.ctx, D), "cc": (32, 128),
        "mod_w": (D, 6 * D), "mod_b": (96, 128), "norm1_g": (16, 128), "w_in": (D, INC),
        "rw_mu": (4 * 28, 128),
        "s5_are": (64, 128), "s5_aim": (64, 128), "s5_ldt": (64, 128), "s5_d": (8, 128), "s5_glub": (8, 128),
        "s5_bre": (2 * 64 * 64, 16), "s5_bim": (2 * 64 * 64, 16), "s5_cre": (2 * 64 * 16, 64), "s5_cim": (2 * 64 * 16, 64),
        "s5_gluw": (1024, 16),
        "rw_w0": (16, 128), "rw_a0": (16, 128), "rw_w2": (128, 1024), "rw_a2": (128, 1024), "rw_g2": (160, 1024),
        "rw_kk": (8, 128), "rw_ka": (8, 128), "rw_rk": (8, 128), "rw_lnw": (1, 1024), "rw_lnb": (1, 1024),
        "w_out": (D, D), "norm2_g": (16, 128), "router_w": (D, 32), "router_b": (1, 32), "final_g": (16, 128),
        "exp_wgu": (NEXP, D, 2 * cfg.dexp), "exp_bgu": (NEXP * 2 * cfg.dexp // 128, 128),
        "exp_wdn": (NEXP, cfg.dexp, D), "exp_bdn": (NEXP, D),
    }


def stage_consts(c):
    nc = c.nc
    ident = c.alloc("ident", 128)
    c.ident = ident
    c.memset("pool", ident.ap, 1.0, ident)
    c.op("pool", lambda e: e.affine_select(out=ident.ap, in_=ident.ap, pattern=[[-1, 128]],
                                           compare_op=ALU.is_equal, fill=0.0, base=0, channel_multiplier=1),
         [ident], [ident])
    onesrow = c.alloc("onesrow", 128)
    c.memset("pool", onesrow.ap, 1.0, onesrow)
    c.onesrow = onesrow
    ones128 = c.alloc("ones128", 128)
    c.memset("pool", ones128.ap, 1.0, ones128)
    c.ones128 = ones128
    c.pers = c.top


def load_colvec(c, src_ap, srcb, nrows, name, bank=7):
    tmp = c.alloc(name + "_r", 128)
    res = c.alloc(name, nrows)
    c.dma("sp", tmp.ap[0:nrows, :], src_ap, tmp, srcb)
    pb = c.bank(bank)
    c.tr(pb.ap[:, 0:nrows], tmp.ap[0:nrows, :], c.ident.ap[0:nrows, 0:nrows], pb, [tmp, c.ident])
    c.copy("dve", res.ap, pb.ap[:, 0:nrows], res, [pb])
    return res


def stage_adaln(c):
    T = c.T
    mT = c.alloc("mT", 96 * 2)
    g1 = load_colvec(c, T["norm1_g"].ap(), c.Dk["norm1_g"], 16, "g1T")
    c.mT, c.g1 = mT, g1
    scl1 = c.alloc("scl1", 32)
    c.scl1 = scl1
    c.fgT = load_colvec(c, T["final_g"].ap(), c.Dk["final_g"], 16, "fgT")
    c.pers = c.top
    bT = load_colvec(c, T["mod_b"].ap(), c.Dk["mod_b"], 96, "modbT")
    craw = c.alloc("craw", 128)
    c.dma("sp", craw.ap[0:32, :], T["cc"].ap(), craw, c.Dk["cc"])
    c.act(craw.ap[0:32, :], craw.ap[0:32, :], AF.Silu, craw, [craw])
    pb = c.bank(6)
    c.tr(pb.ap[:, 0:32], craw.ap[0:32, :], c.ident.ap[0:32, 0:32], pb, [craw, c.ident])
    sT = c.alloc("siluT", 32)
    c.copy("dve", sT.ap, pb.ap[:, 0:32], sT, [pb])
    sTv = sT.ap.rearrange("p (r j) -> p j r", r=2)
    mTv = mT.ap.rearrange("p (o r) -> p o r", r=2)
    wbuf = [c.alloc(f"modw{i}", 16 * 512) for i in range(2)]
    mw = T["mod_w"].ap().rearrange("(kc p) n -> p kc n", p=128)
    for blk in range(24):
        wb = wbuf[blk % 2]
        wv = wb.ap.rearrange("p (kc n) -> p kc n", kc=16)
        c.dma("sp", wv, mw[:, :, blk * 512:(blk + 1) * 512], wb, c.Dk["mod_w"])
        ps = c.bank(blk % 2)
        for o in range(4):
            for kc in range(16):
                c.mm(ps.ap[:, o * 2:o * 2 + 2], wv[:, kc, o * 128:(o + 1) * 128], sTv[:, kc, :], ps, [wb, sT],
                     start=(kc == 0), stop=(kc == 15))
        o0 = blk * 4
        c.tt("dve", mTv[:, o0:o0 + 4, :], ps.ap[:, 0:8].rearrange("p (o r) -> p o r", r=2),
             bT.ap[:, o0:o0 + 4].unsqueeze(2).to_broadcast([128, 4, 2]), ALU.add, mT, [ps, bT])
    for r in range(2):
        c.stt("dve", scl1.ap[:, r * 16:(r + 1) * 16], mTv[:, 16:32, r], 1.0, g1.ap, ALU.add, ALU.mult,
              scl1, [mT, g1])
    c.stage_end()


def stage_inproj(c):
    cfg, T = c.cfg, c.T
    mTv = c.mT.ap.rearrange("p (o r) -> p o r", r=2)
    ntile = cfg.ntok // 128
    nct = cfg.ctx // 128
    xt = [c.alloc(f"xt{i}", D) for i in range(2)]
    junk = c.alloc("junk", D, BF16)
    ssq = c.alloc("ssq", 8)
    xnT = c.alloc("xnT", 16 * 512, BF16)
    xnv = xnT.ap.rearrange("p (kc n) -> p kc n", kc=16)
    wst = [c.alloc(f"wst{i}", 16 * 128) for i in range(2)]
    wbf = [c.alloc(f"wbf{i}", 16 * 128, BF16) for i in range(2)]
    pst = [c.alloc(f"pst{i}", 512) for i in range(2)]
    win = T["w_in"].ap().rearrange("(kc p) n -> p kc n", p=128)
    P = c.P.ap()
    nblk = (ntile + 3) // 4
    gi = 0
    ci = 0
    for blk in range(nblk):
        t0 = blk * 4
        nt = min(4, ntile - t0)
        for ti in range(nt):
            g = t0 + ti
            isctx = g < nct
            r = 1 if isctx else 0
            src = T["ctx"].ap()[g * 128:(g + 1) * 128, :] if isctx else \
                T["x"].ap()[(g - nct) * 128:(g - nct + 1) * 128, :]
            xb = xt[gi % 2]
            gi += 1
            c.dma("sp", xb.ap, src, xb, c.Dk["ctx" if isctx else "x"])
            sq = ssq.ap[:, (g % 8):(g % 8) + 1]
            c.act(junk.ap, xb.ap, AF.Square, junk, [xb], accum=sq, accb=ssq)
            c.op("dve", lambda e, sq=sq: e.tensor_scalar(out=sq, in0=sq, scalar1=1.0 / D, scalar2=1e-5,
                                                         op0=ALU.mult, op1=ALU.add), [junk, ssq], [ssq])
            c.act(sq, sq, AF.Sqrt, ssq, [ssq])
            c.op("dve", lambda e, sq=sq: e.reciprocal(out=sq, in_=sq), [ssq], [ssq])
            c.op("dve", lambda e, sq=sq, xb=xb: e.tensor_scalar(out=xb.ap, in0=xb.ap, scalar1=sq, scalar2=None,
                                                                op0=ALU.mult), [ssq, xb], [xb])
            for q in range(4):
                ps = c.bank(q + 4 * (g % 2))
                for jj in range(4):
                    j = q * 4 + jj
                    c.tr(ps.ap[:, jj * 128:(jj + 1) * 128], xb.ap[:, j * 128:(j + 1) * 128], c.ident.ap,
                         ps, [xb, c.ident])
                for jj in range(4):
                    j = q * 4 + jj
                    c.act(xnv[:, j, ti * 128:(ti + 1) * 128], ps.ap[:, jj * 128:(jj + 1) * 128], AF.Identity,
                          xnT, [ps, c.scl1, c.mT], bias=mTv[:, j, r:r + 1], scale=c.scl1.ap[:, r * 16 + j:r * 16 + j + 1])
        n = nt * 128
        p0 = t0 * 128
        for cc in range((INC + 127) // 128):
            ncol = min(128, INC - cc * 128)
            ws, wb, po = wst[ci % 2], wbf[ci % 2], pst[ci % 2]
            ci += 1
            wsv = ws.ap.rearrange("p (kc n) -> p kc n", kc=16)
            wbv = wb.ap.rearrange("p (kc n) -> p kc n", kc=16)
            c.dma("sp", wsv[:, :, 0:ncol], win[:, :, cc * 128:cc * 128 + ncol], ws, c.Dk["w_in"])
            c.copy("pool", wbv[:, :, 0:ncol], wsv[:, :, 0:ncol], wb, [ws])
            ps = c.bank(ci % 2)
            for kc in range(16):
                c.mm(ps.ap[0:ncol, 0:n], wbv[:, kc, 0:ncol], xnv[:, kc, 0:n], ps, [wb, xnT],
                     start=(kc == 0), stop=(kc == 15))
            c.copy("dve", po.ap[0:ncol, 0:n], ps.ap[0:ncol, 0:n], po, [ps])
            segs = []
            a, b_ = p0, p0 + n
            if a < cfg.ctx:
                e = min(b_, cfg.ctx)
                segs.append((a, e, a))
                segs.append((a, e, cfg.ctx + cfg.seq + a))
                a = e
            if a < b_:
                segs.append((a, b_, a))
            for (sa, sb, dpos) in segs:
                c.dma("pool", P[cc * 128:cc * 128 + ncol, dpos:dpos + (sb - sa)],
                      po.ap[0:ncol, sa - p0:sb - p0], c.Pk, po)
    c.stage_end()


def regions(cfg):
    return [(0, cfg.ctx, 1, cfg.ctx), (cfg.ctx, cfg.seq, cfg.nrow, 64), (cfg.ctx + cfg.seq, cfg.ctx, 1, cfg.ctx)]


def stage_shift(c):
    cfg, T = c.cfg, c.T
    nch = (RWC + 127) // 128
    muT = load_colvec(c, T["rw_mu"].ap(), c.Dk["rw_mu"], 4 * nch, "muT")
    P, ZS = c.P.ap(), c.ZS.ap()
    zb = [c.alloc(f"z{i}", cfg.npos) for i in range(2)]
    ob = [c.alloc(f"zo{i}", cfg.npos) for i in range(2)]
    tmp = c.alloc("ztmp", cfg.npos)
    for cc in range(nch):
        ncol = min(128, RWC - cc * 128)
        z, o = zb[cc % 2], ob[cc % 2]
        c.dma("sp", z.ap[0:ncol, :], P[S5W + cc * 128:S5W + cc * 128 + ncol, :], z, c.Pk)
        c.copy("act", o.ap[0:ncol, :], z.ap[0:ncol, :], o, [z])
        for (a, ln, rows, w) in regions(cfg):
            zv = z.ap[0:ncol, a:a + ln].rearrange("p (r w) -> p r w", w=w)
            ov = o.ap[0:ncol, a:a + ln].rearrange("p (r w) -> p r w", w=w)
            tv = tmp.ap[0:ncol, a:a + ln].rearrange("p (r w) -> p r w", w=w)
            pairs = [(0, (slice(None), slice(1, w)), (slice(None), slice(0, w - 1))),
                     (1, (slice(None), slice(0, w - 1)), (slice(None), slice(1, w)))]
            if rows > 1:
                pairs += [(2, (slice(1, rows), slice(None)), (slice(0, rows - 1), slice(None))),
                          (3, (slice(0, rows - 1), slice(None)), (slice(1, rows), slice(None)))]
            for (j, dst, src) in pairs:
                mu = muT.ap[0:ncol, j * nch + cc:j * nch + cc + 1]
                c.tt("dve", tv[:, dst[0], dst[1]], zv[:, src[0], src[1]], zv[:, dst[0], dst[1]], ALU.subtract,
                     tmp, [z])
                c.stt("dve", ov[:, dst[0], dst[1]], tv[:, dst[0], dst[1]], mu, ov[:, dst[0], dst[1]],
                      ALU.mult, ALU.add, o, [tmp, muT, o])
        c.dma("pool", ZS[cc * 128:cc * 128 + ncol, :], o.ap[0:ncol, :], c.ZSk, o)
    c.stage_end()


def stage_s5(c):
    cfg, T = c.cfg, c.T
    L = cfg.ntok
    P = c.P.ap()
    TWO_PI = 2.0 * math.pi
    are = load_colvec(c, T["s5_are"].ap(), c.Dk["s5_are"], 64, "are")
    aim = load_colvec(c, T["s5_aim"].ap(), c.Dk["s5_aim"], 64, "aim")
    ldt = load_colvec(c, T["s5_ldt"].ap(), c.Dk["s5_ldt"], 64, "ldt")
    dT = load_colvec(c, T["s5_d"].ap(), c.Dk["s5_d"], 8, "s5dT")
    gbT = load_colvec(c, T["s5_glub"].ap(), c.Dk["s5_glub"], 8, "s5gbT")
    xr = c.alloc("xr", 64); xi = c.alloc("xi", 64); mag = c.alloc("mag", 64)
    t1 = c.alloc("t1", 64); t2 = c.alloc("t2", 64)
    NP = 13
    while (1 << (NP - 1)) >= L:
        NP -= 1
    pr = c.alloc("pw_re", NP * 64); pi_ = c.alloc("pw_im", NP * 64); pn = c.alloc("pw_nim", NP * 64)
    cr = c.alloc("coef_re", 64); ci = c.alloc("coef_im", 64); nci = c.alloc("coef_nim", 64)
    c.act(ldt.ap, ldt.ap, AF.Exp, ldt, [ldt])
    c.tt("dve", xr.ap, are.ap, ldt.ap, ALU.mult, xr, [are, ldt])
    c.tt("dve", xi.ap, aim.ap, ldt.ap, ALU.mult, xi, [aim, ldt])
    c.ts("dve", t1.ap, xi.ap, 1.0 / 16, None, ALU.mult, None, t1, [xi])
    c.ts("dve", t2.ap, xi.ap, 1.0 / 16, math.pi / 2, ALU.mult, ALU.add, t2, [xi])
    c.act(t1.ap, t1.ap, AF.Sin, t1, [t1])
    c.act(t2.ap, t2.ap, AF.Sin, t2, [t2])
    c.act(mag.ap, xr.ap, AF.Exp, mag, [xr], scale=1.0 / 16)
    br_ = c.alloc("lb_r", 64); bi_ = c.alloc("lb_i", 64); tq = c.alloc("lb_t", 64)
    c.tt("dve", br_.ap, mag.ap, t2.ap, ALU.mult, br_, [mag, t2])
    c.tt("dve", bi_.ap, mag.ap, t1.ap, ALU.mult, bi_, [mag, t1])
    for q in range(4):
        dr_, di_ = (pr.ap[:, 0:64], pi_.ap[:, 0:64]) if q == 3 else (br_.ap, bi_.ap)
        drb, dib = (pr, pi_) if q == 3 else (br_, bi_)
        c.tt("dve", t1.ap, br_.ap, br_.ap, ALU.mult, t1, [br_])
        c.tt("dve", t2.ap, bi_.ap, bi_.ap, ALU.mult, t2, [bi_])
        c.tt("dve", tq.ap, br_.ap, bi_.ap, ALU.mult, tq, [br_, bi_])
        c.tt("dve", dr_, t1.ap, t2.ap, ALU.subtract, drb, [t1, t2])
        c.ts("dve", di_, tq.ap, 2.0, None, ALU.mult, None, dib, [tq])
    c.ts("dve", t1.ap, pr.ap[:, 0:64], -1.0, None, ALU.add, None, t1, [pr])
    c.tt("dve", t2.ap, are.ap, are.ap, ALU.mult, t2, [are])
    c.tt("dve", xr.ap, aim.ap, aim.ap, ALU.mult, xr, [aim])
    c.tt("dve", t2.ap, t2.ap, xr.ap, ALU.add, t2, [t2, xr])
    c.op("dve", lambda e: e.reciprocal(out=t2.ap, in_=t2.ap), [t2], [t2])
    c.tt("dve", cr.ap, t1.ap, are.ap, ALU.mult, cr, [t1, are])
    c.tt("dve", xr.ap, pi_.ap[:, 0:64], aim.ap, ALU.mult, xr, [pi_, aim])
    c.tt("dve", cr.ap, cr.ap, xr.ap, ALU.add, cr, [cr, xr])
    c.tt("dve", cr.ap, cr.ap, t2.ap, ALU.mult, cr, [cr, t2])
    c.tt("dve", ci.ap, pi_.ap[:, 0:64], are.ap, ALU.mult, ci, [pi_, are])
    c.tt("dve", xr.ap, t1.ap, aim.ap, ALU.mult, xr, [t1, aim])
    c.tt("dve", ci.ap, ci.ap, xr.ap, ALU.subtract, ci, [ci, xr])
    c.tt("dve", ci.ap, ci.ap, t2.ap, ALU.mult, ci, [ci, t2])
    c.ts("dve", nci.ap, ci.ap, -1.0, None, ALU.mult, None, nci, [ci])
    for j in range(1, NP):
        a, b_ = pr.ap[:, (j - 1) * 64:j * 64], pi_.ap[:, (j - 1) * 64:j * 64]
        c.tt("dve", t1.ap, a, a, ALU.mult, t1, [pr])
        c.tt("dve", t2.ap, b_, b_, ALU.mult, t2, [pi_])
        c.tt("dve", pr.ap[:, j * 64:(j + 1) * 64], t1.ap, t2.ap, ALU.subtract, pr, [t1, t2])
        c.tt("dve", t1.ap, a, b_, ALU.mult, t1, [pr, pi_])
        c.ts("dve", pi_.ap[:, j * 64:(j + 1) * 64], t1.ap, 2.0, None, ALU.mult, None, pi_, [t1])
    c.ts("dve", pn.ap, pi_.ap, -1.0, None, ALU.mult, None, pn, [pi_])
    u = c.alloc("s5u", cfg.npos)
    yacc = c.alloc("s5y", cfg.seq)
    hb = [[c.alloc(f"h{k}{i}", L) for i in range(2)] for k in range(2)]
    braw = [c.alloc(f"braw{i}", 16) for i in range(2)]
    bbar = [c.alloc(f"bbar{i}", 16) for i in range(2)]
    xpad = [c.alloc(f"xpad{i}", 128) for i in range(2)]
    lB = [c.alloc(f"lB{i}", 128) for i in range(2)]
    craw = [c.alloc(f"craw{i}", 128) for i in range(2)]
    lC = [c.alloc(f"lC{i}", 128) for i in range(2)]
    lG = c.alloc("lG", 128)
    gt = c.alloc("s5g", cfg.seq)
    yo = c.alloc("s5o", cfg.seq, BF16)
    nblk_l = (L + 511) // 512
    nblk_s = (cfg.seq + 511) // 512
    bre, bim = T["s5_bre"].ap(), T["s5_bim"].ap()
    cre, cim = T["s5_cre"].ap(), T["s5_cim"].ap()
    gw = T["s5_gluw"].ap()
    YM = c.YM.ap()
    for ch in range(8):
        c.dma("sp", u.ap, P[ch * 128:(ch + 1) * 128, :], u, c.Pk)
        first = True
        for d in range(2):
            off = 0 if d == 0 else cfg.ctx
            for gpl in range(4):
                gp = ch * 4 + gpl
                col = d * 32 + gp
                rowb = (d * 64 + gp * 2) * 64
                c.dma("sp", braw[0].ap, bre[rowb:rowb + 128, :], braw[0], c.Dk["s5_bre"])
                c.dma("sp", braw[1].ap, bim[rowb:rowb + 128, :], braw[1], c.Dk["s5_bim"])
                crc, cic, ncic = cr.ap[:, col:col + 1], ci.ap[:, col:col + 1], nci.ap[:, col:col + 1]
                c.ts("dve", bbar[0].ap, braw[0].ap, crc, None, ALU.mult, None, bbar[0], [braw[0], cr])
                c.stt("dve", bbar[0].ap, braw[1].ap, ncic, bbar[0].ap, ALU.mult, ALU.add, bbar[0], [braw[1], nci, bbar[0]])
                c.ts("dve", bbar[1].ap, braw[1].ap, crc, None, ALU.mult, None, bbar[1], [braw[1], cr])
                c.stt("dve", bbar[1].ap, braw[0].ap, cic, bbar[1].ap, ALU.mult, ALU.add, bbar[1], [braw[0], ci, bbar[1]])
                for k in range(2):
                    c.memset("pool", xpad[k].ap, 0.0, xpad[k])
                    c.copy("pool", xpad[k].ap[0:64, 32 * gpl:32 * gpl + 16], bbar[k].ap[0:64, :], xpad[k], [bbar[k]])
                    c.copy("pool", xpad[k].ap[64:128, 32 * gpl + 16:32 * gpl + 32], bbar[k].ap[64:128, :], xpad[k], [bbar[k]])
                    ps = c.bank(6 + k)
                    c.tr(ps.ap[:, 0:128], xpad[k].ap, c.ident.ap, ps, [xpad[k], c.ident])
                    c.copy("act", lB[k].ap, ps.ap[:, 0:128], lB[k], [ps])
                rowc = (d * 64 + gp * 2) * 16
                for k, src in ((0, cre), (1, cim)):
                    c.memset("pool", craw[k].ap[0:32, :], 0.0, craw[k])
                    c.dma("sp", craw[k].ap[0:16, 0:64], src[rowc:rowc + 16, :], craw[k], c.Dk["s5_cre" if k == 0 else "s5_cim"])
                    c.dma("sp", craw[k].ap[16:32, 64:128], src[rowc + 16:rowc + 32, :], craw[k], c.Dk["s5_cre" if k == 0 else "s5_cim"])
                    ps = c.bank(6 + k)
                    c.tr(ps.ap[:, 0:32], craw[k].ap[0:32, :], c.ident.ap[0:32, 0:32], ps, [craw[k], c.ident])
                    c.memset("pool", lC[k].ap, 0.0, lC[k])
                    if k == 0:
                        c.copy("act", lC[k].ap[:, 32 * gpl:32 * gpl + 32], ps.ap[:, 0:32], lC[k], [ps])
                    else:
                        c.op("act", lambda e, o=lC[k].ap[:, 32 * gpl:32 * gpl + 32], i=ps.ap[:, 0:32]: e.mul(out=o, in_=i, mul=-1.0),
                             [ps], [lC[k]])
                for blk in range(nblk_l):
                    n = min(512, L - blk * 512)
                    for k in range(2):
                        ps = c.bank((blk * 2 + k) % 4)
                        c.mm(ps.ap[:, 0:n], lB[k].ap, u.ap[:, off + blk * 512:off + blk * 512 + n], ps, [lB[k], u])
                        c.copy("act", hb[k][0].ap[:, blk * 512:blk * 512 + n], ps.ap[:, 0:n], hb[k][0], [ps])
                cur = 0
                for j in range(NP):
                    s_ = 1 << j
                    if s_ >= L:
                        break
                    mr = pr.ap[:, j * 64 + col:j * 64 + col + 1]
                    mi = pi_.ap[:, j * 64 + col:j * 64 + col + 1]
                    nmi = pn.ap[:, j * 64 + col:j * 64 + col + 1]
                    sr, si = hb[0][cur], hb[1][cur]
                    dr, di = hb[0][1 - cur], hb[1][1 - cur]
                    if d == 0:
                        dst, sh, keep = slice(s_, L), slice(0, L - s_), slice(0, s_)
                    else:
                        dst, sh, keep = slice(0, L - s_), slice(s_, L), slice(L - s_, L)
                    c.stt("dve", dr.ap[:, dst], sr.ap[:, sh], mr, sr.ap[:, dst], ALU.mult, ALU.add, dr, [sr, pr])
                    c.stt("dve", dr.ap[:, dst], si.ap[:, sh], nmi, dr.ap[:, dst], ALU.mult, ALU.add, dr, [si, pn, dr])
                    c.copy("act", dr.ap[:, keep], sr.ap[:, keep], dr, [sr])
                    c.stt("dve", di.ap[:, dst], si.ap[:, sh], mr, si.ap[:, dst], ALU.mult, ALU.add, di, [si, pr])
                    c.stt("dve", di.ap[:, dst], sr.ap[:, sh], mi, di.ap[:, dst], ALU.mult, ALU.add, di, [sr, pi_, di])
                    c.copy("pool", di.ap[:, keep], si.ap[:, keep], di, [si])
                    cur = 1 - cur
                loff = cfg.ctx if d == 0 else 0
                for blk in range(nblk_s):
                    n = min(512, cfg.seq - blk * 512)
                    ps = c.bank(4 + blk % 2)
                    c.mm(ps.ap[:, 0:n], lC[0].ap, hb[0][cur].ap[:, loff + blk * 512:loff + blk * 512 + n], ps, [lC[0], hb[0][cur]],
                         start=True, stop=False)
                    c.mm(ps.ap[:, 0:n], lC[1].ap, hb[1][cur].ap[:, loff + blk * 512:loff + blk * 512 + n], ps, [lC[1], hb[1][cur]],
                         start=False, stop=True)
                    ysl = yacc.ap[:, blk * 512:blk * 512 + n]
                    if first:
                        c.copy("dve", ysl, ps.ap[:, 0:n], yacc, [ps])
                    else:
                        c.tt("dve", ysl, ysl, ps.ap[:, 0:n], ALU.add, yacc, [ps, yacc])
                first = False
        ul = u.ap[:, cfg.ctx:cfg.ctx + cfg.seq]
        c.stt("dve", yacc.ap, ul, dT.ap[:, ch:ch + 1], yacc.ap, ALU.mult, ALU.add, yacc, [u, dT, yacc])
        c.tt("dve", gt.ap, yacc.ap, yacc.ap, ALU.mult, gt, [yacc])
        c.ts("dve", gt.ap, gt.ap, 0.044715, 1.0, ALU.mult, ALU.add, gt, [gt])
        c.tt("dve", gt.ap, gt.ap, yacc.ap, ALU.mult, gt, [gt, yacc])
        c.act(gt.ap, gt.ap, AF.Tanh, gt, [gt], scale=0.7978845608028654)
        c.ts("dve", gt.ap, gt.ap, 1.0, 0.5, ALU.add, ALU.mult, gt, [gt])
        c.tt("dve", yacc.ap, yacc.ap, gt.ap, ALU.mult, yacc, [yacc, gt])
        c.memset("pool", lG.ap, 0.0, lG)
        for g in range(8):
            c.dma("sp", lG.ap[16 * g:16 * g + 16, 16 * g:16 * g + 16], gw[(ch * 8 + g) * 16:(ch * 8 + g) * 16 + 16, :],
                  lG, c.Dk["s5_gluw"])
        for blk in range(nblk_s):
            n = min(512, cfg.seq - blk * 512)
            ps = c.bank(4 + blk % 2)
            c.mm(ps.ap[:, 0:n], lG.ap, yacc.ap[:, blk * 512:blk * 512 + n], ps, [lG, yacc])
            c.act(gt.ap[:, blk * 512:blk * 512 + n], ps.ap[:, 0:n], AF.Sigmoid, gt, [ps, gbT], bias=gbT.ap[:, ch:ch + 1])
        c.tt("dve", yo.ap, yacc.ap, gt.ap, ALU.mult, yo, [yacc, gt])
        c.dma("pool", YM[ch * 128:(ch + 1) * 128, :], yo.ap, c.YMk, yo)
    c.stage_end()


def stage_rw_lora(c):
    cfg, T = c.cfg, c.T
    ZS, LO = c.ZS.ap(), c.LO.ap()
    NP_ = cfg.npos
    w0T = load_colvec(c, T["rw_w0"].ap(), c.Dk["rw_w0"], 16, "w0T")
    a0T = load_colvec(c, T["rw_a0"].ap(), c.Dk["rw_a0"], 16, "a0T")
    txw = c.alloc("txw", NP_); xa = c.alloc("xa", NP_)
    sg0 = c.alloc("sg0", cfg.seq); sg1 = c.alloc("sg1", cfg.seq)
    w2 = c.alloc("w2", 1024); a2 = c.alloc("a2", 1024); g2a = c.alloc("g2a", 1024); g2b = c.alloc("g2b", 1024)
    c.dma("sp", txw.ap, ZS[3072:3200, :], txw, c.ZSk)
    c.dma("sp", xa.ap, ZS[3200:3328, :], xa, c.ZSk)
    c.dma("sp", sg0.ap, ZS[3328:3456, cfg.ctx:cfg.ctx + cfg.seq], sg0, c.ZSk)
    c.dma("sp", sg1.ap[0:32, :], ZS[3456:3488, cfg.ctx:cfg.ctx + cfg.seq], sg1, c.ZSk)
    c.dma("sp", w2.ap, T["rw_w2"].ap(), w2, c.Dk["rw_w2"])
    c.dma("sp", a2.ap, T["rw_a2"].ap(), a2, c.Dk["rw_a2"])
    c.dma("sp", g2a.ap, T["rw_g2"].ap()[0:128, :], g2a, c.Dk["rw_g2"])
    c.dma("sp", g2b.ap[0:32, :], T["rw_g2"].ap()[128:160, :], g2b, c.Dk["rw_g2"])
    c.act(txw.ap, txw.ap, AF.Tanh, txw, [txw])
    c.act(sg0.ap, sg0.ap, AF.Sigmoid, sg0, [sg0])
    c.act(sg1.ap[0:32, :], sg1.ap[0:32, :], AF.Sigmoid, sg1, [sg1])
    ob = [c.alloc(f"lo{i}", NP_) for i in range(2)]
    oi = 0
    nb = (NP_ + 511) // 512
    for hp in range(8):
        cs = slice(hp * 128, (hp + 1) * 128)
        for kind in range(5):
            o = ob[oi % 2]; oi += 1
            if kind < 4:
                d = kind % 2
                wt, src, bT = (w2, txw, w0T) if kind < 2 else (a2, xa, a0T)
                for blk in range(nb):
                    n = min(512, NP_ - blk * 512)
                    ps = c.bank(blk % 4)
                    c.mm(ps.ap[:, 0:n], wt.ap[d * 64:(d + 1) * 64, cs], src.ap[d * 64:(d + 1) * 64, blk * 512:blk * 512 + n],
                         ps, [wt, src])
                    c.act(o.ap[:, blk * 512:blk * 512 + n], ps.ap[:, 0:n], AF.Sigmoid, o, [ps, bT],
                          bias=bT.ap[:, d * 8 + hp:d * 8 + hp + 1])
                c.dma("pool", LO[kind, hp], o.ap, c.LOk, o)
            else:
                for blk in range((cfg.seq + 511) // 512):
                    n = min(512, cfg.seq - blk * 512)
                    ps = c.bank(blk % 4)
                    c.mm(ps.ap[:, 0:n], g2a.ap[:, cs], sg0.ap[:, blk * 512:blk * 512 + n], ps, [g2a, sg0], start=True, stop=False)
                    c.mm(ps.ap[:, 0:n], g2b.ap[0:32, cs], sg1.ap[0:32, blk * 512:blk * 512 + n], ps, [g2b, sg1], start=False, stop=True)
                    c.copy("act", o.ap[:, blk * 512:blk * 512 + n], ps.ap[:, 0:n], o, [ps])
                c.dma("pool", LO[4, hp, :, 0:cfg.seq], o.ap[:, 0:cfg.seq], c.LOk, o)
    c.stage_end()


def rw_consts(c):
    m = {}
    for name, pat, base, cm, cmp_ in (("SU", 1, 0, -1, ALU.is_gt), ("SL", -1, 0, 1, ALU.is_gt),
                                       ("IU", 1, 0, -1, ALU.is_ge), ("IL", -1, 0, 1, ALU.is_ge)):
        t = c.alloc("mask" + name, 128)
        c.memset("pool", t.ap, 1.0, t)
        c.op("pool", lambda e, t=t, pat=pat, base=base, cm=cm, cmp_=cmp_: e.affine_select(
            out=t.ap, in_=t.ap, pattern=[[pat, 128]], compare_op=cmp_, fill=0.0, base=base, channel_multiplier=cm), [t], [t])
        for j in range(NSUB):
            if j > 0:
                c.memset("pool", t.ap[j * CS:(j + 1) * CS, 0:j * CS], 0.0, t)
            if j < NSUB - 1:
                c.memset("pool", t.ap[j * CS:(j + 1) * CS, (j + 1) * CS:128], 0.0, t)
        m[name] = t
    bo = c.alloc("blockones", 128)
    c.memset("pool", bo.ap, 0.0, bo)
    c.memset("pool", bo.ap[0:64, 0:64], 1.0, bo)
    c.memset("pool", bo.ap[64:128, 64:128], 1.0, bo)
    hs = c.alloc("headsel", 2)
    c.memset("pool", hs.ap, 0.0, hs)
    c.memset("pool", hs.ap[0:64, 0:1], 1.0, hs)
    c.memset("pool", hs.ap[64:128, 1:2], 1.0, hs)
    ones1 = c.alloc("ones1", 128)
    c.memset("pool", ones1.ap[0:1, :], 1.0, ones1)
    m["BO"], m["HS"], m["ONES"] = bo, hs, ones1
    return m


def stage_rw(c):
    cfg, T = c.cfg, c.T
    ZS, LO, RW, YD, YM = c.ZS.ap(), c.LO.ap(), c.RW.ap(), c.YD.ap(), c.YM.ap()
    NP_, L = cfg.npos, cfg.ntok
    nblk = L // 128
    nlat = cfg.seq // 128
    nctx = cfg.ctx // 128
    M = rw_consts(c)
    kkT = load_colvec(c, T["rw_kk"].ap(), c.Dk["rw_kk"], 8, "kkT")
    kaT = load_colvec(c, T["rw_ka"].ap(), c.Dk["rw_ka"], 8, "kaT")
    rkT = load_colvec(c, T["rw_rk"].ap(), c.Dk["rw_rk"], 8, "rkT")
    lnrow = c.alloc("lnrow", 2048)
    c.dma("sp", lnrow.ap[0:1, 0:1024], T["rw_lnw"].ap(), lnrow, c.Dk["rw_lnw"])
    c.dma("sp", lnrow.ap[0:1, 1024:2048], T["rw_lnb"].ap(), lnrow, c.Dk["rw_lnb"])
    lnbc = c.alloc("lnbc", 2048)
    for q in range(4):
        ps = c.bank(q)
        c.mm(ps.ap[:, 0:512], M["ONES"].ap[0:1, :], lnrow.ap[0:1, q * 512:(q + 1) * 512], ps, [M["ONES"], lnrow])
        c.copy("act", lnbc.ap[:, q * 512:(q + 1) * 512], ps.ap[:, 0:512], lnbc, [ps])
    bonus = c.alloc("bonus", nlat * 2)
    gCt = c.alloc("gCt", 2 * (NP_ // CS))
    SEG = 1536 if NP_ % 1536 == 0 else NP_
    rmask = c.alloc("rmask", SEG)
    c.memset("pool", rmask.ap, 1.0, rmask)
    c.memset("pool", rmask.ap[:, 0:SEG:CS], 0.0, rmask)
    c.pers2 = c.top
    nseg = NP_ // SEG
    for hp in range(8):
        c.p.barrier()
        c.top = c.pers2
        names = ["r", "k", "kh", "kka", "sg", "a", "cs", "lg", "kt", "E", "q"]
        S = {n_: c.alloc("f_" + n_, SEG) for n_ in names}
        O = [c.alloc(f"f_o{i}", SEG) for i in range(3)]
        oi = 0
        for sg_i in range(nseg):
            p0 = sg_i * SEG
            psl = slice(p0, p0 + SEG)
            nch = SEG // CS
            c.dma("sp", S["r"].ap, ZS[hp * 128:(hp + 1) * 128, psl], S["r"], c.ZSk)
            c.dma("sp", S["k"].ap, ZS[1024 + hp * 128:1024 + (hp + 1) * 128, psl], S["k"], c.ZSk)
            kkc, kac, rkc = kkT.ap[:, hp:hp + 1], kaT.ap[:, hp:hp + 1], rkT.ap[:, hp:hp + 1]
            c.ts("dve", S["kh"].ap, S["k"].ap, kkc, None, ALU.mult, None, S["kh"], [S["k"], kkT])
            c.tt("dve", S["E"].ap, S["kh"].ap, S["kh"].ap, ALU.mult, S["E"], [S["kh"]])
            for blk in range((SEG + 511) // 512):
                n = min(512, SEG - blk * 512)
                ps = c.bank(blk % 4)
                c.mm(ps.ap[:, 0:n], M["BO"].ap, S["E"].ap[:, blk * 512:blk * 512 + n], ps, [M["BO"], S["E"]])
                c.act(S["cs"].ap[:, blk * 512:blk * 512 + n], ps.ap[:, 0:n], AF.Sqrt, S["cs"], [ps])
            c.ts("dve", S["cs"].ap, S["cs"].ap, 1e-12, None, ALU.max, None, S["cs"], [S["cs"]])
            c.op("dve", lambda e, o=S["cs"].ap: e.reciprocal(out=o, in_=o), [S["cs"]], [S["cs"]])
            c.tt("dve", S["kh"].ap, S["kh"].ap, S["cs"].ap, ALU.mult, S["kh"], [S["kh"], S["cs"]])
            c.ts("dve", S["kka"].ap, S["k"].ap, kac, None, ALU.mult, None, S["kka"], [S["k"], kaT])
            for d in range(2):
                c.dma("sp", S["sg"].ap, LO[d, hp, :, psl], S["sg"], c.LOk)
                c.dma("sp", S["a"].ap, LO[2 + d, hp, :, psl], S["a"], c.LOk)
                c.ts("dve", S["sg"].ap, S["sg"].ap, -math.exp(-0.5), None, ALU.mult, None, S["sg"], [S["sg"]])
                csv = S["cs"].ap.rearrange("p (c t) -> p c t", t=CS)
                sgv = S["sg"].ap.rearrange("p (c t) -> p c t", t=CS)
                lgv = S["lg"].ap.rearrange("p (c t) -> p c t", t=CS)
                c.op("dve", lambda e, o=S["cs"].ap, i=S["sg"].ap: e.tensor_tensor_scan(
                    out=o, data0=rmask.ap, data1=i, initial=0.0, op0=ALU.mult, op1=ALU.add),
                    [S["sg"], rmask], [S["cs"]])
                ctot = csv[:, :, CS - 1:CS]
                if d == 0:
                    c.copy("act", S["lg"].ap, S["cs"].ap, S["lg"], [S["cs"]])
                else:
                    c.tt("dve", lgv, sgv, csv, ALU.subtract, S["lg"], [S["sg"], S["cs"]])
                    c.tt("dve", lgv, lgv, ctot.to_broadcast([128, nch, CS]), ALU.add, S["lg"], [S["lg"], S["cs"]])
                gcs = gCt.ap[:, d * (NP_ // CS) + p0 // CS:d * (NP_ // CS) + p0 // CS + nch]
                c.act(gcs, S["cs"].ap[:, CS - 1:SEG:CS], AF.Exp, gCt, [S["cs"]])
                c.stt("dve", S["kt"].ap, S["a"].ap, -1.0, S["kka"].ap, ALU.add, ALU.mult, S["kt"], [S["a"], S["kka"]])
                c.tt("dve", S["kt"].ap, S["kt"].ap, S["k"].ap, ALU.add, S["kt"], [S["kt"], S["k"]])
                c.tt("dve", S["a"].ap, S["a"].ap, S["kh"].ap, ALU.mult, S["a"], [S["a"], S["kh"]])
                if d == 0:
                    c.stt("dve", S["q"].ap, S["r"].ap, rkc, S["kt"].ap, ALU.mult, ALU.mult, S["q"], [S["r"], rkT, S["kt"]])
                else:
                    c.stt("dve", S["E"].ap, S["r"].ap, rkc, S["kt"].ap, ALU.mult, ALU.mult, S["E"], [S["r"], rkT, S["kt"]])
                    c.tt("dve", S["q"].ap, S["q"].ap, S["E"].ap, ALU.add, S["q"], [S["q"], S["E"]])

                def emit_out(idx, base, ex, neg):
                    nonlocal oi
                    o = O[oi % 3]; oi += 1
                    c.tt("dve", o.ap, base.ap, ex.ap, ALU.mult, o, [base, ex])
                    if neg:
                        c.op("act", lambda e, o=o: e.mul(out=o.ap, in_=o.ap, mul=-1.0), [o], [o])
                    c.dma("pool", RW[d, hp, :, idx, psl], o.ap, c.RWk, o)
                c.act(S["E"].ap, S["lg"].ap, AF.Exp, S["E"], [S["lg"]], scale=-1.0)
                emit_out(0, S["a"], S["E"], True)
                emit_out(2, S["kt"], S["E"], False)
                c.act(S["E"].ap, S["lg"].ap, AF.Exp, S["E"], [S["lg"]])
                emit_out(3, S["r"], S["E"], False)
                c.tt("dve", S["E"].ap, S["lg"].ap, S["sg"].ap, ALU.subtract, S["E"], [S["lg"], S["sg"]])
                c.act(S["E"].ap, S["E"].ap, AF.Exp, S["E"], [S["E"]])
                emit_out(1, S["kh"], S["E"], False)
                Ev = S["E"].ap.rearrange("p (c t) -> p c t", t=CS)
                c.tt("dve", Ev, ctot.to_broadcast([128, nch, CS]), lgv, ALU.subtract, S["E"], [S["cs"], S["lg"]])
                c.act(S["E"].ap, S["E"].ap, AF.Exp, S["E"], [S["E"]])
                emit_out(4, S["a"], S["E"], True)
                emit_out(5, S["kt"], S["E"], False)
            for bl in range(SEG // 128):
                gpos = p0 + bl * 128
                if gpos < cfg.ctx or gpos >= cfg.ctx + cfg.seq:
                    continue
                lb = (gpos - cfg.ctx) // 128
                ps = c.bank(4 + bl % 2)
                c.mm(ps.ap[:, 0:2], S["q"].ap[:, bl * 128:(bl + 1) * 128], M["HS"].ap, ps, [S["q"], M["HS"]])
                c.copy("act", bonus.ap[:, lb * 2:lb * 2 + 2], ps.ap[:, 0:2], bonus, [ps])
        c.p.barrier()
        c.top = c.pers2
        rw_chain(c, hp, M, gCt)
        c.p.barrier()
        c.top = c.pers2
        rw_readout(c, hp, M, bonus, lnbc)
    c.stage_end()


def rw_chain(c, hp, M, gCt):
    cfg = c.cfg
    ZS, RW, YD = c.ZS.ap(), c.RW.ap(), c.YD.ap()
    NP_, L = cfg.npos, cfg.ntok
    nblk = L // 128
    NG = 1
    ident = c.ident
    H = [[c.alloc(f"H{d}{hl}", 64) for hl in range(2)] for d in range(2)]
    for d in range(2):
        for hl in range(2):
            c.memset("pool", H[d][hl].ap[0:64, :], 0.0, H[d][hl])
    nset = 2
    gC1 = c.alloc("gC1", 2 * (NP_ // CS))
    c.dma("sp", gC1.ap[0:64, :], gCt.ap[64:128, :], gC1, gCt)
    gC = [gCt, gC1]
    def mk(name, n):
        return [[[c.alloc(f"{name}{s_}{g}{d}", n) for d in range(2)] for g in range(NG)] for s_ in range(nset)]
    OP = [[[[c.alloc(f"op{s_}{g}{d}{hl}", 6 * 128) for hl in range(2)] for d in range(2)] for g in range(NG)] for s_ in range(nset)]
    VF = mk("vf", 128)
    VT = mk("vt", 128)
    TM = [[[[c.alloc(f"tm{s_}{g}{d}{hl}", 192) for hl in range(2)] for d in range(2)] for g in range(NG)] for s_ in range(nset)]
    RES = [[[[c.alloc(f"res{s_}{g}{d}{hl}", 2 * NSUB * 64 + 192) for hl in range(2)] for d in range(2)] for g in range(NG)] for s_ in range(nset)]
    TMP = [[[[[c.alloc(f"t{s_}{g}{d}{hl}{i}", 128) for i in range(9)] for hl in range(2)] for d in range(2)] for g in range(NG)] for s_ in range(nset)]
    YO = [[c.alloc(f"yo{s_}{d}", 128) for d in range(2)] for s_ in range(4)]
    slot_ctr = [0]

    def unit(s_, g, d, hl, blk_pos, islat, slotbase, gci0, gcb):
        op = OP[s_][g][d][hl]
        opv = op.ap.rearrange("p (i t) -> p i t", i=6)
        At, Bt, Kt, Rt, Ab, Kb = (opv[0:64, i, :] for i in range(6))
        tm = TM[s_][g][d][hl]
        Btm, Abm, Kbm = tm.ap[:, 0:64], tm.ap[:, 64:128], tm.ap[:, 128:192]
        Vtm = VT[s_][g][d].ap[:, hl * 64:(hl + 1) * 64]
        vtb = VT[s_][g][d]
        res = RES[s_][g][d][hl]
        PhiT, Psi = res.ap[0:64, 0:NSUB * 64], res.ap[0:64, NSUB * 64:2 * NSUB * 64]
        PT, Y2 = res.ap[0:64, 2 * NSUB * 64:2 * NSUB * 64 + 128], res.ap[:, 2 * NSUB * 64 + 128:2 * NSUB * 64 + 192]
        t = TMP[s_][g][d][hl]
        strict = M["SU"] if d == 0 else M["SL"]
        strictT = M["SL"] if d == 0 else M["SU"]
        incl = M["IU"] if d == 0 else M["IL"]

        def slot(i):
            bk = c.bank(slotbase)
            return B(bk.ap[:, i * 128:(i + 1) * 128], bk.key)
        s0, s1 = slot(0), slot(1)
        for src, dst in ((Bt, Btm), (Ab, Abm), (Kb, Kbm)):
            c.tr(s0.ap[:, 0:64], src, ident.ap[0:64, 0:64], s0, [op, ident])
            c.copy("act", dst, s0.ap[:, 0:64], tm, [s0])
            yield
        LAT, LA, LKT, MAT, MKT = t[0], t[1], t[4], t[5], t[6]
        c.mm(s0.ap, At, Bt, s0, [op]); c.tt("dve", LAT.ap, s0.ap, strict.ap, ALU.mult, LAT, [s0, strict])
        c.mm(s1.ap, Bt, At, s1, [op]); c.tt("dve", LA.ap, s1.ap, strictT.ap, ALU.mult, LA, [s1, strictT])
        yield
        c.mm(s0.ap, Kt, Bt, s0, [op]); c.tt("dve", LKT.ap, s0.ap, strict.ap, ALU.mult, LKT, [s0, strict])
        if islat:
            c.mm(s1.ap, At, Rt, s1, [op]); c.tt("dve", MAT.ap, s1.ap, incl.ap, ALU.mult, MAT, [s1, incl])
        yield
        if islat:
            c.mm(s0.ap, Kt, Rt, s0, [op]); c.tt("dve", MKT.ap, s0.ap, incl.ap, ALU.mult, MKT, [s0, incl])
        Z = [t[7], t[8]]
        c.mm(s1.ap[:, 0:64], LKT.ap, Vtm, s1, [LKT, vtb])
        c.copy("act", Z[0].ap[:, 64:128], s1.ap[:, 0:64], Z[0], [s1])
        c.copy("act", Z[0].ap[:, 0:64], Btm, Z[0], [tm])
        yield
        X, XT = LA, LAT
        alt = [(t[2], t[3]), (t[1], t[0])]
        zc = 0
        NL = int(math.log2(CS))
        for i in range(NL):
            c.mm(s0.ap, XT.ap, Z[zc].ap, s0, [XT, Z[zc]])
            c.tt("dve", Z[1 - zc].ap, Z[zc].ap, s0.ap, ALU.add, Z[1 - zc], [Z[zc], s0])
            zc = 1 - zc
            if i < NL - 1:
                nX, nXT = alt[i % 2]
                c.mm(s1.ap, X.ap, XT.ap, s1, [X, XT])
                c.copy("act", nXT.ap, s1.ap, nXT, [s1])
                if i < NL - 2:
                    yield
                    c.mm(s1.ap, XT.ap, X.ap, s1, [X, XT])
                    c.copy("act", nX.ap, s1.ap, nX, [s1])
                X, XT = nX, nXT
            yield
        Zf = Z[zc]
        W1, U2 = Zf.ap[:, 0:64], Zf.ap[:, 64:128]
        for j in range(NSUB):
            js = slice(j * CS, (j + 1) * CS)
            c.mm(s0.ap[0:64, 0:64], Zf.ap[js, 0:64], tm.ap[js, 64:128], s0, [Zf, tm])
            c.stt("dve", PhiT[:, j * 64:(j + 1) * 64], c.ident.ap[0:64, 0:64], gcb.ap[0:64, gci0 + j:gci0 + j + 1],
                  s0.ap[0:64, 0:64], ALU.mult, ALU.add, res, [c.ident, gcb, s0])
            c.mm(s1.ap[0:64, 0:64], tm.ap[js, 64:128], Zf.ap[js, 64:128], s1, [Zf, tm], start=True, stop=False)
            c.mm(s1.ap[0:64, 0:64], tm.ap[js, 128:192], VT[s_][g][d].ap[js, hl * 64:(hl + 1) * 64], s1, [tm, vtb], start=False, stop=True)
            c.copy("act", Psi[:, j * 64:(j + 1) * 64], s1.ap[0:64, 0:64], res, [s1])
            yield
        if islat:
            c.mm(s0.ap[:, 0:64], MAT.ap, U2, s0, [MAT, Zf], start=True, stop=False)
            c.mm(s0.ap[:, 0:64], MKT.ap, Vtm, s0, [MKT, vtb], start=False, stop=True)
            c.copy("act", Y2, s0.ap[:, 0:64], res, [s0])
            c.mm(s1.ap[0:64, :], W1, MAT.ap, s1, [Zf, MAT])
            c.tt("dve", PT, s1.ap[0:64, :], Rt, ALU.add, res, [s1, op])
        yield

    nsteps = nblk
    step = 0
    gi = 0
    yo_i = 0
    while step < nsteps:
        s_ = gi % nset
        gi += 1
        ng = min(NG, nsteps - step)
        gens = []
        info = []
        for g in range(ng):
            for d in range(2):
                bi = step + g
                blk = bi if d == 0 else (nblk - 1 - bi)
                pos = blk * 128 + (0 if d == 0 else cfg.ctx)
                islat = cfg.ctx <= pos < cfg.ctx + cfg.seq
                c.dma("sp", VF[s_][g][d].ap, ZS[2048 + hp * 128:2048 + (hp + 1) * 128, pos:pos + 128], VF[s_][g][d], c.ZSk)
                pb = c.bank(4 + d)
                c.tr(pb.ap[:, 0:128], VF[s_][g][d].ap, c.ident.ap, pb, [VF[s_][g][d], c.ident])
                c.copy("act", VT[s_][g][d].ap, pb.ap[:, 0:128], VT[s_][g][d], [pb])
                for hl in range(2):
                    op = OP[s_][g][d][hl]
                    c.dma("sp", op.ap[0:64, :].rearrange("p (i t) -> p i t", i=6),
                          RW[d, hp, hl * 64:(hl + 1) * 64, :, pos:pos + 128], op, c.RWk)
                    u_idx = (g * 2 + d) * 2 + hl
                    gci = d * (NP_ // CS) + pos // CS
                    gcb = gC[hl]
                    gens.append(unit(s_, g, d, hl, pos, islat, u_idx, gci, gcb))
                    info.append((g, d, hl, pos, islat))
        results = [None] * len(gens)
        live = list(range(len(gens)))
        while live:
            nxt = []
            for ui in live:
                try:
                    r = next(gens[ui])
                    if r is not None:
                        results[ui] = r
                    nxt.append(ui)
                except StopIteration:
                    pass
            live = nxt
        for g in range(ng):
            for d in range(2):
                bi = step + g
                blk = bi if d == 0 else (nblk - 1 - bi)
                pos = blk * 128 + (0 if d == 0 else cfg.ctx)
                islat = cfg.ctx <= pos < cfg.ctx + cfg.seq
                gcol_i = d * (NP_ // 128) + pos // 128
                yo = YO[yo_i % 4][d]
                for hl in range(2):
                    res = RES[s_][g][d][hl]
                    Hh = H[d][hl]
                    o2 = 2 * NSUB * 64
                    PT, Y2 = res.ap[0:64, o2:o2 + 128], res.ap[:, o2 + 128:o2 + 192]
                    pb = c.bank(6 + hl)
                    order = range(NSUB) if d == 0 else range(NSUB - 1, -1, -1)
                    for j in order:
                        js = slice(j * CS, (j + 1) * CS)
                        PhiT, Psi = res.ap[0:64, j * 64:(j + 1) * 64], res.ap[0:64, NSUB * 64 + j * 64:NSUB * 64 + (j + 1) * 64]
                        if islat:
                            pbs = B(pb.ap[js, 0:64], pb.key)
                            c.mm(pbs.ap, PT[:, js], Hh.ap[0:64, :], pbs, [res, Hh])
                            c.tt("dve", yo.ap[js, hl * 64:(hl + 1) * 64], pbs.ap, Y2[js, :], ALU.add, yo, [pbs, res])
                        pbs2 = B(pb.ap[0:64, 128:192], pb.key)
                        c.mm(pbs2.ap, PhiT, Hh.ap[0:64, :], pbs2, [res, Hh])
                        c.tt("dve", Hh.ap[0:64, :], pbs2.ap, Psi, ALU.add, Hh, [pbs2, res])
                if islat:
                    lpos = pos - cfg.ctx
                    c.dma("pool", YD[d, lpos:lpos + 128, hp * 128:(hp + 1) * 128], yo.ap, c.YDk, yo)
            yo_i += 1
        step += ng


def rw_readout(c, hp, M, bonus, lnbc):
    cfg = c.cfg
    ZS, LO, YD, YM = c.ZS.ap(), c.LO.ap(), c.YD.ap(), c.YM.ap()
    nlat = cfg.seq // 128
    yo = c.alloc("rd_out", cfg.seq, BF16)
    bufs = [[c.alloc(f"rd{n_}{i}", 128) for i in range(2)] for n_ in ("yf", "yb", "vf", "g", "t", "sq")]
    st = [c.alloc(f"rdst{i}", 8) for i in range(2)]
    for lb in range(nlat):
        yf, yb, vf, gf, tt_, sq = (bufs[i][lb % 2] for i in range(6))
        s_ = st[lb % 2]
        pos = cfg.ctx + lb * 128
        c.dma("sp", yf.ap, YD[0, lb * 128:(lb + 1) * 128, hp * 128:(hp + 1) * 128], yf, c.YDk)
        c.dma("sp", yb.ap, YD[1, lb * 128:(lb + 1) * 128, hp * 128:(hp + 1) * 128], yb, c.YDk)
        c.dma("sp", vf.ap, ZS[2048 + hp * 128:2048 + (hp + 1) * 128, pos:pos + 128], vf, c.ZSk)
        c.dma("sp", gf.ap, LO[4, hp, :, lb * 128:(lb + 1) * 128], gf, c.LOk)
        c.tt("dve", yf.ap, yf.ap, yb.ap, ALU.add, yf, [yf, yb])
        yv = yf.ap.rearrange("p (h v) -> p h v", h=2)
        c.op("dve", lambda e, o=s_.ap[:, 0:2], i=yv: e.tensor_reduce(out=o, in_=i, axis=AX.X, op=ALU.add), [yf], [s_])
        c.ts("dve", s_.ap[:, 0:2], s_.ap[:, 0:2], 1.0 / 64, None, ALU.mult, None, s_, [s_])
        c.tt("dve", yv, yv, s_.ap[:, 0:2].unsqueeze(2).to_broadcast([128, 2, 64]), ALU.subtract, yf, [yf, s_])
        c.tt("dve", sq.ap, yf.ap, yf.ap, ALU.mult, sq, [yf])
        c.op("dve", lambda e, o=s_.ap[:, 2:4], i=sq.ap.rearrange("p (h v) -> p h v", h=2): e.tensor_reduce(
            out=o, in_=i, axis=AX.X, op=ALU.add), [sq], [s_])
        c.ts("dve", s_.ap[:, 2:4], s_.ap[:, 2:4], 1.0 / 64, 64e-5, ALU.mult, ALU.add, s_, [s_])
        c.act(s_.ap[:, 2:4], s_.ap[:, 2:4], AF.Sqrt, s_, [s_])
        c.op("dve", lambda e, o=s_.ap[:, 2:4]: e.reciprocal(out=o, in_=o), [s_], [s_])
        c.tt("dve", yv, yv, s_.ap[:, 2:4].unsqueeze(2).to_broadcast([128, 2, 64]), ALU.mult, yf, [yf, s_])
        c.tt("dve", yf.ap, yf.ap, lnbc.ap[:, hp * 128:(hp + 1) * 128], ALU.mult, yf, [yf, lnbc])
        c.tt("dve", yf.ap, yf.ap, lnbc.ap[:, 1024 + hp * 128:1024 + (hp + 1) * 128], ALU.add, yf, [yf, lnbc])
        pb = c.bank(4 + lb % 2)
        c.tr(pb.ap[:, 0:128], vf.ap, c.ident.ap, pb, [vf, c.ident])
        tv = tt_.ap.rearrange("p (h v) -> p h v", h=2)
        c.tt("dve", tv, pb.ap[:, 0:128].rearrange("p (h v) -> p h v", h=2),
             bonus.ap[:, lb * 2:lb * 2 + 2].unsqueeze(2).to_broadcast([128, 2, 64]), ALU.mult, tt_, [pb, bonus])
        c.tt("dve", yf.ap, yf.ap, tt_.ap, ALU.add, yf, [yf, tt_])
        pb2 = c.bank(6 + lb % 2)
        c.tr(pb2.ap[:, 0:128], yf.ap, c.ident.ap, pb2, [yf, c.ident])
        c.tt("dve", yo.ap[:, lb * 128:(lb + 1) * 128], pb2.ap[:, 0:128], gf.ap, ALU.mult, yo, [pb2, gf])
    c.dma("pool", YM[1024 + hp * 128:1024 + (hp + 1) * 128, :], yo.ap, c.YMk, yo)


def bcast_rows(c, colT, name, bank0=0, res=None, dg=None):
    if res is None:
        res = c.alloc(name, D)
    if dg is None:
        dg = c.alloc(name + "_dg", D)
    for j in range(16):
        c.ts("dve", dg.ap[:, j * 128:(j + 1) * 128], c.ident.ap, colT[:, j:j + 1], None, ALU.mult, None, dg, [c.ident, c.mT, c.fgT])
    for q in range(4):
        ps = c.bank(bank0 + q)
        c.mm(ps.ap, c.ones128.ap, dg.ap[:, q * 512:(q + 1) * 512], ps, [c.ones128, dg])
        c.copy("act", res.ap[:, q * 512:(q + 1) * 512], ps.ap, res, [ps])
    return res


def stage_outproj(c):
    cfg, T = c.cfg, c.T
    mTv = c.mT.ap.rearrange("p (o r) -> p o r", r=2)
    YM, X1, FT, GT = c.YM.ap(), c.X1.ap(), c.FT.ap(), c.GT.ap()
    g2 = load_colvec(c, T["norm2_g"].ap(), c.Dk["norm2_g"], 16, "g2T")
    scl2 = c.alloc("scl2", 16)
    c.stt("dve", scl2.ap, mTv[:, 64:80, 0], 1.0, g2.ap, ALU.add, ALU.mult, scl2, [c.mT, g2])
    gt1bc = bcast_rows(c, mTv[:, 32:48, 0], "gt1bc")
    wout = c.alloc("wout", 16 * D, BF16)
    wov = wout.ap.rearrange("p (jc n) -> p jc n", jc=16)
    wsrc = T["w_out"].ap().rearrange("(jc p) n -> p jc n", p=128)
    for jc in range(16):
        c.dma("pool", wov[:, jc, :], wsrc[:, jc, :], wout, c.Dk["w_out"])
    rw = c.alloc("routerw", 16 * 32)
    rwv = rw.ap.rearrange("p (kc e) -> p kc e", kc=16)
    c.dma("sp", rwv, T["router_w"].ap().rearrange("(kc p) e -> p kc e", p=128), rw, c.Dk["router_w"])
    rb = c.alloc("routerb", 32)
    c.dma("sp", rb.ap[0:1, :], T["router_b"].ap(), rb, c.Dk["router_b"])
    TBK = min(512, cfg.seq)
    ymb = [c.alloc(f"ymb{i}", 16 * TBK, BF16) for i in range(2)]
    ftb = [c.alloc(f"ftb{i}", 16 * TBK, BF16) for i in range(2)]
    gtb = [c.alloc(f"gtb{i}", TBK) for i in range(2)]
    xt = [c.alloc(f"ox{i}", D) for i in range(2)]
    xs = c.alloc("oxs", D)
    tmp = c.alloc("otmp", 512)
    junk = c.alloc("ojunk", D, BF16)
    f32 = c.alloc("of32", 16 * 128)
    f32v = f32.ap.rearrange("p (j t) -> p j t", j=16)
    st = c.alloc("ost", 8)
    lg = c.alloc("olg", 32); mx = c.alloc("omx", 8); ex = c.alloc("oex", 32); mk = c.alloc("omk", 32)
    ymsrc = YM.rearrange("(jc p) t -> p jc t", p=128)
    ftdst = FT.rearrange("(jc p) t -> p jc t", p=128)
    for blk in range(cfg.seq // TBK):
        yb, fb, gb = ymb[blk % 2], ftb[blk % 2], gtb[blk % 2]
        ybv = yb.ap.rearrange("p (jc t) -> p jc t", jc=16)
        fbv = fb.ap.rearrange("p (jc t) -> p jc t", jc=16)
        c.dma("sp", ybv, ymsrc[:, :, blk * TBK:(blk + 1) * TBK], yb, c.YMk)
        for ti in range(TBK // 128):
            tok0 = blk * TBK + ti * 128
            x = xt[ti % 2]
            c.dma("sp", x.ap, T["x"].ap()[tok0:tok0 + 128, :], x, c.Dk["x"])
            for cb in range(4):
                ps = c.bank(cb)
                for jc in range(16):
                    c.mm(ps.ap, ybv[:, jc, ti * 128:(ti + 1) * 128], wov[:, jc, cb * 512:(cb + 1) * 512], ps, [yb, wout],
                         start=(jc == 0), stop=(jc == 15))
                c.tt("dve", tmp.ap, ps.ap, gt1bc.ap[:, cb * 512:(cb + 1) * 512], ALU.mult, tmp, [ps, gt1bc])
                c.tt("dve", x.ap[:, cb * 512:(cb + 1) * 512], x.ap[:, cb * 512:(cb + 1) * 512], tmp.ap, ALU.add, x, [x, tmp])
            c.dma("act", X1[tok0:tok0 + 128, :], x.ap, c.X1k, x)
            c.act(junk.ap, x.ap, AF.Square, junk, [x], accum=st.ap[:, 0:1], accb=st)
            c.ts("dve", st.ap[:, 0:1], st.ap[:, 0:1], 1.0 / D, 1e-5, ALU.mult, ALU.add, st, [st, junk])
            c.act(st.ap[:, 0:1], st.ap[:, 0:1], AF.Sqrt, st, [st])
            c.op("dve", lambda e: e.reciprocal(out=st.ap[:, 0:1], in_=st.ap[:, 0:1]), [st], [st])
            c.ts("dve", xs.ap, x.ap, st.ap[:, 0:1], None, ALU.mult, None, xs, [x, st])
            for q in range(4):
                ps = c.bank(4 + q % 2)
                for jj in range(4):
                    j = q * 4 + jj
                    c.tr(ps.ap[:, jj * 128:(jj + 1) * 128], xs.ap[:, j * 128:(j + 1) * 128], c.ident.ap, ps, [xs, c.ident])
                for jj in range(4):
                    j = q * 4 + jj
                    c.act(f32v[:, j, :], ps.ap[:, jj * 128:(jj + 1) * 128], AF.Identity, f32, [ps, scl2, c.mT],
                          bias=mTv[:, 48 + j, 0:1], scale=scl2.ap[:, j:j + 1])
            c.copy("pool", fbv[:, :, ti * 128:(ti + 1) * 128], f32v, fb, [f32])
            ps = c.bank(6)
            for j in range(16):
                c.mm(ps.ap[:, 0:32], f32v[:, j, :], rwv[:, j, :], ps, [f32, rw], start=(j == 0), stop=False)
            c.mm(ps.ap[:, 0:32], c.onesrow.ap[0:1, :], rb.ap[0:1, :], ps, [c.onesrow, rb], start=False, stop=True)
            c.copy("act", lg.ap, ps.ap[:, 0:32], lg, [ps])
            c.op("dve", lambda e: e.max(out=mx.ap, in_=lg.ap), [lg], [mx])
            c.ts("dve", mk.ap, lg.ap, mx.ap[:, 3:4], None, ALU.is_ge, None, mk, [lg, mx])
            c.ts("dve", mx.ap[:, 4:5], mx.ap[:, 0:1], -1.0, None, ALU.mult, None, mx, [mx])
            c.act(ex.ap, lg.ap, AF.Exp, ex, [lg, mx], bias=mx.ap[:, 4:5])
            c.tt("dve", ex.ap, ex.ap, mk.ap, ALU.mult, ex, [ex, mk])
            c.op("dve", lambda e: e.tensor_reduce(out=mx.ap[:, 5:6], in_=ex.ap, axis=AX.X, op=ALU.add), [ex], [mx])
            c.op("dve", lambda e: e.reciprocal(out=mx.ap[:, 5:6], in_=mx.ap[:, 5:6]), [mx], [mx])
            c.ts("dve", ex.ap, ex.ap, mx.ap[:, 5:6], None, ALU.mult, None, ex, [ex, mx])
            ps2 = c.bank(7)
            c.tr(ps2.ap[0:32, 0:128], ex.ap, c.ident.ap, ps2, [ex, c.ident])
            c.copy("act", gb.ap[0:32, ti * 128:(ti + 1) * 128], ps2.ap[0:32, 0:128], gb, [ps2])
        c.dma("act", ftdst[:, :, blk * TBK:(blk + 1) * TBK], fbv, c.FTk, fb)
        c.dma("act", GT[:, blk * TBK:(blk + 1) * TBK], gb.ap[0:32, :], c.GTk, gb)
    c.stage_end()


def stage_moe(c):
    cfg, T = c.cfg, c.T
    mTv = c.mT.ap.rearrange("p (o r) -> p o r", r=2)
    X1, FT, GT = c.X1.ap(), c.FT.ap(), c.GT.ap()
    NJ = cfg.dexp // 128
    TB = min(1024, cfg.seq)
    NH = (TB + 511) // 512
    HW = TB // NH
    nrow_b = NEXP * 2 * NJ
    bgu = c.alloc("bguT", nrow_b)
    bdn = c.alloc("bdn", D)
    gt2bc = c.alloc("gt2bc", D)
    fgbc = c.alloc("fgbc", D)
    base = c.top
    bsrc = T["exp_bgu"].ap()
    for r0 in range(0, nrow_b, 128):
        n = min(128, nrow_b - r0)
        c.top = base
        tmpc = load_colvec(c, bsrc[r0:r0 + n, :], c.Dk["exp_bgu"], n, f"bgu{r0}")
        c.copy("dve", bgu.ap[:, r0:r0 + n], tmpc.ap, bgu, [tmpc])
        c.p.barrier()
    c.top = base
    c.dma("sp", bdn.ap[0:32, :], T["exp_bdn"].ap(), bdn, c.Dk["exp_bdn"])
    dg = c.alloc("dgscr", D)
    bcast_rows(c, mTv[:, 80:96, 0], "gt2bc", res=gt2bc, dg=dg)
    bcast_rows(c, c.fgT.ap, "fgbc", bank0=4, res=fgbc, dg=dg)
    c.p.barrier()
    c.top = base
    for blk in range(cfg.seq // TB):
        c.p.barrier()
        c.top = base
        t0 = blk * TB
        ft = c.alloc("m_ft", 16 * TB, BF16)
        ftv = ft.ap.rearrange("p (kc t) -> p kc t", kc=16)
        yacc = c.alloc("m_yacc", 16 * TB)
        yav = yacc.ap.rearrange("p (cc t) -> p cc t", cc=16)
        base2 = c.top
        gts = c.alloc("m_gt", TB)
        actT = c.alloc("m_act", NJ * TB, BF16)
        acv = actT.ap.rearrange("p (j t) -> p j t", j=NJ)
        gbc = c.alloc("m_gbc", TB)
        gl = c.alloc("m_gl", HW); ln = c.alloc("m_ln", HW); sg = c.alloc("m_sg", HW)
        wg = [[c.alloc(f"m_wg{k}{i}", 16 * 128, BF16) for i in range(2)] for k in range(2)]
        wd = [c.alloc(f"m_wd{i}", D, BF16) for i in range(2)]
        c.dma("sp", ftv, FT.rearrange("(kc p) t -> p kc t", p=128)[:, :, t0:t0 + TB], ft, c.FTk)
        c.dma("sp", gts.ap[0:32, :], GT[:, t0:t0 + TB], gts, c.GTk)
        for cc in range(16):
            for h in range(NH):
                ps = c.bank((cc * NH + h) % 4)
                c.mm(ps.ap[:, 0:HW], bdn.ap[0:32, cc * 128:(cc + 1) * 128], gts.ap[0:32, h * HW:(h + 1) * HW], ps, [bdn, gts])
                c.copy("act", yav[:, cc, h * HW:(h + 1) * HW], ps.ap[:, 0:HW], yacc, [ps])
        wi = 0
        di = 0
        for e in range(NEXP):
            for h in range(NH):
                ps = c.bank(4 + h)
                c.mm(ps.ap[:, 0:HW], c.ident.ap[0:32, e:e + 1].to_broadcast([32, 128]), gts.ap[0:32, h * HW:(h + 1) * HW], ps,
                     [c.ident, gts])
                c.copy("act", gbc.ap[:, h * HW:(h + 1) * HW], ps.ap[:, 0:HW], gbc, [ps])
            wsrc = T["exp_wgu"].ap()[e].rearrange("(kc p) n -> p kc n", p=128)
            for j in range(NJ):
                wt = []
                for kind in range(2):
                    w = wg[kind][wi % 2]
                    wv = w.ap.rearrange("p (kc n) -> p kc n", kc=16)
                    c.dma("pool", wv, wsrc[:, :, kind * cfg.dexp + j * 128:kind * cfg.dexp + (j + 1) * 128], w, c.Dk["exp_wgu"])
                    wt.append((w, wv))
                wi += 1
                bcol = e * 2 * NJ + j
                for h in range(NH):
                    hs = slice(h * HW, (h + 1) * HW)
                    pg, pl = c.bank(2 * h), c.bank(2 * h + 1)
                    for kind, ps in ((0, pg), (1, pl)):
                        w, wv = wt[kind]
                        for kc in range(16):
                            c.mm(ps.ap[:, 0:HW], wv[:, kc, :], ftv[:, kc, hs], ps, [w, ft], start=(kc == 0), stop=(kc == 15))
                    c.ts("dve", gl.ap, pg.ap[:, 0:HW], bgu.ap[:, bcol:bcol + 1], 7.0, ALU.add, ALU.min, gl, [pg, bgu])
                    c.act(sg.ap, gl.ap, AF.Sigmoid, sg, [gl], scale=1.702)
                    c.ts("dve", ln.ap, pl.ap[:, 0:HW], bgu.ap[:, bcol + NJ:bcol + NJ + 1], 7.0, ALU.add, ALU.min, ln, [pl, bgu])
                    c.ts("dve", ln.ap, ln.ap, -7.0, 1.0, ALU.max, ALU.add, ln, [ln])
                    c.tt("dve", gl.ap, gl.ap, sg.ap, ALU.mult, gl, [gl, sg])
                    c.tt("dve", gl.ap, gl.ap, ln.ap, ALU.mult, gl, [gl, ln])
                    c.tt("dve", acv[:, j, hs], gl.ap, gbc.ap[:, hs], ALU.mult, actT, [gl, gbc])
            dsrc = T["exp_wdn"].ap()[e].rearrange("(jc p) n -> p jc n", p=128)
            for cc in range(16):
                w = wd[di % 2]
                di += 1
                wv = w.ap.rearrange("p (jc n) -> p jc n", jc=16)
                c.dma("pool", wv[:, 0:NJ, :], dsrc[:, :, cc * 128:(cc + 1) * 128], w, c.Dk["exp_wdn"])
                for h in range(NH):
                    ps = c.bank(4 + (cc * NH + h) % 4)
                    for j in range(NJ):
                        c.mm(ps.ap[:, 0:HW], wv[:, j, :], acv[:, j, h * HW:(h + 1) * HW], ps, [w, actT],
                             start=(j == 0), stop=(j == NJ - 1))
                    c.tt("dve", yav[:, cc, h * HW:(h + 1) * HW], yav[:, cc, h * HW:(h + 1) * HW], ps.ap[:, 0:HW], ALU.add, yacc, [yacc, ps])
        c.p.barrier()
        c.top = base2
        x1t = [c.alloc(f"fx{i}", D) for i in range(2)]
        tmp = c.alloc("ftmp", 512)
        junk = c.alloc("fjunk", D, BF16)
        st = c.alloc("fst", 8)
        for ti in range(TB // 128):
            tok0 = t0 + ti * 128
            x = x1t[ti % 2]
            c.dma("sp", x.ap, X1[tok0:tok0 + 128, :], x, c.X1k)
            for q in range(4):
                ps = c.bank(q)
                for jj in range(4):
                    cc = q * 4 + jj
                    c.tr(ps.ap[:, jj * 128:(jj + 1) * 128], yav[:, cc, ti * 128:(ti + 1) * 128], c.ident.ap, ps, [yacc, c.ident])
                c.tt("dve", tmp.ap, ps.ap, gt2bc.ap[:, q * 512:(q + 1) * 512], ALU.mult, tmp, [ps, gt2bc])
                c.tt("dve", x.ap[:, q * 512:(q + 1) * 512], x.ap[:, q * 512:(q + 1) * 512], tmp.ap, ALU.add, x, [x, tmp])
            c.act(junk.ap, x.ap, AF.Square, junk, [x], accum=st.ap[:, 0:1], accb=st)
            c.ts("dve", st.ap[:, 0:1], st.ap[:, 0:1], 1.0 / D, 1e-5, ALU.mult, ALU.add, st, [st, junk])
            c.act(st.ap[:, 0:1], st.ap[:, 0:1], AF.Sqrt, st, [st])
            c.op("dve", lambda e: e.reciprocal(out=st.ap[:, 0:1], in_=st.ap[:, 0:1]), [st], [st])
            c.stt("dve", x.ap, x.ap, st.ap[:, 0:1], fgbc.ap, ALU.mult, ALU.mult, x, [x, st, fgbc])
            c.dma("act", c.out.ap()[tok0:tok0 + 128, :], x.ap, c.Dk["out"], x)
    c.stage_end()


def make_inputs(cfg, b, inp):
    inp = {k: np.asarray(v) for k, v in inp.items()}
    f = lambda a: np.ascontiguousarray(a, dtype=np.float32)
    cc = np.stack([inp["c"][b], inp["c_ctx"]]).reshape(32, 128)
    return {
        "x": f(inp["x"][b]), "ctx": f(inp["ctx"][b]), "cc": f(cc),
        "mod_w": f(inp["mod_w"][0]), "mod_b": f(inp["mod_b"][0].reshape(96, 128)),
        "norm1_g": f(inp["norm1_g"][0].reshape(16, 128)), "w_in": f(inp["w_in"][0]),
        "rw_mu": f(np.pad(inp["rw_mu"][0], ((0, 0), (0, 28 * 128 - RWC))).reshape(4 * 28, 128)),
        "s5_are": f(inp["s5_a_re"][0].reshape(64, 128)), "s5_aim": f(inp["s5_a_im"][0].reshape(64, 128)),
        "s5_ldt": f(np.repeat(inp["s5_log_dt"][0].reshape(2, 32, 2, 1), 64, axis=3).reshape(64, 128)),
        "s5_d": f(inp["s5_d"][0].reshape(8, 128)), "s5_glub": f(inp["s5_glu_b"][0].reshape(8, 128)),
        "s5_bre": f(inp["s5_b_re"][0].reshape(-1, 16)), "s5_bim": f(inp["s5_b_im"][0].reshape(-1, 16)),
        "s5_cre": f(inp["s5_c_re"][0].reshape(-1, 64)), "s5_cim": f(inp["s5_c_im"][0].reshape(-1, 64)),
        "s5_gluw": f(inp["s5_glu_w"][0].reshape(1024, 16)),
        "rw_w0": f(inp["rw_w0"][0].reshape(16, 128)), "rw_a0": f(inp["rw_a0"][0].reshape(16, 128)),
        "rw_w2": f(inp["rw_w2"][0].reshape(128, 1024)), "rw_a2": f(inp["rw_a2"][0].reshape(128, 1024)),
        "rw_g2": f(inp["rw_g2"][0]), "rw_kk": f(inp["rw_k_k"][0].reshape(8, 128)), "rw_ka": f(inp["rw_k_a"][0].reshape(8, 128)),
        "rw_rk": f(inp["rw_r_k"][0].reshape(8, 128)), "rw_lnw": f(inp["rw_ln_w"][0].reshape(1, 1024)),
        "rw_lnb": f(inp["rw_ln_b"][0].reshape(1, 1024)),
        "w_out": f(inp["w_out"][0]), "norm2_g": f(inp["norm2_g"][0].reshape(16, 128)),
        "router_w": f(inp["router_w"][0]), "router_b": f(inp["router_b"][0].reshape(1, 32)),
        "final_g": f(inp["final_g"].reshape(16, 128)),
        "exp_wgu": f(inp["exp_w_gu"][0]), "exp_bgu": f(inp["exp_b_gu"][0].reshape(-1, 128)),
        "exp_wdn": f(inp["exp_w_dn"][0]), "exp_bdn": f(inp["exp_b_dn"][0]),
    }


_NC_CACHE = {}


def kernel(**inp):
    cfg = Cfg()
    if "nc" not in _NC_CACHE:
        _NC_CACHE["nc"] = build(cfg)
    nc = _NC_CACHE["nc"]
    in_maps = [make_inputs(cfg, b, inp) for b in range(2)]
    res = run_bass_kernel_spmd(nc, in_maps, core_ids=[0, 1])
    return np.stack([res.results[b]["out"] for b in range(2)]).astype(np.float32)
```

```python
import math
from contextlib import ExitStack

import numpy as np
import concourse.bass as bass
import concourse.mybir as mybir
from concourse.bass_utils import run_bass_kernel_spmd

F32 = mybir.dt.float32
BF16 = mybir.dt.bfloat16
ALU = mybir.AluOpType
AF = mybir.ActivationFunctionType
AX = mybir.AxisListType

D = 2048
KC = D // 128
S5W = 1024
RWW = 1024
RWC = 3 * RWW + 128 + 128 + 160
INC = S5W + RWC
NEXP = 32
CS = 64
NSUB = 128 // CS
ARENA = 52800


class Cfg:
    def __init__(self, nrow=64, ctx=256, dexp=2048, stages=99, nq=4):
        self.nrow = nrow
        self.seq = nrow * 64
        self.ctx = ctx
        self.ntok = self.seq + ctx
        self.npos = self.seq + 2 * ctx
        self.dexp = dexp
        self.stages = stages
        self.nq = nq
        self.nown = self.seq // nq


ENGS = ("sp", "act", "dve", "pool", "pe")
NDSEM = 6
SELF_SYNC = {"pool", "dve", "act"}


class Op:
    __slots__ = ("eng", "fn", "deps", "sig", "sem", "val", "dma", "prev_dma")

    def __init__(self, eng, fn, dma):
        self.eng, self.fn, self.dma = eng, fn, dma
        self.deps = []
        self.sig = dma
        self.sem = None
        self.val = 0
        self.prev_dma = None


class Prog:
    def __init__(self, nc, es):
        self.nc = nc
        self.ops = {e: [] for e in ENGS}
        self.tab = {}
        self.csem = {e: es.enter_context(nc.semaphore("c_" + e)) for e in ENGS}
        self.dsem = {e: [es.enter_context(nc.semaphore(f"d_{e}{i}")) for i in range(NDSEM)]
                     for e in ("sp", "act", "pool")}
        self.dcnt = {e: [0] * NDSEM for e in self.dsem}
        self.dlast = {e: [None] * NDSEM for e in self.dsem}
        self.drr = {e: 0 for e in self.dsem}

    def _entries(self, key):
        name, sub = key
        t = self.tab.setdefault(name, {})
        if sub is None:
            return list(t.values())
        out = []
        if sub in t:
            out.append(t[sub])
        if None in t:
            out.append(t[None])
        return out

    def add(self, eng, fn, reads=(), writes=(), dma=False):
        op = Op(eng, fn, dma)
        deps = []
        for k in reads:
            for ent in self._entries(k):
                if ent[0] is not None:
                    deps.append(ent[0])
        for k in writes:
            for ent in self._entries(k):
                if ent[0] is not None:
                    deps.append(ent[0])
                deps.extend(ent[1].values())
        if dma:
            i = self.drr[eng]
            self.drr[eng] = (i + 1) % NDSEM
            op.sem = self.dsem[eng][i]
            self.dcnt[eng][i] += 1
            op.val = 16 * self.dcnt[eng][i]
            op.prev_dma = self.dlast[eng][i]
            self.dlast[eng][i] = op
            if op.prev_dma is not None:
                deps.append(op.prev_dma)
        seen = set()
        for d in deps:
            if d is op or id(d) in seen:
                continue
            seen.add(id(d))
            if (not d.dma) and d.eng == eng and eng not in SELF_SYNC:
                continue
            d.sig = True
            op.deps.append(d)
        for k in writes:
            name, sub = k
            t = self.tab.setdefault(name, {})
            if sub is None:
                t.clear()
            t[sub] = [op, {}]
        for k in reads:
            name, sub = k
            t = self.tab.setdefault(name, {})
            if sub not in t:
                t[sub] = [None, {}]
            t[sub][1][eng if not dma else (eng, id(op))] = op
        self.ops[eng].append(op)
        return op

    def barrier(self):
        lasts = []
        for e in ENGS:
            for o in reversed(self.ops[e]):
                if o.fn is not None and not o.dma:
                    lasts.append(o)
                    break
        for e in self.dsem:
            for d in self.dlast[e]:
                if d is not None:
                    lasts.append(d)
        for e in ENGS:
            op = Op(e, None, False)
            for d in lasts:
                if (not d.dma) and d.eng == e:
                    continue
                d.sig = True
                op.deps.append(d)
            self.ops[e].append(op)
        self.tab = {}

    def emit(self):
        nc = self.nc
        for e in ENGS:
            n = 0
            for op in self.ops[e]:
                if not op.dma and op.sig:
                    n += 1
                    op.sem = self.csem[e]
                    op.val = n
        with nc.Block() as block:
            def run(e, eng):
                seen = {}
                for op in self.ops[e]:
                    need = {}
                    for d in op.deps:
                        k = id(d.sem)
                        if d.val > seen.get(k, 0) and d.val > need.get(k, (None, 0))[1]:
                            need[k] = (d.sem, d.val)
                    for k, (s, v) in need.items():
                        eng.wait_ge(s, v)
                        seen[k] = v
                    if op.fn is not None:
                        ins = op.fn(eng)
                        if op.sig:
                            ins.then_inc(op.sem, 16 if op.dma else 1)

            @block.sync
            def _(eng):
                run("sp", eng)

            @block.scalar
            def _(eng):
                run("act", eng)

            @block.vector
            def _(eng):
                run("dve", eng)

            @block.gpsimd
            def _(eng):
                run("pool", eng)

            @block.tensor
            def _(eng):
                run("pe", eng)


class B:
    __slots__ = ("ap", "key")

    def __init__(self, ap, key):
        self.ap, self.key = ap, key


class Ctx:
    def __init__(self, nc, es, cfg):
        self.nc, self.cfg = nc, cfg
        self.p = Prog(nc, es)
        self.arena = es.enter_context(nc.sbuf_tensor("arena", [128, ARENA], F32))
        self.psum = es.enter_context(nc.psum_tensor("psum", [128, 4096], F32))
        self.top = 0
        self.uid = 0
        self.pers = 0

    def alloc(self, name, n, dt=F32):
        words = n if dt == F32 else (n + 1) // 2
        words = (words + 7) // 8 * 8
        assert self.top + words <= ARENA, (name, self.top, words)
        ap = self.arena[:, self.top:self.top + words]
        if dt != F32:
            ap = ap.bitcast(dt)[:, 0:n]
        else:
            ap = ap[:, 0:n]
        self.top += words
        self.uid += 1
        return B(ap, (f"{name}#{self.uid}", None))

    def bank(self, i, name=None):
        return B(self.psum[:, i * 512:(i + 1) * 512], (f"bank{i}", None))

    def stage_end(self):
        self.p.barrier()
        self.top = self.pers

    def op(self, eng, fn, reads, writes, dma=False):
        return self.p.add(eng, fn, [b.key for b in reads], [b.key for b in writes], dma)

    def dma(self, q, out, in_, outb, inb, **kw):
        return self.op(q, lambda e: e.dma_start(out=out, in_=in_, **kw), [inb], [outb], dma=True)

    def mm(self, out, lhsT, rhs, outb, inbs, start=True, stop=True):
        return self.op("pe", lambda e: e.matmul(out, lhsT, rhs, start=start, stop=stop), inbs,
                       [outb] if start else [outb])

    def tr(self, out, in_, ident, outb, inbs):
        return self.op("pe", lambda e: e.transpose(out, in_, ident), inbs, [outb])

    def act(self, out, in_, func, outb, inbs, bias=None, scale=None, accum=None, accb=None, eng="act"):
        kw = {}
        if bias is not None:
            kw["bias"] = bias
        if scale is not None:
            kw["scale"] = scale
        if accum is not None:
            kw["accum_out"] = accum
        return self.op(eng, lambda e: e.activation(out=out, in_=in_, func=func, **kw), inbs,
                       [outb] + ([accb] if accb is not None else []))

    def ts(self, eng, out, in0, s1, s2, op0, op1, outb, inbs):
        if op1 is None:
            return self.op(eng, lambda e: e.tensor_scalar(out=out, in0=in0, scalar1=s1, scalar2=None, op0=op0),
                           inbs, [outb])
        return self.op(eng, lambda e: e.tensor_scalar(out=out, in0=in0, scalar1=s1, scalar2=s2, op0=op0, op1=op1),
                       inbs, [outb])

    def tt(self, eng, out, in0, in1, op, outb, inbs):
        return self.op(eng, lambda e: e.tensor_tensor(out=out, in0=in0, in1=in1, op=op), inbs, [outb])

    def stt(self, eng, out, in0, scalar, in1, op0, op1, outb, inbs):
        return self.op(eng, lambda e: e.scalar_tensor_tensor(out=out, in0=in0, scalar=scalar, in1=in1,
                                                             op0=op0, op1=op1), inbs, [outb])

    def copy(self, eng, out, in_, outb, inbs):
        if eng == "act":
            return self.op(eng, lambda e: e.copy(out=out, in_=in_), inbs, [outb])
        return self.op(eng, lambda e: e.tensor_copy(out=out, in_=in_), inbs, [outb])

    def memset(self, eng, ap, val, outb):
        return self.op(eng, lambda e: e.memset(ap, val), [], [outb])


def dram_in(nc, name, shape):
    return nc.dram_tensor(name, list(shape), F32, kind="ExternalInput")


def build(cfg):
    nc = bass.Bass("TRN2", target_bir_lowering=False)
    es = ExitStack()
    T = {}
    shapes = input_shapes(cfg)
    for k, s in shapes.items():
        T[k] = dram_in(nc, k, s)
    out = nc.dram_tensor("out", [cfg.nown, D], F32, kind="ExternalOutput")
    with es:
        c = Ctx(nc, es, cfg)
        c.T = T
        c.out = out
        c.Dk = {k: B(None, ("dram_" + k, None)) for k in T}
        c.Dk["out"] = B(None, ("dram_out", None))
        c.P = nc.dram_tensor("P_s", [INC, cfg.npos], F32, kind="Internal")
        c.Pk = B(None, ("P_s", None))
        stage_consts(c)
        stage_adaln(c)
        nch = (RWC + 127) // 128
        c.ZS = nc.dram_tensor("ZS_s", [nch * 128, cfg.npos], F32, kind="Internal")
        c.ZSk = B(None, ("ZS_s", None))
        c.YM = nc.dram_tensor("YM_s", [D, cfg.seq], BF16, kind="Internal")
        c.YMk = B(None, ("YM_s", None))
        if cfg.stages >= 1:
            stage_inproj(c)
        if cfg.stages >= 2:
            stage_shift(c)
        if cfg.stages >= 3:
            stage_s5(c)
        c.LO = nc.dram_tensor("LO_s", [5, 8, 128, cfg.npos], F32, kind="Internal")
        c.LOk = B(None, ("LO_s", None))
        c.RW = nc.dram_tensor("RW_s", [2, 8, 128, 6, cfg.npos], F32, kind="Internal")
        c.RWk = B(None, ("RW_s", None))
        c.YD = nc.dram_tensor("YD_s", [2, cfg.seq, 1024], F32, kind="Internal")
        c.YDk = B(None, ("YD_s", None))
        if cfg.stages >= 4:
            stage_rw_lora(c)
        if cfg.stages >= 5:
            stage_rw(c)
        c.qv = nc.partition_id() % cfg.nq
        c.X1 = nc.dram_tensor("X1_s", [cfg.nown, D], F32, kind="Internal")
        c.X1k = B(None, ("X1_s", None))
        c.FT = nc.dram_tensor("FT_s", [D, cfg.nown], BF16, kind="Internal")
        c.FTk = B(None, ("FT_s", None))
        c.GT = nc.dram_tensor("GT_s", [32, cfg.nown], F32, kind="Internal")
        c.GTk = B(None, ("GT_s", None))
        if cfg.stages >= 6:
            stage_outproj(c)
        if cfg.stages >= 7:
            stage_moe(c)
        c.p.barrier()
        c.p.emit()
    return nc


def input_shapes(cfg):
    return {
        "x": (cfg.seq, D), "x_own": (cfg.nown, D), "ctx": (cfg.ctx, D), "cc": (32, 128),
        "mod_w": (D, 6 * D), "mod_b": (96, 128), "norm1_g": (16, 128), "w_in": (D, INC),
        "rw_mu": (4 * 28, 128),
        "s5_are": (64, 128), "s5_aim": (64, 128), "s5_ldt": (64, 128), "s5_d": (8, 128), "s5_glub": (8, 128),
        "s5_bre": (2 * 64 * 64, 16), "s5_bim": (2 * 64 * 64, 16), "s5_cre": (2 * 64 * 16, 64), "s5_cim": (2 * 64 * 16, 64),
        "s5_gluw": (1024, 16),
        "rw_w0": (16, 128), "rw_a0": (16, 128), "rw_w2": (128, 1024), "rw_a2": (128, 1024), "rw_g2": (160, 1024),
        "rw_kk": (8, 128), "rw_ka": (8, 128), "rw_rk": (8, 128), "rw_lnw": (1, 1024), "rw_lnb": (1, 1024),
        "w_out": (D, D), "norm2_g": (16, 128), "router_w": (D, 32), "router_b": (1, 32), "final_g": (16, 128),
        "exp_wgu": (NEXP, D, 2 * cfg.dexp), "exp_bgu": (NEXP * 2 * cfg.dexp // 128, 128),
        "exp_wdn": (NEXP, cfg.dexp, D), "exp_bdn": (NEXP, D),
    }


def stage_consts(c):
    nc = c.nc
    ident = c.alloc("ident", 128)
    c.ident = ident
    c.memset("pool", ident.ap, 1.0, ident)
    c.op("pool", lambda e: e.affine_select(out=ident.ap, in_=ident.ap, pattern=[[-1, 128]],
                                           compare_op=ALU.is_equal, fill=0.0, base=0, channel_multiplier=1),
         [ident], [ident])
    onesrow = c.alloc("onesrow", 128)
    c.memset("pool", onesrow.ap, 1.0, onesrow)
    c.onesrow = onesrow
    ones128 = c.alloc("ones128", 128)
    c.memset("pool", ones128.ap, 1.0, ones128)
    c.ones128 = ones128
    c.pers = c.top


def load_colvec(c, src_ap, srcb, nrows, name, bank=7):
    tmp = c.alloc(name + "_r", 128)
    res = c.alloc(name, nrows)
    c.dma("sp", tmp.ap[0:nrows, :], src_ap, tmp, srcb)
    pb = c.bank(bank)
    c.tr(pb.ap[:, 0:nrows], tmp.ap[0:nrows, :], c.ident.ap[0:nrows, 0:nrows], pb, [tmp, c.ident])
    c.copy("dve", res.ap, pb.ap[:, 0:nrows], res, [pb])
    return res


def stage_adaln(c):
    T = c.T
    mT = c.alloc("mT", 96 * 2)
    g1 = load_colvec(c, T["norm1_g"].ap(), c.Dk["norm1_g"], 16, "g1T")
    c.mT, c.g1 = mT, g1
    scl1 = c.alloc("scl1", 32)
    c.scl1 = scl1
    c.fgT = load_colvec(c, T["final_g"].ap(), c.Dk["final_g"], 16, "fgT")
    c.pers = c.top
    bT = load_colvec(c, T["mod_b"].ap(), c.Dk["mod_b"], 96, "modbT")
    craw = c.alloc("craw", 128)
    c.dma("sp", craw.ap[0:32, :], T["cc"].ap(), craw, c.Dk["cc"])
    c.act(craw.ap[0:32, :], craw.ap[0:32, :], AF.Silu, craw, [craw])
    pb = c.bank(6)
    c.tr(pb.ap[:, 0:32], craw.ap[0:32, :], c.ident.ap[0:32, 0:32], pb, [craw, c.ident])
    sT = c.alloc("siluT", 32)
    c.copy("dve", sT.ap, pb.ap[:, 0:32], sT, [pb])
    sTv = sT.ap.rearrange("p (r j) -> p j r", r=2)
    mTv = mT.ap.rearrange("p (o r) -> p o r", r=2)
    wbuf = [c.alloc(f"modw{i}", 16 * 512) for i in range(2)]
    mw = T["mod_w"].ap().rearrange("(kc p) n -> p kc n", p=128)
    for blk in range(24):
        wb = wbuf[blk % 2]
        wv = wb.ap.rearrange("p (kc n) -> p kc n", kc=16)
        c.dma("sp", wv, mw[:, :, blk * 512:(blk + 1) * 512], wb, c.Dk["mod_w"])
        ps = c.bank(blk % 2)
        for o in range(4):
            for kc in range(16):
                c.mm(ps.ap[:, o * 2:o * 2 + 2], wv[:, kc, o * 128:(o + 1) * 128], sTv[:, kc, :], ps, [wb, sT],
                     start=(kc == 0), stop=(kc == 15))
        o0 = blk * 4
        c.tt("dve", mTv[:, o0:o0 + 4, :], ps.ap[:, 0:8].rearrange("p (o r) -> p o r", r=2),
             bT.ap[:, o0:o0 + 4].unsqueeze(2).to_broadcast([128, 4, 2]), ALU.add, mT, [ps, bT])
    for r in range(2):
        c.stt("dve", scl1.ap[:, r * 16:(r + 1) * 16], mTv[:, 16:32, r], 1.0, g1.ap, ALU.add, ALU.mult,
              scl1, [mT, g1])
    c.stage_end()


def stage_inproj(c):
    cfg, T = c.cfg, c.T
    mTv = c.mT.ap.rearrange("p (o r) -> p o r", r=2)
    ntile = cfg.ntok // 128
    nct = cfg.ctx // 128
    xt = [c.alloc(f"xt{i}", D) for i in range(2)]
    junk = c.alloc("junk", D, BF16)
    ssq = c.alloc("ssq", 8)
    xnT = c.alloc("xnT", 16 * 512, BF16)
    xnv = xnT.ap.rearrange("p (kc n) -> p kc n", kc=16)
    wgrp = [c.alloc(f"wgrp{i}", 16 * 512, BF16) for i in range(2)]
    pst = [c.alloc(f"pst{i}", 512) for i in range(2)]
    win = T["w_in"].ap().rearrange("(kc p) n -> p kc n", p=128)
    P = c.P.ap()
    nblk = (ntile + 3) // 4
    gi = 0
    ci = 0
    for blk in range(nblk):
        t0 = blk * 4
        nt = min(4, ntile - t0)
        for ti in range(nt):
            g = t0 + ti
            isctx = g < nct
            r = 1 if isctx else 0
            src = T["ctx"].ap()[g * 128:(g + 1) * 128, :] if isctx else \
                T["x"].ap()[(g - nct) * 128:(g - nct + 1) * 128, :]
            xb = xt[gi % 2]
            gi += 1
            c.dma("sp", xb.ap, src, xb, c.Dk["ctx" if isctx else "x"])
            sq = ssq.ap[:, (g % 8):(g % 8) + 1]
            c.act(junk.ap, xb.ap, AF.Square, junk, [xb], accum=sq, accb=ssq)
            c.op("dve", lambda e, sq=sq: e.tensor_scalar(out=sq, in0=sq, scalar1=1.0 / D, scalar2=1e-5,
                                                         op0=ALU.mult, op1=ALU.add), [junk, ssq], [ssq])
            c.act(sq, sq, AF.Sqrt, ssq, [ssq])
            c.op("dve", lambda e, sq=sq: e.reciprocal(out=sq, in_=sq), [ssq], [ssq])
            c.op("dve", lambda e, sq=sq, xb=xb: e.tensor_scalar(out=xb.ap, in0=xb.ap, scalar1=sq, scalar2=None,
                                                                op0=ALU.mult), [ssq, xb], [xb])
            for q in range(4):
                ps = c.bank(q + 4 * (g % 2))
                for jj in range(4):
                    j = q * 4 + jj
                    c.tr(ps.ap[:, jj * 128:(jj + 1) * 128], xb.ap[:, j * 128:(j + 1) * 128], c.ident.ap,
                         ps, [xb, c.ident])
                for jj in range(4):
                    j = q * 4 + jj
                    c.act(xnv[:, j, ti * 128:(ti + 1) * 128], ps.ap[:, jj * 128:(jj + 1) * 128], AF.Identity,
                          xnT, [ps, c.scl1, c.mT], bias=mTv[:, j, r:r + 1], scale=c.scl1.ap[:, r * 16 + j:r * 16 + j + 1])
        n = nt * 128
        p0 = t0 * 128
        for cc in range((INC + 127) // 128):
            ncol = min(128, INC - cc * 128)
            po = pst[ci % 2]
            ci += 1
            if cc % 4 == 0:
                wg_ = wgrp[(cc // 4 + blk) % 2]
                wgv = wg_.ap.rearrange("p (kc n) -> p kc n", kc=16)
                gcols = min(512, INC - cc * 128)
                c.dma("pool", wgv[:, :, 0:gcols], win[:, :, cc * 128:cc * 128 + gcols], wg_, c.Dk["w_in"])
            cl = (cc % 4) * 128
            ps = c.bank(ci % 2)
            for kc in range(16):
                c.mm(ps.ap[0:ncol, 0:n], wgv[:, kc, cl:cl + ncol], xnv[:, kc, 0:n], ps, [wg_, xnT],
                     start=(kc == 0), stop=(kc == 15))
            c.copy("dve", po.ap[0:ncol, 0:n], ps.ap[0:ncol, 0:n], po, [ps])
            segs = []
            a, b_ = p0, p0 + n
            if a < cfg.ctx:
                e = min(b_, cfg.ctx)
                segs.append((a, e, a))
                segs.append((a, e, cfg.ctx + cfg.seq + a))
                a = e
            if a < b_:
                segs.append((a, b_, a))
            for (sa, sb, dpos) in segs:
                c.dma("pool", P[cc * 128:cc * 128 + ncol, dpos:dpos + (sb - sa)],
                      po.ap[0:ncol, sa - p0:sb - p0], c.Pk, po)
    c.stage_end()


def regions(cfg):
    return [(0, cfg.ctx, 1, cfg.ctx), (cfg.ctx, cfg.seq, cfg.nrow, 64), (cfg.ctx + cfg.seq, cfg.ctx, 1, cfg.ctx)]


def stage_shift(c):
    cfg, T = c.cfg, c.T
    nch = (RWC + 127) // 128
    muT = load_colvec(c, T["rw_mu"].ap(), c.Dk["rw_mu"], 4 * nch, "muT")
    P, ZS = c.P.ap(), c.ZS.ap()
    zb = [c.alloc(f"z{i}", cfg.npos) for i in range(2)]
    ob = [c.alloc(f"zo{i}", cfg.npos) for i in range(2)]
    tmp = c.alloc("ztmp", cfg.npos)
    for cc in range(nch):
        ncol = min(128, RWC - cc * 128)
        z, o = zb[cc % 2], ob[cc % 2]
        c.dma("sp", z.ap[0:ncol, :], P[S5W + cc * 128:S5W + cc * 128 + ncol, :], z, c.Pk)
        c.copy("act", o.ap[0:ncol, :], z.ap[0:ncol, :], o, [z])
        for (a, ln, rows, w) in regions(cfg):
            zv = z.ap[0:ncol, a:a + ln].rearrange("p (r w) -> p r w", w=w)
            ov = o.ap[0:ncol, a:a + ln].rearrange("p (r w) -> p r w", w=w)
            tv = tmp.ap[0:ncol, a:a + ln].rearrange("p (r w) -> p r w", w=w)
            pairs = [(0, (slice(None), slice(1, w)), (slice(None), slice(0, w - 1))),
                     (1, (slice(None), slice(0, w - 1)), (slice(None), slice(1, w)))]
            if rows > 1:
                pairs += [(2, (slice(1, rows), slice(None)), (slice(0, rows - 1), slice(None))),
                          (3, (slice(0, rows - 1), slice(None)), (slice(1, rows), slice(None)))]
            for (j, dst, src) in pairs:
                mu = muT.ap[0:ncol, j * nch + cc:j * nch + cc + 1]
                c.tt("dve", tv[:, dst[0], dst[1]], zv[:, src[0], src[1]], zv[:, dst[0], dst[1]], ALU.subtract,
                     tmp, [z])
                c.stt("dve", ov[:, dst[0], dst[1]], tv[:, dst[0], dst[1]], mu, ov[:, dst[0], dst[1]],
                      ALU.mult, ALU.add, o, [tmp, muT, o])
        c.dma("pool", ZS[cc * 128:cc * 128 + ncol, :], o.ap[0:ncol, :], c.ZSk, o)
    c.stage_end()


def stage_s5(c):
    cfg, T = c.cfg, c.T
    L = cfg.ntok
    P = c.P.ap()
    CH = next(x for x in (1088, 512, 384, 256, 128) if L % x == 0)
    if getattr(cfg, "s5_ch", None):
        CH = cfg.s5_ch
    NCH = L // CH
    NLV = max(1, int(math.ceil(math.log2(CH))))
    are = load_colvec(c, T["s5_are"].ap(), c.Dk["s5_are"], 64, "are")
    aim = load_colvec(c, T["s5_aim"].ap(), c.Dk["s5_aim"], 64, "aim")
    ldt = load_colvec(c, T["s5_ldt"].ap(), c.Dk["s5_ldt"], 64, "ldt")
    dT = load_colvec(c, T["s5_d"].ap(), c.Dk["s5_d"], 8, "s5dT")
    gbT = load_colvec(c, T["s5_glub"].ap(), c.Dk["s5_glub"], 8, "s5gbT")
    xr = c.alloc("xr", 64); xi = c.alloc("xi", 64); rho = c.alloc("rho", 64)
    t1 = c.alloc("t1", 64); t2 = c.alloc("t2", 64); tq = c.alloc("lb_t", 64)
    ur = c.alloc("u_re", (NLV + 1) * 64); ui = c.alloc("u_im", (NLV + 1) * 64); un = c.alloc("u_nim", (NLV + 1) * 64)
    wr = c.alloc("w_re", 64); wi = c.alloc("w_im", 64); wn = c.alloc("w_nim", 64)
    lbr = c.alloc("lb_re", 64); lbi = c.alloc("lb_im", 64)
    cr = c.alloc("coef_re", 64); ci = c.alloc("coef_im", 64); nci = c.alloc("coef_nim", 64)
    c.act(ldt.ap, ldt.ap, AF.Exp, ldt, [ldt])
    c.tt("dve", xr.ap, are.ap, ldt.ap, ALU.mult, xr, [are, ldt])
    c.tt("dve", xi.ap, aim.ap, ldt.ap, ALU.mult, xi, [aim, ldt])
    c.act(rho.ap, xr.ap, AF.Exp, rho, [xr])
    c.ts("dve", t1.ap, xi.ap, 1.0 / 16, None, ALU.mult, None, t1, [xi])
    c.ts("dve", t2.ap, xi.ap, 1.0 / 16, math.pi / 2, ALU.mult, ALU.add, t2, [xi])
    br_ = c.alloc("lb_r", 64); bi_ = c.alloc("lb_i", 64)
    c.act(bi_.ap, t1.ap, AF.Sin, bi_, [t1])
    c.act(br_.ap, t2.ap, AF.Sin, br_, [t2])

    def csq(dr_, di_, drb, dib, sr_, si_, srb, sib):
        c.tt("dve", t1.ap, sr_, sr_, ALU.mult, t1, [srb])
        c.tt("dve", t2.ap, si_, si_, ALU.mult, t2, [sib])
        c.tt("dve", tq.ap, sr_, si_, ALU.mult, tq, [srb, sib])
        c.tt("dve", dr_, t1.ap, t2.ap, ALU.subtract, drb, [t1, t2])
        c.ts("dve", di_, tq.ap, 2.0, None, ALU.mult, None, dib, [tq])
    for q in range(4):
        if q == 3:
            csq(ur.ap[:, 0:64], ui.ap[:, 0:64], ur, ui, br_.ap, bi_.ap, br_, bi_)
        else:
            csq(br_.ap, bi_.ap, br_, bi_, br_.ap, bi_.ap, br_, bi_)
    for k in range(1, NLV + 1):
        csq(ur.ap[:, k * 64:(k + 1) * 64], ui.ap[:, k * 64:(k + 1) * 64], ur, ui,
            ur.ap[:, (k - 1) * 64:k * 64], ui.ap[:, (k - 1) * 64:k * 64], ur, ui)
    c.ts("dve", un.ap, ui.ap, -1.0, None, ALU.mult, None, un, [ui])
    bits = [k for k in range(NLV + 1) if (CH >> k) & 1]
    c.copy("dve", wr.ap, ur.ap[:, bits[0] * 64:(bits[0] + 1) * 64], wr, [ur])
    c.copy("dve", wi.ap, ui.ap[:, bits[0] * 64:(bits[0] + 1) * 64], wi, [ui])
    for k in bits[1:]:
        a_, b_ = ur.ap[:, k * 64:(k + 1) * 64], ui.ap[:, k * 64:(k + 1) * 64]
        c.tt("dve", t1.ap, wr.ap, a_, ALU.mult, t1, [wr, ur])
        c.tt("dve", t2.ap, wi.ap, b_, ALU.mult, t2, [wi, ui])
        c.tt("dve", tq.ap, wr.ap, b_, ALU.mult, tq, [wr, ui])
        c.tt("dve", wi.ap, wi.ap, a_, ALU.mult, wi, [wi, ur])
        c.tt("dve", wi.ap, wi.ap, tq.ap, ALU.add, wi, [wi, tq])
        c.tt("dve", wr.ap, t1.ap, t2.ap, ALU.subtract, wr, [t1, t2])
    c.ts("dve", wn.ap, wi.ap, -1.0, None, ALU.mult, None, wn, [wi])
    c.tt("dve", lbr.ap, rho.ap, ur.ap[:, 0:64], ALU.mult, lbr, [rho, ur])
    c.tt("dve", lbi.ap, rho.ap, ui.ap[:, 0:64], ALU.mult, lbi, [rho, ui])
    c.ts("dve", t1.ap, lbr.ap, -1.0, None, ALU.add, None, t1, [lbr])
    c.tt("dve", t2.ap, are.ap, are.ap, ALU.mult, t2, [are])
    c.tt("dve", xr.ap, aim.ap, aim.ap, ALU.mult, xr, [aim])
    c.tt("dve", t2.ap, t2.ap, xr.ap, ALU.add, t2, [t2, xr])
    c.op("dve", lambda e: e.reciprocal(out=t2.ap, in_=t2.ap), [t2], [t2])
    c.tt("dve", cr.ap, t1.ap, are.ap, ALU.mult, cr, [t1, are])
    c.tt("dve", xr.ap, lbi.ap, aim.ap, ALU.mult, xr, [lbi, aim])
    c.tt("dve", cr.ap, cr.ap, xr.ap, ALU.add, cr, [cr, xr])
    c.tt("dve", cr.ap, cr.ap, t2.ap, ALU.mult, cr, [cr, t2])
    c.tt("dve", ci.ap, lbi.ap, are.ap, ALU.mult, ci, [lbi, are])
    c.tt("dve", xr.ap, t1.ap, aim.ap, ALU.mult, xr, [t1, aim])
    c.tt("dve", ci.ap, ci.ap, xr.ap, ALU.subtract, ci, [ci, xr])
    c.tt("dve", ci.ap, ci.ap, t2.ap, ALU.mult, ci, [ci, t2])
    c.ts("dve", nci.ap, ci.ap, -1.0, None, ALU.mult, None, nci, [ci])
    u = c.alloc("s5u", cfg.npos)
    yacc = c.alloc("s5y", cfg.seq)
    bb = [c.alloc(f"s5b{k}", L) for k in range(2)]
    mb = [c.alloc(f"s5m{k}", L) for k in range(2)]
    tmpL = c.alloc("s5tmp", L)
    Ec = c.alloc("s5Ec", CH); Es = c.alloc("s5Es", CH)
    rt = c.alloc("s5rt", CH)
    onesC = c.alloc("s5ones", CH)
    c.memset("pool", onesC.ap, 1.0, onesC)
    car = [c.alloc(f"s5car{i}", 4) for i in range(2)]
    braw = [c.alloc(f"braw{i}", 16) for i in range(2)]
    bbar = [c.alloc(f"bbar{i}", 16) for i in range(2)]
    xpad = [c.alloc(f"xpad{i}", 128) for i in range(2)]
    lB = [c.alloc(f"lB{i}", 128) for i in range(2)]
    craw = [c.alloc(f"craw{i}", 128) for i in range(2)]
    lC = [c.alloc(f"lC{i}", 128) for i in range(2)]
    lG = c.alloc("lG", 128)
    gt = c.alloc("s5g", cfg.seq)
    yo = c.alloc("s5o", cfg.seq, BF16)
    nblk_l = (L + 511) // 512
    nblk_s = (cfg.seq + 511) // 512
    bre, bim = T["s5_bre"].ap(), T["s5_bim"].ap()
    cre, cim = T["s5_cre"].ap(), T["s5_cim"].ap()
    gw = T["s5_gluw"].ap()
    YM = c.YM.ap()

    def rev(ap_, lo, n, base):
        hi = base + L - 1 - lo
        end = hi - n
        return ap_[:, hi:(end if end >= 0 else None):-1]

    for ch in range(8):
        c.dma("sp", u.ap, P[ch * 128:(ch + 1) * 128, :], u, c.Pk)
        first = True
        for d in range(2):
            off = 0 if d == 0 else cfg.ctx
            for gpl in range(4):
                gp = ch * 4 + gpl
                col = d * 32 + gp
                rowb = (d * 64 + gp * 2) * 64
                c.dma("sp", braw[0].ap, bre[rowb:rowb + 128, :], braw[0], c.Dk["s5_bre"])
                c.dma("sp", braw[1].ap, bim[rowb:rowb + 128, :], braw[1], c.Dk["s5_bim"])
                crc, cic, ncic = cr.ap[:, col:col + 1], ci.ap[:, col:col + 1], nci.ap[:, col:col + 1]
                c.ts("dve", bbar[0].ap, braw[0].ap, crc, None, ALU.mult, None, bbar[0], [braw[0], cr])
                c.stt("dve", bbar[0].ap, braw[1].ap, ncic, bbar[0].ap, ALU.mult, ALU.add, bbar[0], [braw[1], nci, bbar[0]])
                c.ts("dve", bbar[1].ap, braw[1].ap, crc, None, ALU.mult, None, bbar[1], [braw[1], cr])
                c.stt("dve", bbar[1].ap, braw[0].ap, cic, bbar[1].ap, ALU.mult, ALU.add, bbar[1], [braw[0], ci, bbar[1]])
                for k in range(2):
                    c.memset("pool", xpad[k].ap, 0.0, xpad[k])
                    c.copy("pool", xpad[k].ap[0:64, 32 * gpl:32 * gpl + 16], bbar[k].ap[0:64, :], xpad[k], [bbar[k]])
                    c.copy("pool", xpad[k].ap[64:128, 32 * gpl + 16:32 * gpl + 32], bbar[k].ap[64:128, :], xpad[k], [bbar[k]])
                    ps = c.bank(6 + k)
                    c.tr(ps.ap[:, 0:128], xpad[k].ap, c.ident.ap, ps, [xpad[k], c.ident])
                    c.copy("act", lB[k].ap, ps.ap[:, 0:128], lB[k], [ps])
                rowc = (d * 64 + gp * 2) * 16
                for k, src in ((0, cre), (1, cim)):
                    c.memset("pool", craw[k].ap[0:32, :], 0.0, craw[k])
                    c.dma("sp", craw[k].ap[0:16, 0:64], src[rowc:rowc + 16, :], craw[k], c.Dk["s5_cre" if k == 0 else "s5_cim"])
                    c.dma("sp", craw[k].ap[16:32, 64:128], src[rowc + 16:rowc + 32, :], craw[k], c.Dk["s5_cre" if k == 0 else "s5_cim"])
                    ps = c.bank(6 + k)
                    c.tr(ps.ap[:, 0:32], craw[k].ap[0:32, :], c.ident.ap[0:32, 0:32], ps, [craw[k], c.ident])
                    c.memset("pool", lC[k].ap, 0.0, lC[k])
                    if k == 0:
                        c.copy("act", lC[k].ap[:, 32 * gpl:32 * gpl + 32], ps.ap[:, 0:32], lC[k], [ps])
                    else:
                        c.op("act", lambda e, o=lC[k].ap[:, 32 * gpl:32 * gpl + 32], i=ps.ap[:, 0:32]: e.mul(out=o, in_=i, mul=-1.0),
                             [ps], [lC[k]])
                c.memset("pool", Ec.ap[:, 0:1], 1.0, Ec)
                c.memset("pool", Es.ap[:, 0:1], 0.0, Es)
                for k in range(NLV):
                    n_ = 1 << k
                    cnt = min(n_, CH - n_)
                    if cnt <= 0:
                        break
                    a_ = ur.ap[:, k * 64 + col:k * 64 + col + 1]
                    b_ = ui.ap[:, k * 64 + col:k * 64 + col + 1]
                    nb_ = un.ap[:, k * 64 + col:k * 64 + col + 1]
                    c.ts("dve", Ec.ap[:, n_:n_ + cnt], Ec.ap[:, 0:cnt], a_, None, ALU.mult, None, Ec, [Ec, ur])
                    c.stt("dve", Ec.ap[:, n_:n_ + cnt], Es.ap[:, 0:cnt], nb_, Ec.ap[:, n_:n_ + cnt], ALU.mult, ALU.add, Ec, [Es, un, Ec])
                    c.ts("dve", Es.ap[:, n_:n_ + cnt], Es.ap[:, 0:cnt], a_, None, ALU.mult, None, Es, [Es, ur])
                    c.stt("dve", Es.ap[:, n_:n_ + cnt], Ec.ap[:, 0:cnt], b_, Es.ap[:, n_:n_ + cnt], ALU.mult, ALU.add, Es, [Ec, ui, Es])
                c.ts("dve", rt.ap, onesC.ap, rho.ap[:, col:col + 1], None, ALU.mult, None, rt, [onesC, rho])
                for blk in range(nblk_l):
                    n = min(512, L - blk * 512)
                    for k in range(2):
                        ps = c.bank((blk * 2 + k) % 4)
                        rhs = u.ap[:, off + blk * 512:off + blk * 512 + n] if d == 0 else rev(u.ap, blk * 512, n, off)
                        c.mm(ps.ap[:, 0:n], lB[k].ap, rhs, ps, [lB[k], u])
                        c.copy("act", bb[k].ap[:, blk * 512:blk * 512 + n], ps.ap[:, 0:n], bb[k], [ps])
                v3 = lambda t_: t_.ap.rearrange("p (c j) -> p c j", j=CH)
                Ecb = Ec.ap.unsqueeze(1).to_broadcast([128, NCH, CH])
                Esb = Es.ap.unsqueeze(1).to_broadcast([128, NCH, CH])
                c.tt("dve", v3(tmpL), v3(bb[1]), Esb, ALU.mult, tmpL, [bb[1], Es])
                c.tt("dve", v3(mb[0]), v3(bb[0]), Ecb, ALU.mult, mb[0], [bb[0], Ec])
                c.tt("dve", mb[0].ap, mb[0].ap, tmpL.ap, ALU.add, mb[0], [mb[0], tmpL])
                c.tt("dve", v3(tmpL), v3(bb[0]), Esb, ALU.mult, tmpL, [bb[0], Es])
                c.tt("dve", v3(mb[1]), v3(bb[1]), Ecb, ALU.mult, mb[1], [bb[1], Ec])
                c.tt("dve", mb[1].ap, mb[1].ap, tmpL.ap, ALU.subtract, mb[1], [mb[1], tmpL])
                wrc, wic, wnc = wr.ap[:, col:col + 1], wi.ap[:, col:col + 1], wn.ap[:, col:col + 1]
                for cc in range(NCH):
                    sl = slice(cc * CH, (cc + 1) * CH)
                    cb = car[cc % 2]
                    for k in range(2):
                        init = 0.0 if cc == 0 else cb.ap[:, k:k + 1]
                        c.op("dve", lambda e, o=bb[k].ap[:, sl], i=mb[k].ap[:, sl], init=init: e.tensor_tensor_scan(
                            out=o, data0=rt.ap, data1=i, initial=init, op0=ALU.mult, op1=ALU.add),
                            [mb[k], rt] + ([cb] if cc > 0 else []), [bb[k]])
                    if cc < NCH - 1:
                        nb_ = car[(cc + 1) % 2]
                        lr, li = bb[0].ap[:, (cc + 1) * CH - 1:(cc + 1) * CH], bb[1].ap[:, (cc + 1) * CH - 1:(cc + 1) * CH]
                        c.ts("dve", nb_.ap[:, 2:3], li, wnc, None, ALU.mult, None, nb_, [bb[1], wn])
                        c.stt("dve", nb_.ap[:, 0:1], lr, wrc, nb_.ap[:, 2:3], ALU.mult, ALU.add, nb_, [bb[0], wr, nb_])
                        c.ts("dve", nb_.ap[:, 3:4], lr, wic, None, ALU.mult, None, nb_, [bb[0], wi])
                        c.stt("dve", nb_.ap[:, 1:2], li, wrc, nb_.ap[:, 3:4], ALU.mult, ALU.add, nb_, [bb[1], wr, nb_])
                c.tt("dve", v3(tmpL), v3(bb[1]), Esb, ALU.mult, tmpL, [bb[1], Es])
                c.tt("dve", v3(mb[0]), v3(bb[0]), Ecb, ALU.mult, mb[0], [bb[0], Ec])
                c.tt("dve", mb[0].ap, mb[0].ap, tmpL.ap, ALU.subtract, mb[0], [mb[0], tmpL])
                c.tt("dve", v3(tmpL), v3(bb[0]), Esb, ALU.mult, tmpL, [bb[0], Es])
                c.tt("dve", v3(mb[1]), v3(bb[1]), Ecb, ALU.mult, mb[1], [bb[1], Ec])
                c.tt("dve", mb[1].ap, mb[1].ap, tmpL.ap, ALU.add, mb[1], [mb[1], tmpL])
                for blk in range(nblk_s):
                    n = min(512, cfg.seq - blk * 512)
                    ps = c.bank(4 + blk % 2)
                    for k in range(2):
                        if d == 0:
                            rhs = mb[k].ap[:, cfg.ctx + blk * 512:cfg.ctx + blk * 512 + n]
                        else:
                            hi = L - 1 - blk * 512
                            end = hi - n
                            rhs = mb[k].ap[:, hi:(end if end >= 0 else None):-1]
                        c.mm(ps.ap[:, 0:n], lC[k].ap, rhs, ps, [lC[k], mb[k]], start=(k == 0), stop=(k == 1))
                    ysl = yacc.ap[:, blk * 512:blk * 512 + n]
                    if first:
                        c.copy("dve", ysl, ps.ap[:, 0:n], yacc, [ps])
                    else:
                        c.tt("dve", ysl, ysl, ps.ap[:, 0:n], ALU.add, yacc, [ps, yacc])
                first = False
        ul = u.ap[:, cfg.ctx:cfg.ctx + cfg.seq]
        c.stt("dve", yacc.ap, ul, dT.ap[:, ch:ch + 1], yacc.ap, ALU.mult, ALU.add, yacc, [u, dT, yacc])
        c.tt("dve", gt.ap, yacc.ap, yacc.ap, ALU.mult, gt, [yacc])
        c.ts("dve", gt.ap, gt.ap, 0.044715, 1.0, ALU.mult, ALU.add, gt, [gt])
        c.tt("dve", gt.ap, gt.ap, yacc.ap, ALU.mult, gt, [gt, yacc])
        c.act(gt.ap, gt.ap, AF.Tanh, gt, [gt], scale=0.7978845608028654)
        c.ts("dve", gt.ap, gt.ap, 1.0, 0.5, ALU.add, ALU.mult, gt, [gt])
        c.tt("dve", yacc.ap, yacc.ap, gt.ap, ALU.mult, yacc, [yacc, gt])
        c.memset("pool", lG.ap, 0.0, lG)
        for g in range(8):
            c.dma("sp", lG.ap[16 * g:16 * g + 16, 16 * g:16 * g + 16], gw[(ch * 8 + g) * 16:(ch * 8 + g) * 16 + 16, :],
                  lG, c.Dk["s5_gluw"])
        for blk in range(nblk_s):
            n = min(512, cfg.seq - blk * 512)
            ps = c.bank(4 + blk % 2)
            c.mm(ps.ap[:, 0:n], lG.ap, yacc.ap[:, blk * 512:blk * 512 + n], ps, [lG, yacc])
            c.act(gt.ap[:, blk * 512:blk * 512 + n], ps.ap[:, 0:n], AF.Sigmoid, gt, [ps, gbT], bias=gbT.ap[:, ch:ch + 1])
        c.tt("dve", yo.ap, yacc.ap, gt.ap, ALU.mult, yo, [yacc, gt])
        c.dma("pool", YM[ch * 128:(ch + 1) * 128, :], yo.ap, c.YMk, yo)
    c.stage_end()


def stage_rw_lora(c):
    cfg, T = c.cfg, c.T
    ZS, LO = c.ZS.ap(), c.LO.ap()
    NP_ = cfg.npos
    w0T = load_colvec(c, T["rw_w0"].ap(), c.Dk["rw_w0"], 16, "w0T")
    a0T = load_colvec(c, T["rw_a0"].ap(), c.Dk["rw_a0"], 16, "a0T")
    txw = c.alloc("txw", NP_); xa = c.alloc("xa", NP_)
    sg0 = c.alloc("sg0", cfg.seq); sg1 = c.alloc("sg1", cfg.seq)
    w2 = c.alloc("w2", 1024); a2 = c.alloc("a2", 1024); g2a = c.alloc("g2a", 1024); g2b = c.alloc("g2b", 1024)
    c.dma("sp", txw.ap, ZS[3072:3200, :], txw, c.ZSk)
    c.dma("sp", xa.ap, ZS[3200:3328, :], xa, c.ZSk)
    c.dma("sp", sg0.ap, ZS[3328:3456, cfg.ctx:cfg.ctx + cfg.seq], sg0, c.ZSk)
    c.dma("sp", sg1.ap[0:32, :], ZS[3456:3488, cfg.ctx:cfg.ctx + cfg.seq], sg1, c.ZSk)
    c.dma("sp", w2.ap, T["rw_w2"].ap(), w2, c.Dk["rw_w2"])
    c.dma("sp", a2.ap, T["rw_a2"].ap(), a2, c.Dk["rw_a2"])
    c.dma("sp", g2a.ap, T["rw_g2"].ap()[0:128, :], g2a, c.Dk["rw_g2"])
    c.dma("sp", g2b.ap[0:32, :], T["rw_g2"].ap()[128:160, :], g2b, c.Dk["rw_g2"])
    c.act(txw.ap, txw.ap, AF.Tanh, txw, [txw])
    c.act(sg0.ap, sg0.ap, AF.Sigmoid, sg0, [sg0])
    c.act(sg1.ap[0:32, :], sg1.ap[0:32, :], AF.Sigmoid, sg1, [sg1])
    ob = [c.alloc(f"lo{i}", NP_) for i in range(2)]
    oi = 0
    nb = (NP_ + 511) // 512
    for hp in range(8):
        cs = slice(hp * 128, (hp + 1) * 128)
        for kind in range(5):
            o = ob[oi % 2]; oi += 1
            if kind < 4:
                d = kind % 2
                wt, src, bT = (w2, txw, w0T) if kind < 2 else (a2, xa, a0T)
                for blk in range(nb):
                    n = min(512, NP_ - blk * 512)
                    ps = c.bank(blk % 4)
                    c.mm(ps.ap[:, 0:n], wt.ap[d * 64:(d + 1) * 64, cs], src.ap[d * 64:(d + 1) * 64, blk * 512:blk * 512 + n],
                         ps, [wt, src])
                    c.act(o.ap[:, blk * 512:blk * 512 + n], ps.ap[:, 0:n], AF.Sigmoid, o, [ps, bT],
                          bias=bT.ap[:, d * 8 + hp:d * 8 + hp + 1])
                c.dma("pool", LO[kind, hp], o.ap, c.LOk, o)
            else:
                for blk in range((cfg.seq + 511) // 512):
                    n = min(512, cfg.seq - blk * 512)
                    ps = c.bank(blk % 4)
                    c.mm(ps.ap[:, 0:n], g2a.ap[:, cs], sg0.ap[:, blk * 512:blk * 512 + n], ps, [g2a, sg0], start=True, stop=False)
                    c.mm(ps.ap[:, 0:n], g2b.ap[0:32, cs], sg1.ap[0:32, blk * 512:blk * 512 + n], ps, [g2b, sg1], start=False, stop=True)
                    c.copy("act", o.ap[:, blk * 512:blk * 512 + n], ps.ap[:, 0:n], o, [ps])
                c.dma("pool", LO[4, hp, :, 0:cfg.seq], o.ap[:, 0:cfg.seq], c.LOk, o)
    c.stage_end()


def rw_consts(c):
    m = {}
    for name, pat, base, cm, cmp_ in (("SU", 1, 0, -1, ALU.is_gt), ("SL", -1, 0, 1, ALU.is_gt),
                                       ("IU", 1, 0, -1, ALU.is_ge), ("IL", -1, 0, 1, ALU.is_ge)):
        t = c.alloc("mask" + name, 128)
        c.memset("pool", t.ap, 1.0, t)
        c.op("pool", lambda e, t=t, pat=pat, base=base, cm=cm, cmp_=cmp_: e.affine_select(
            out=t.ap, in_=t.ap, pattern=[[pat, 128]], compare_op=cmp_, fill=0.0, base=base, channel_multiplier=cm), [t], [t])
        for j in range(NSUB):
            if j > 0:
                c.memset("pool", t.ap[j * CS:(j + 1) * CS, 0:j * CS], 0.0, t)
            if j < NSUB - 1:
                c.memset("pool", t.ap[j * CS:(j + 1) * CS, (j + 1) * CS:128], 0.0, t)
        m[name] = t
    bo = c.alloc("blockones", 128)
    c.memset("pool", bo.ap, 0.0, bo)
    c.memset("pool", bo.ap[0:64, 0:64], 1.0, bo)
    c.memset("pool", bo.ap[64:128, 64:128], 1.0, bo)
    hs = c.alloc("headsel", 2)
    c.memset("pool", hs.ap, 0.0, hs)
    c.memset("pool", hs.ap[0:64, 0:1], 1.0, hs)
    c.memset("pool", hs.ap[64:128, 1:2], 1.0, hs)
    ones1 = c.alloc("ones1", 128)
    c.memset("pool", ones1.ap[0:1, :], 1.0, ones1)
    m["BO"], m["HS"], m["ONES"] = bo, hs, ones1
    return m


def stage_rw(c):
    cfg, T = c.cfg, c.T
    ZS, LO, RW, YD, YM = c.ZS.ap(), c.LO.ap(), c.RW.ap(), c.YD.ap(), c.YM.ap()
    NP_, L = cfg.npos, cfg.ntok
    nblk = L // 128
    nlat = cfg.seq // 128
    nctx = cfg.ctx // 128
    M = rw_consts(c)
    kkT = load_colvec(c, T["rw_kk"].ap(), c.Dk["rw_kk"], 8, "kkT")
    kaT = load_colvec(c, T["rw_ka"].ap(), c.Dk["rw_ka"], 8, "kaT")
    rkT = load_colvec(c, T["rw_rk"].ap(), c.Dk["rw_rk"], 8, "rkT")
    lnrow = c.alloc("lnrow", 2048)
    c.dma("sp", lnrow.ap[0:1, 0:1024], T["rw_lnw"].ap(), lnrow, c.Dk["rw_lnw"])
    c.dma("sp", lnrow.ap[0:1, 1024:2048], T["rw_lnb"].ap(), lnrow, c.Dk["rw_lnb"])
    lnbc = c.alloc("lnbc", 2048)
    for q in range(4):
        ps = c.bank(q)
        c.mm(ps.ap[:, 0:512], M["ONES"].ap[0:1, :], lnrow.ap[0:1, q * 512:(q + 1) * 512], ps, [M["ONES"], lnrow])
        c.copy("act", lnbc.ap[:, q * 512:(q + 1) * 512], ps.ap[:, 0:512], lnbc, [ps])
    bonus = c.alloc("bonus", nlat * 2)
    gCt = c.alloc("gCt", 2 * (NP_ // CS))
    SEG = 1536 if NP_ % 1536 == 0 else NP_
    rmask = c.alloc("rmask", SEG)
    c.memset("pool", rmask.ap, 1.0, rmask)
    c.memset("pool", rmask.ap[:, 0:SEG:CS], 0.0, rmask)
    c.pers2 = c.top
    nseg = NP_ // SEG
    for hp in range(8):
        c.p.barrier()
        c.top = c.pers2
        names = ["r", "k", "kh", "kka", "sg", "a", "cs", "lg", "kt", "E", "q"]
        S = {n_: c.alloc("f_" + n_, SEG) for n_ in names}
        O = [c.alloc(f"f_o{i}", SEG) for i in range(3)]
        oi = 0
        for sg_i in range(nseg):
            p0 = sg_i * SEG
            psl = slice(p0, p0 + SEG)
            nch = SEG // CS
            c.dma("sp", S["r"].ap, ZS[hp * 128:(hp + 1) * 128, psl], S["r"], c.ZSk)
            c.dma("sp", S["k"].ap, ZS[1024 + hp * 128:1024 + (hp + 1) * 128, psl], S["k"], c.ZSk)
            kkc, kac, rkc = kkT.ap[:, hp:hp + 1], kaT.ap[:, hp:hp + 1], rkT.ap[:, hp:hp + 1]
            c.ts("dve", S["kh"].ap, S["k"].ap, kkc, None, ALU.mult, None, S["kh"], [S["k"], kkT])
            c.tt("dve", S["E"].ap, S["kh"].ap, S["kh"].ap, ALU.mult, S["E"], [S["kh"]])
            for blk in range((SEG + 511) // 512):
                n = min(512, SEG - blk * 512)
                ps = c.bank(blk % 4)
                c.mm(ps.ap[:, 0:n], M["BO"].ap, S["E"].ap[:, blk * 512:blk * 512 + n], ps, [M["BO"], S["E"]])
                c.act(S["cs"].ap[:, blk * 512:blk * 512 + n], ps.ap[:, 0:n], AF.Sqrt, S["cs"], [ps])
            c.ts("dve", S["cs"].ap, S["cs"].ap, 1e-12, None, ALU.max, None, S["cs"], [S["cs"]])
            c.op("dve", lambda e, o=S["cs"].ap: e.reciprocal(out=o, in_=o), [S["cs"]], [S["cs"]])
            c.tt("dve", S["kh"].ap, S["kh"].ap, S["cs"].ap, ALU.mult, S["kh"], [S["kh"], S["cs"]])
            c.ts("dve", S["kka"].ap, S["k"].ap, kac, None, ALU.mult, None, S["kka"], [S["k"], kaT])
            for d in range(2):
                c.dma("sp", S["sg"].ap, LO[d, hp, :, psl], S["sg"], c.LOk)
                c.dma("sp", S["a"].ap, LO[2 + d, hp, :, psl], S["a"], c.LOk)
                c.ts("dve", S["sg"].ap, S["sg"].ap, -math.exp(-0.5), None, ALU.mult, None, S["sg"], [S["sg"]])
                csv = S["cs"].ap.rearrange("p (c t) -> p c t", t=CS)
                sgv = S["sg"].ap.rearrange("p (c t) -> p c t", t=CS)
                lgv = S["lg"].ap.rearrange("p (c t) -> p c t", t=CS)
                c.op("dve", lambda e, o=S["cs"].ap, i=S["sg"].ap: e.tensor_tensor_scan(
                    out=o, data0=rmask.ap, data1=i, initial=0.0, op0=ALU.mult, op1=ALU.add),
                    [S["sg"], rmask], [S["cs"]])
                ctot = csv[:, :, CS - 1:CS]
                if d == 0:
                    c.copy("act", S["lg"].ap, S["cs"].ap, S["lg"], [S["cs"]])
                else:
                    c.tt("dve", lgv, sgv, csv, ALU.subtract, S["lg"], [S["sg"], S["cs"]])
                    c.tt("dve", lgv, lgv, ctot.to_broadcast([128, nch, CS]), ALU.add, S["lg"], [S["lg"], S["cs"]])
                gcs = gCt.ap[:, d * (NP_ // CS) + p0 // CS:d * (NP_ // CS) + p0 // CS + nch]
                c.act(gcs, S["cs"].ap[:, CS - 1:SEG:CS], AF.Exp, gCt, [S["cs"]])
                c.stt("dve", S["kt"].ap, S["a"].ap, -1.0, S["kka"].ap, ALU.add, ALU.mult, S["kt"], [S["a"], S["kka"]])
                c.tt("dve", S["kt"].ap, S["kt"].ap, S["k"].ap, ALU.add, S["kt"], [S["kt"], S["k"]])
                c.tt("dve", S["a"].ap, S["a"].ap, S["kh"].ap, ALU.mult, S["a"], [S["a"], S["kh"]])
                if d == 0:
                    c.stt("dve", S["q"].ap, S["r"].ap, rkc, S["kt"].ap, ALU.mult, ALU.mult, S["q"], [S["r"], rkT, S["kt"]])
                else:
                    c.stt("dve", S["E"].ap, S["r"].ap, rkc, S["kt"].ap, ALU.mult, ALU.mult, S["E"], [S["r"], rkT, S["kt"]])
                    c.tt("dve", S["q"].ap, S["q"].ap, S["E"].ap, ALU.add, S["q"], [S["q"], S["E"]])

                def emit_out(idx, base, ex, neg):
                    nonlocal oi
                    o = O[oi % 3]; oi += 1
                    c.tt("dve", o.ap, base.ap, ex.ap, ALU.mult, o, [base, ex])
                    if neg:
                        c.op("act", lambda e, o=o: e.mul(out=o.ap, in_=o.ap, mul=-1.0), [o], [o])
                    c.dma("pool", RW[d, hp, :, idx, psl], o.ap, c.RWk, o)
                c.act(S["E"].ap, S["lg"].ap, AF.Exp, S["E"], [S["lg"]], scale=-1.0)
                emit_out(0, S["a"], S["E"], True)
                emit_out(2, S["kt"], S["E"], False)
                c.act(S["E"].ap, S["lg"].ap, AF.Exp, S["E"], [S["lg"]])
                emit_out(3, S["r"], S["E"], False)
                c.tt("dve", S["E"].ap, S["lg"].ap, S["sg"].ap, ALU.subtract, S["E"], [S["lg"], S["sg"]])
                c.act(S["E"].ap, S["E"].ap, AF.Exp, S["E"], [S["E"]])
                emit_out(1, S["kh"], S["E"], False)
                Ev = S["E"].ap.rearrange("p (c t) -> p c t", t=CS)
                c.tt("dve", Ev, ctot.to_broadcast([128, nch, CS]), lgv, ALU.subtract, S["E"], [S["cs"], S["lg"]])
                c.act(S["E"].ap, S["E"].ap, AF.Exp, S["E"], [S["E"]])
                emit_out(4, S["a"], S["E"], True)
                emit_out(5, S["kt"], S["E"], False)
            for bl in range(SEG // 128):
                gpos = p0 + bl * 128
                if gpos < cfg.ctx or gpos >= cfg.ctx + cfg.seq:
                    continue
                lb = (gpos - cfg.ctx) // 128
                ps = c.bank(4 + bl % 2)
                c.mm(ps.ap[:, 0:2], S["q"].ap[:, bl * 128:(bl + 1) * 128], M["HS"].ap, ps, [S["q"], M["HS"]])
                c.copy("act", bonus.ap[:, lb * 2:lb * 2 + 2], ps.ap[:, 0:2], bonus, [ps])
        c.p.barrier()
        c.top = c.pers2
        rw_chain(c, hp, M, gCt)
        c.p.barrier()
        c.top = c.pers2
        rw_readout(c, hp, M, bonus, lnbc)
    c.stage_end()


def rw_chain(c, hp, M, gCt):
    cfg = c.cfg
    ZS, RW, YD = c.ZS.ap(), c.RW.ap(), c.YD.ap()
    NP_, L = cfg.npos, cfg.ntok
    nblk = L // 128
    NG = 2
    ident = c.ident
    H = [[c.alloc(f"H{d}{hl}", 64) for hl in range(2)] for d in range(2)]
    for d in range(2):
        for hl in range(2):
            c.memset("pool", H[d][hl].ap[0:64, :], 0.0, H[d][hl])
    nset = 2
    gC1 = c.alloc("gC1", 2 * (NP_ // CS))
    c.dma("sp", gC1.ap[0:64, :], gCt.ap[64:128, :], gC1, gCt)
    gC = [gCt, gC1]
    def mk(name, n):
        return [[[c.alloc(f"{name}{s_}{g}{d}", n) for d in range(2)] for g in range(NG)] for s_ in range(nset)]
    OP = [[[[c.alloc(f"op{s_}{g}{d}{hl}", 6 * 128) for hl in range(2)] for d in range(2)] for g in range(NG)] for s_ in range(nset)]
    VF = mk("vf", 128)
    VT = mk("vt", 128)
    TM = [[[[c.alloc(f"tm{s_}{g}{d}{hl}", 192) for hl in range(2)] for d in range(2)] for g in range(NG)] for s_ in range(nset)]
    RES = [[[[c.alloc(f"res{s_}{g}{d}{hl}", 2 * NSUB * 64 + 192) for hl in range(2)] for d in range(2)] for g in range(NG)] for s_ in range(nset)]
    TMP = [[[[[c.alloc(f"t{s_}{g}{d}{hl}{i}", 128) for i in range(9)] for hl in range(2)] for d in range(2)] for g in range(NG)] for s_ in range(nset)]
    YO = [[c.alloc(f"yo{s_}{d}", 128) for d in range(2)] for s_ in range(4)]
    slot_ctr = [0]

    def unit(s_, g, d, hl, blk_pos, islat, slotbase, gci0, gcb):
        op = OP[s_][g][d][hl]
        opv = op.ap.rearrange("p (i t) -> p i t", i=6)
        At, Bt, Kt, Rt, Ab, Kb = (opv[0:64, i, :] for i in range(6))
        tm = TM[s_][g][d][hl]
        Btm, Abm, Kbm = tm.ap[:, 0:64], tm.ap[:, 64:128], tm.ap[:, 128:192]
        Vtm = VT[s_][g][d].ap[:, hl * 64:(hl + 1) * 64]
        vtb = VT[s_][g][d]
        res = RES[s_][g][d][hl]
        PhiT, Psi = res.ap[0:64, 0:NSUB * 64], res.ap[0:64, NSUB * 64:2 * NSUB * 64]
        PT, Y2 = res.ap[0:64, 2 * NSUB * 64:2 * NSUB * 64 + 128], res.ap[:, 2 * NSUB * 64 + 128:2 * NSUB * 64 + 192]
        t = TMP[s_][g][d][hl]
        strict = M["SU"] if d == 0 else M["SL"]
        strictT = M["SL"] if d == 0 else M["SU"]
        incl = M["IU"] if d == 0 else M["IL"]

        def slot(i):
            bk = c.bank(slotbase)
            return B(bk.ap[:, i * 128:(i + 1) * 128], bk.key)
        s0, s1 = slot(0), slot(1)
        for src, dst in ((Bt, Btm), (Ab, Abm), (Kb, Kbm)):
            c.tr(s0.ap[:, 0:64], src, ident.ap[0:64, 0:64], s0, [op, ident])
            c.copy("act", dst, s0.ap[:, 0:64], tm, [s0])
            yield
        LAT, LA, LKT, MAT, MKT = t[0], t[1], t[4], t[5], t[6]
        c.mm(s0.ap, At, Bt, s0, [op]); c.tt("dve", LAT.ap, s0.ap, strict.ap, ALU.mult, LAT, [s0, strict])
        c.mm(s1.ap, Bt, At, s1, [op]); c.tt("dve", LA.ap, s1.ap, strictT.ap, ALU.mult, LA, [s1, strictT])
        yield
        c.mm(s0.ap, Kt, Bt, s0, [op]); c.tt("dve", LKT.ap, s0.ap, strict.ap, ALU.mult, LKT, [s0, strict])
        if islat:
            c.mm(s1.ap, At, Rt, s1, [op]); c.tt("dve", MAT.ap, s1.ap, incl.ap, ALU.mult, MAT, [s1, incl])
        yield
        if islat:
            c.mm(s0.ap, Kt, Rt, s0, [op]); c.tt("dve", MKT.ap, s0.ap, incl.ap, ALU.mult, MKT, [s0, incl])
        Z = [t[7], t[8]]
        c.mm(s1.ap[:, 0:64], LKT.ap, Vtm, s1, [LKT, vtb])
        c.copy("act", Z[0].ap[:, 64:128], s1.ap[:, 0:64], Z[0], [s1])
        c.copy("act", Z[0].ap[:, 0:64], Btm, Z[0], [tm])
        yield
        X, XT = LA, LAT
        alt = [(t[2], t[3]), (t[1], t[0])]
        zc = 0
        NL = int(math.log2(CS))
        for i in range(NL):
            c.mm(s0.ap, XT.ap, Z[zc].ap, s0, [XT, Z[zc]])
            c.tt("dve", Z[1 - zc].ap, Z[zc].ap, s0.ap, ALU.add, Z[1 - zc], [Z[zc], s0])
            zc = 1 - zc
            if i < NL - 1:
                nX, nXT = alt[i % 2]
                c.mm(s1.ap, X.ap, XT.ap, s1, [X, XT])
                c.copy("act", nXT.ap, s1.ap, nXT, [s1])
                if i < NL - 2:
                    yield
                    c.mm(s1.ap, XT.ap, X.ap, s1, [X, XT])
                    c.copy("act", nX.ap, s1.ap, nX, [s1])
                X, XT = nX, nXT
            yield
        Zf = Z[zc]
        W1, U2 = Zf.ap[:, 0:64], Zf.ap[:, 64:128]
        for j in range(NSUB):
            js = slice(j * CS, (j + 1) * CS)
            c.mm(s0.ap[0:64, 0:64], Zf.ap[js, 0:64], tm.ap[js, 64:128], s0, [Zf, tm])
            c.stt("dve", PhiT[:, j * 64:(j + 1) * 64], c.ident.ap[0:64, 0:64], gcb.ap[0:64, gci0 + j:gci0 + j + 1],
                  s0.ap[0:64, 0:64], ALU.mult, ALU.add, res, [c.ident, gcb, s0])
            c.mm(s1.ap[0:64, 0:64], tm.ap[js, 64:128], Zf.ap[js, 64:128], s1, [Zf, tm], start=True, stop=False)
            c.mm(s1.ap[0:64, 0:64], tm.ap[js, 128:192], VT[s_][g][d].ap[js, hl * 64:(hl + 1) * 64], s1, [tm, vtb], start=False, stop=True)
            c.copy("act", Psi[:, j * 64:(j + 1) * 64], s1.ap[0:64, 0:64], res, [s1])
            yield
        if islat:
            c.mm(s0.ap[:, 0:64], MAT.ap, U2, s0, [MAT, Zf], start=True, stop=False)
            c.mm(s0.ap[:, 0:64], MKT.ap, Vtm, s0, [MKT, vtb], start=False, stop=True)
            c.copy("act", Y2, s0.ap[:, 0:64], res, [s0])
            c.mm(s1.ap[0:64, :], W1, MAT.ap, s1, [Zf, MAT])
            c.tt("dve", PT, s1.ap[0:64, :], Rt, ALU.add, res, [s1, op])
        yield

    nsteps = nblk
    step = 0
    gi = 0
    yo_i = 0
    while step < nsteps:
        s_ = gi % nset
        gi += 1
        ng = min(NG, nsteps - step)
        gens = []
        info = []
        for g in range(ng):
            for d in range(2):
                bi = step + g
                blk = bi if d == 0 else (nblk - 1 - bi)
                pos = blk * 128 + (0 if d == 0 else cfg.ctx)
                islat = cfg.ctx <= pos < cfg.ctx + cfg.seq
                c.dma("sp", VF[s_][g][d].ap, ZS[2048 + hp * 128:2048 + (hp + 1) * 128, pos:pos + 128], VF[s_][g][d], c.ZSk)
                pb = c.bank(4 + d)
                c.tr(pb.ap[:, 0:128], VF[s_][g][d].ap, c.ident.ap, pb, [VF[s_][g][d], c.ident])
                c.copy("act", VT[s_][g][d].ap, pb.ap[:, 0:128], VT[s_][g][d], [pb])
                for hl in range(2):
                    op = OP[s_][g][d][hl]
                    c.dma("sp", op.ap[0:64, :].rearrange("p (i t) -> p i t", i=6),
                          RW[d, hp, hl * 64:(hl + 1) * 64, :, pos:pos + 128], op, c.RWk)
                    u_idx = (g * 2 + d) * 2 + hl
                    gci = d * (NP_ // CS) + pos // CS
                    gcb = gC[hl]
                    gens.append(unit(s_, g, d, hl, pos, islat, u_idx, gci, gcb))
                    info.append((g, d, hl, pos, islat))
        results = [None] * len(gens)
        live = list(range(len(gens)))
        while live:
            nxt = []
            for ui in live:
                try:
                    r = next(gens[ui])
                    if r is not None:
                        results[ui] = r
                    nxt.append(ui)
                except StopIteration:
                    pass
            live = nxt
        for g in range(ng):
            for d in range(2):
                bi = step + g
                blk = bi if d == 0 else (nblk - 1 - bi)
                pos = blk * 128 + (0 if d == 0 else cfg.ctx)
                islat = cfg.ctx <= pos < cfg.ctx + cfg.seq
                gcol_i = d * (NP_ // 128) + pos // 128
                yo = YO[yo_i % 4][d]
                for hl in range(2):
                    res = RES[s_][g][d][hl]
                    Hh = H[d][hl]
                    o2 = 2 * NSUB * 64
                    PT, Y2 = res.ap[0:64, o2:o2 + 128], res.ap[:, o2 + 128:o2 + 192]
                    pb = c.bank(6 + hl)
                    order = range(NSUB) if d == 0 else range(NSUB - 1, -1, -1)
                    for j in order:
                        js = slice(j * CS, (j + 1) * CS)
                        PhiT, Psi = res.ap[0:64, j * 64:(j + 1) * 64], res.ap[0:64, NSUB * 64 + j * 64:NSUB * 64 + (j + 1) * 64]
                        if islat:
                            pbs = B(pb.ap[js, 0:64], pb.key)
                            c.mm(pbs.ap, PT[:, js], Hh.ap[0:64, :], pbs, [res, Hh])
                            c.tt("dve", yo.ap[js, hl * 64:(hl + 1) * 64], pbs.ap, Y2[js, :], ALU.add, yo, [pbs, res])
                        pbs2 = B(pb.ap[0:64, 128:192], pb.key)
                        c.mm(pbs2.ap, PhiT, Hh.ap[0:64, :], pbs2, [res, Hh])
                        c.tt("dve", Hh.ap[0:64, :], pbs2.ap, Psi, ALU.add, Hh, [pbs2, res])
                if islat:
                    lpos = pos - cfg.ctx
                    c.dma("pool", YD[d, lpos:lpos + 128, hp * 128:(hp + 1) * 128], yo.ap, c.YDk, yo)
            yo_i += 1
        step += ng


def rw_readout(c, hp, M, bonus, lnbc):
    cfg = c.cfg
    ZS, LO, YD, YM = c.ZS.ap(), c.LO.ap(), c.YD.ap(), c.YM.ap()
    nlat = cfg.seq // 128
    yo = c.alloc("rd_out", cfg.seq, BF16)
    bufs = [[c.alloc(f"rd{n_}{i}", 128) for i in range(2)] for n_ in ("yf", "yb", "vf", "g", "t", "sq")]
    st = [c.alloc(f"rdst{i}", 8) for i in range(2)]
    for lb in range(nlat):
        yf, yb, vf, gf, tt_, sq = (bufs[i][lb % 2] for i in range(6))
        s_ = st[lb % 2]
        pos = cfg.ctx + lb * 128
        c.dma("sp", yf.ap, YD[0, lb * 128:(lb + 1) * 128, hp * 128:(hp + 1) * 128], yf, c.YDk)
        c.dma("sp", yb.ap, YD[1, lb * 128:(lb + 1) * 128, hp * 128:(hp + 1) * 128], yb, c.YDk)
        c.dma("sp", vf.ap, ZS[2048 + hp * 128:2048 + (hp + 1) * 128, pos:pos + 128], vf, c.ZSk)
        c.dma("sp", gf.ap, LO[4, hp, :, lb * 128:(lb + 1) * 128], gf, c.LOk)
        c.tt("dve", yf.ap, yf.ap, yb.ap, ALU.add, yf, [yf, yb])
        yv = yf.ap.rearrange("p (h v) -> p h v", h=2)
        c.op("dve", lambda e, o=s_.ap[:, 0:2], i=yv: e.tensor_reduce(out=o, in_=i, axis=AX.X, op=ALU.add), [yf], [s_])
        c.ts("dve", s_.ap[:, 0:2], s_.ap[:, 0:2], 1.0 / 64, None, ALU.mult, None, s_, [s_])
        c.tt("dve", yv, yv, s_.ap[:, 0:2].unsqueeze(2).to_broadcast([128, 2, 64]), ALU.subtract, yf, [yf, s_])
        c.tt("dve", sq.ap, yf.ap, yf.ap, ALU.mult, sq, [yf])
        c.op("dve", lambda e, o=s_.ap[:, 2:4], i=sq.ap.rearrange("p (h v) -> p h v", h=2): e.tensor_reduce(
            out=o, in_=i, axis=AX.X, op=ALU.add), [sq], [s_])
        c.ts("dve", s_.ap[:, 2:4], s_.ap[:, 2:4], 1.0 / 64, 64e-5, ALU.mult, ALU.add, s_, [s_])
        c.act(s_.ap[:, 2:4], s_.ap[:, 2:4], AF.Sqrt, s_, [s_])
        c.op("dve", lambda e, o=s_.ap[:, 2:4]: e.reciprocal(out=o, in_=o), [s_], [s_])
        c.tt("dve", yv, yv, s_.ap[:, 2:4].unsqueeze(2).to_broadcast([128, 2, 64]), ALU.mult, yf, [yf, s_])
        c.tt("dve", yf.ap, yf.ap, lnbc.ap[:, hp * 128:(hp + 1) * 128], ALU.mult, yf, [yf, lnbc])
        c.tt("dve", yf.ap, yf.ap, lnbc.ap[:, 1024 + hp * 128:1024 + (hp + 1) * 128], ALU.add, yf, [yf, lnbc])
        pb = c.bank(4 + lb % 2)
        c.tr(pb.ap[:, 0:128], vf.ap, c.ident.ap, pb, [vf, c.ident])
        tv = tt_.ap.rearrange("p (h v) -> p h v", h=2)
        c.tt("dve", tv, pb.ap[:, 0:128].rearrange("p (h v) -> p h v", h=2),
             bonus.ap[:, lb * 2:lb * 2 + 2].unsqueeze(2).to_broadcast([128, 2, 64]), ALU.mult, tt_, [pb, bonus])
        c.tt("dve", yf.ap, yf.ap, tt_.ap, ALU.add, yf, [yf, tt_])
        pb2 = c.bank(6 + lb % 2)
        c.tr(pb2.ap[:, 0:128], yf.ap, c.ident.ap, pb2, [yf, c.ident])
        c.tt("dve", yo.ap[:, lb * 128:(lb + 1) * 128], pb2.ap[:, 0:128], gf.ap, ALU.mult, yo, [pb2, gf])
    c.dma("pool", YM[1024 + hp * 128:1024 + (hp + 1) * 128, :], yo.ap, c.YMk, yo)


def bcast_rows(c, colT, name, bank0=0, res=None, dg=None):
    if res is None:
        res = c.alloc(name, D)
    if dg is None:
        dg = c.alloc(name + "_dg", D)
    for j in range(16):
        c.ts("dve", dg.ap[:, j * 128:(j + 1) * 128], c.ident.ap, colT[:, j:j + 1], None, ALU.mult, None, dg, [c.ident, c.mT, c.fgT])
    for q in range(4):
        ps = c.bank(bank0 + q)
        c.mm(ps.ap, c.ones128.ap, dg.ap[:, q * 512:(q + 1) * 512], ps, [c.ones128, dg])
        c.copy("act", res.ap[:, q * 512:(q + 1) * 512], ps.ap, res, [ps])
    return res


def stage_outproj(c):
    cfg, T = c.cfg, c.T
    mTv = c.mT.ap.rearrange("p (o r) -> p o r", r=2)
    YM, X1, FT, GT = c.YM.ap(), c.X1.ap(), c.FT.ap(), c.GT.ap()
    g2 = load_colvec(c, T["norm2_g"].ap(), c.Dk["norm2_g"], 16, "g2T")
    scl2 = c.alloc("scl2", 16)
    c.stt("dve", scl2.ap, mTv[:, 64:80, 0], 1.0, g2.ap, ALU.add, ALU.mult, scl2, [c.mT, g2])
    gt1bc = bcast_rows(c, mTv[:, 32:48, 0], "gt1bc")
    wout = c.alloc("wout", 16 * D, BF16)
    wov = wout.ap.rearrange("p (jc n) -> p jc n", jc=16)
    wsrc = T["w_out"].ap().rearrange("(jc p) n -> p jc n", p=128)
    for jc in range(16):
        c.dma("pool", wov[:, jc, :], wsrc[:, jc, :], wout, c.Dk["w_out"])
    rw = c.alloc("routerw", 16 * 32)
    rwv = rw.ap.rearrange("p (kc e) -> p kc e", kc=16)
    c.dma("sp", rwv, T["router_w"].ap().rearrange("(kc p) e -> p kc e", p=128), rw, c.Dk["router_w"])
    rb = c.alloc("routerb", 32)
    c.dma("sp", rb.ap[0:1, :], T["router_b"].ap(), rb, c.Dk["router_b"])
    TBK = min(512, cfg.nown)
    ymb = [c.alloc(f"ymb{i}", 16 * TBK, BF16) for i in range(2)]
    ftb = [c.alloc(f"ftb{i}", 16 * TBK, BF16) for i in range(2)]
    gtb = [c.alloc(f"gtb{i}", TBK) for i in range(2)]
    xt = [c.alloc(f"ox{i}", D) for i in range(2)]
    xs = c.alloc("oxs", D)
    tmp = c.alloc("otmp", 512)
    junk = c.alloc("ojunk", D, BF16)
    f32 = c.alloc("of32", 16 * 128)
    f32v = f32.ap.rearrange("p (j t) -> p j t", j=16)
    st = c.alloc("ost", 8)
    lg = c.alloc("olg", 32); mx = c.alloc("omx", 8); ex = c.alloc("oex", 32); mk = c.alloc("omk", 32)
    ymsrc = YM.rearrange("(jc p) t -> p jc t", p=128)
    ftdst = FT.rearrange("(jc p) t -> p jc t", p=128)
    for blk in range(cfg.nown // TBK):
        yb, fb, gb = ymb[blk % 2], ftb[blk % 2], gtb[blk % 2]
        ybv = yb.ap.rearrange("p (jc t) -> p jc t", jc=16)
        fbv = fb.ap.rearrange("p (jc t) -> p jc t", jc=16)
        c.dma("sp", ybv, ymsrc[:, :, bass.ts(c.qv * (cfg.nown // TBK) + blk, TBK)], yb, c.YMk)
        for ti in range(TBK // 128):
            tok0 = blk * TBK + ti * 128
            x = xt[ti % 2]
            c.dma("sp", x.ap, T["x_own"].ap()[tok0:tok0 + 128, :], x, c.Dk["x_own"])
            for cb in range(4):
                ps = c.bank(cb)
                for jc in range(16):
                    c.mm(ps.ap, ybv[:, jc, ti * 128:(ti + 1) * 128], wov[:, jc, cb * 512:(cb + 1) * 512], ps, [yb, wout],
                         start=(jc == 0), stop=(jc == 15))
                c.tt("dve", tmp.ap, ps.ap, gt1bc.ap[:, cb * 512:(cb + 1) * 512], ALU.mult, tmp, [ps, gt1bc])
                c.tt("dve", x.ap[:, cb * 512:(cb + 1) * 512], x.ap[:, cb * 512:(cb + 1) * 512], tmp.ap, ALU.add, x, [x, tmp])
            c.dma("act", X1[tok0:tok0 + 128, :], x.ap, c.X1k, x)
            c.act(junk.ap, x.ap, AF.Square, junk, [x], accum=st.ap[:, 0:1], accb=st)
            c.ts("dve", st.ap[:, 0:1], st.ap[:, 0:1], 1.0 / D, 1e-5, ALU.mult, ALU.add, st, [st, junk])
            c.act(st.ap[:, 0:1], st.ap[:, 0:1], AF.Sqrt, st, [st])
            c.op("dve", lambda e: e.reciprocal(out=st.ap[:, 0:1], in_=st.ap[:, 0:1]), [st], [st])
            c.ts("dve", xs.ap, x.ap, st.ap[:, 0:1], None, ALU.mult, None, xs, [x, st])
            for q in range(4):
                ps = c.bank(4 + q % 2)
                for jj in range(4):
                    j = q * 4 + jj
                    c.tr(ps.ap[:, jj * 128:(jj + 1) * 128], xs.ap[:, j * 128:(j + 1) * 128], c.ident.ap, ps, [xs, c.ident])
                for jj in range(4):
                    j = q * 4 + jj
                    c.act(f32v[:, j, :], ps.ap[:, jj * 128:(jj + 1) * 128], AF.Identity, f32, [ps, scl2, c.mT],
                          bias=mTv[:, 48 + j, 0:1], scale=scl2.ap[:, j:j + 1])
            c.copy("pool", fbv[:, :, ti * 128:(ti + 1) * 128], f32v, fb, [f32])
            ps = c.bank(6)
            for j in range(16):
                c.mm(ps.ap[:, 0:32], f32v[:, j, :], rwv[:, j, :], ps, [f32, rw], start=(j == 0), stop=False)
            c.mm(ps.ap[:, 0:32], c.onesrow.ap[0:1, :], rb.ap[0:1, :], ps, [c.onesrow, rb], start=False, stop=True)
            c.copy("act", lg.ap, ps.ap[:, 0:32], lg, [ps])
            c.op("dve", lambda e: e.max(out=mx.ap, in_=lg.ap), [lg], [mx])
            c.ts("dve", mk.ap, lg.ap, mx.ap[:, 3:4], None, ALU.is_ge, None, mk, [lg, mx])
            c.ts("dve", mx.ap[:, 4:5], mx.ap[:, 0:1], -1.0, None, ALU.mult, None, mx, [mx])
            c.act(ex.ap, lg.ap, AF.Exp, ex, [lg, mx], bias=mx.ap[:, 4:5])
            c.tt("dve", ex.ap, ex.ap, mk.ap, ALU.mult, ex, [ex, mk])
            c.op("dve", lambda e: e.tensor_reduce(out=mx.ap[:, 5:6], in_=ex.ap, axis=AX.X, op=ALU.add), [ex], [mx])
            c.op("dve", lambda e: e.reciprocal(out=mx.ap[:, 5:6], in_=mx.ap[:, 5:6]), [mx], [mx])
            c.ts("dve", ex.ap, ex.ap, mx.ap[:, 5:6], None, ALU.mult, None, ex, [ex, mx])
            ps2 = c.bank(7)
            c.tr(ps2.ap[0:32, 0:128], ex.ap, c.ident.ap, ps2, [ex, c.ident])
            c.copy("act", gb.ap[0:32, ti * 128:(ti + 1) * 128], ps2.ap[0:32, 0:128], gb, [ps2])
        c.dma("act", ftdst[:, :, blk * TBK:(blk + 1) * TBK], fbv, c.FTk, fb)
        c.dma("act", GT[:, blk * TBK:(blk + 1) * TBK], gb.ap[0:32, :], c.GTk, gb)
    c.stage_end()


def stage_moe(c):
    cfg, T = c.cfg, c.T
    mTv = c.mT.ap.rearrange("p (o r) -> p o r", r=2)
    X1, FT, GT = c.X1.ap(), c.FT.ap(), c.GT.ap()
    NJ = cfg.dexp // 128
    TB = min(1024, cfg.nown)
    NH = (TB + 511) // 512
    HW = TB // NH
    nrow_b = NEXP * 2 * NJ
    bgu = c.alloc("bguT", nrow_b)
    bdn = c.alloc("bdn", D)
    gt2bc = c.alloc("gt2bc", D)
    fgbc = c.alloc("fgbc", D)
    base = c.top
    bsrc = T["exp_bgu"].ap()
    for r0 in range(0, nrow_b, 128):
        n = min(128, nrow_b - r0)
        c.top = base
        tmpc = load_colvec(c, bsrc[r0:r0 + n, :], c.Dk["exp_bgu"], n, f"bgu{r0}")
        c.copy("dve", bgu.ap[:, r0:r0 + n], tmpc.ap, bgu, [tmpc])
        c.p.barrier()
    c.top = base
    c.dma("sp", bdn.ap[0:32, :], T["exp_bdn"].ap(), bdn, c.Dk["exp_bdn"])
    dg = c.alloc("dgscr", D)
    bcast_rows(c, mTv[:, 80:96, 0], "gt2bc", res=gt2bc, dg=dg)
    bcast_rows(c, c.fgT.ap, "fgbc", bank0=4, res=fgbc, dg=dg)
    c.p.barrier()
    c.top = base
    for blk in range(cfg.nown // TB):
        c.p.barrier()
        c.top = base
        t0 = blk * TB
        ft = c.alloc("m_ft", 16 * TB, BF16)
        ftv = ft.ap.rearrange("p (kc t) -> p kc t", kc=16)
        yacc = c.alloc("m_yacc", 16 * TB)
        yav = yacc.ap.rearrange("p (cc t) -> p cc t", cc=16)
        base2 = c.top
        gts = c.alloc("m_gt", TB)
        actT = c.alloc("m_act", NJ * TB, BF16)
        acv = actT.ap.rearrange("p (j t) -> p j t", j=NJ)
        gbc = c.alloc("m_gbc", TB)
        gl = c.alloc("m_gl", HW); ln = c.alloc("m_ln", HW); sg = c.alloc("m_sg", HW)
        wg = [[c.alloc(f"m_wg{k}{i}", 16 * 128, BF16) for i in range(2)] for k in range(2)]
        wd = [c.alloc(f"m_wd{i}", D, BF16) for i in range(2)]
        c.dma("sp", ftv, FT.rearrange("(kc p) t -> p kc t", p=128)[:, :, t0:t0 + TB], ft, c.FTk)
        c.dma("sp", gts.ap[0:32, :], GT[:, t0:t0 + TB], gts, c.GTk)
        for cc in range(16):
            for h in range(NH):
                ps = c.bank((cc * NH + h) % 4)
                c.mm(ps.ap[:, 0:HW], bdn.ap[0:32, cc * 128:(cc + 1) * 128], gts.ap[0:32, h * HW:(h + 1) * HW], ps, [bdn, gts])
                c.copy("act", yav[:, cc, h * HW:(h + 1) * HW], ps.ap[:, 0:HW], yacc, [ps])
        wi = 0
        di = 0
        for e in range(NEXP):
            for h in range(NH):
                ps = c.bank(4 + h)
                c.mm(ps.ap[:, 0:HW], c.ident.ap[0:32, e:e + 1].to_broadcast([32, 128]), gts.ap[0:32, h * HW:(h + 1) * HW], ps,
                     [c.ident, gts])
                c.copy("act", gbc.ap[:, h * HW:(h + 1) * HW], ps.ap[:, 0:HW], gbc, [ps])
            wsrc = T["exp_wgu"].ap()[e].rearrange("(kc p) n -> p kc n", p=128)
            for j in range(NJ):
                wt = []
                for kind in range(2):
                    w = wg[kind][wi % 2]
                    wv = w.ap.rearrange("p (kc n) -> p kc n", kc=16)
                    c.dma("pool", wv, wsrc[:, :, kind * cfg.dexp + j * 128:kind * cfg.dexp + (j + 1) * 128], w, c.Dk["exp_wgu"])
                    wt.append((w, wv))
                wi += 1
                bcol = e * 2 * NJ + j
                for h in range(NH):
                    hs = slice(h * HW, (h + 1) * HW)
                    pg, pl = c.bank(2 * h), c.bank(2 * h + 1)
                    for kind, ps in ((0, pg), (1, pl)):
                        w, wv = wt[kind]
                        for kc in range(16):
                            c.mm(ps.ap[:, 0:HW], wv[:, kc, :], ftv[:, kc, hs], ps, [w, ft], start=(kc == 0), stop=(kc == 15))
                    c.ts("dve", gl.ap, pg.ap[:, 0:HW], bgu.ap[:, bcol:bcol + 1], 7.0, ALU.add, ALU.min, gl, [pg, bgu])
                    c.act(sg.ap, gl.ap, AF.Sigmoid, sg, [gl], scale=1.702)
                    c.ts("dve", ln.ap, pl.ap[:, 0:HW], bgu.ap[:, bcol + NJ:bcol + NJ + 1], 7.0, ALU.add, ALU.min, ln, [pl, bgu])
                    c.ts("dve", ln.ap, ln.ap, -7.0, 1.0, ALU.max, ALU.add, ln, [ln])
                    c.tt("dve", gl.ap, gl.ap, sg.ap, ALU.mult, gl, [gl, sg])
                    c.tt("dve", gl.ap, gl.ap, ln.ap, ALU.mult, gl, [gl, ln])
                    c.tt("dve", acv[:, j, hs], gl.ap, gbc.ap[:, hs], ALU.mult, actT, [gl, gbc])
            dsrc = T["exp_wdn"].ap()[e].rearrange("(jc p) n -> p jc n", p=128)
            for cc in range(16):
                w = wd[di % 2]
                di += 1
                wv = w.ap.rearrange("p (jc n) -> p jc n", jc=16)
                c.dma("pool", wv[:, 0:NJ, :], dsrc[:, :, cc * 128:(cc + 1) * 128], w, c.Dk["exp_wdn"])
                for h in range(NH):
                    ps = c.bank(4 + (cc * NH + h) % 4)
                    for j in range(NJ):
                        c.mm(ps.ap[:, 0:HW], wv[:, j, :], acv[:, j, h * HW:(h + 1) * HW], ps, [w, actT],
                             start=(j == 0), stop=(j == NJ - 1))
                    c.tt("dve", yav[:, cc, h * HW:(h + 1) * HW], yav[:, cc, h * HW:(h + 1) * HW], ps.ap[:, 0:HW], ALU.add, yacc, [yacc, ps])
        c.p.barrier()
        c.top = base2
        x1t = [c.alloc(f"fx{i}", D) for i in range(2)]
        tmp = c.alloc("ftmp", 512)
        junk = c.alloc("fjunk", D, BF16)
        st = c.alloc("fst", 8)
        for ti in range(TB // 128):
            tok0 = t0 + ti * 128
            x = x1t[ti % 2]
            c.dma("sp", x.ap, X1[tok0:tok0 + 128, :], x, c.X1k)
            for q in range(4):
                ps = c.bank(q)
                for jj in range(4):
                    cc = q * 4 + jj
                    c.tr(ps.ap[:, jj * 128:(jj + 1) * 128], yav[:, cc, ti * 128:(ti + 1) * 128], c.ident.ap, ps, [yacc, c.ident])
                c.tt("dve", tmp.ap, ps.ap, gt2bc.ap[:, q * 512:(q + 1) * 512], ALU.mult, tmp, [ps, gt2bc])
                c.tt("dve", x.ap[:, q * 512:(q + 1) * 512], x.ap[:, q * 512:(q + 1) * 512], tmp.ap, ALU.add, x, [x, tmp])
            c.act(junk.ap, x.ap, AF.Square, junk, [x], accum=st.ap[:, 0:1], accb=st)
            c.ts("dve", st.ap[:, 0:1], st.ap[:, 0:1], 1.0 / D, 1e-5, ALU.mult, ALU.add, st, [st, junk])
            c.act(st.ap[:, 0:1], st.ap[:, 0:1], AF.Sqrt, st, [st])
            c.op("dve", lambda e: e.reciprocal(out=st.ap[:, 0:1], in_=st.ap[:, 0:1]), [st], [st])
            c.stt("dve", x.ap, x.ap, st.ap[:, 0:1], fgbc.ap, ALU.mult, ALU.mult, x, [x, st, fgbc])
            c.dma("act", c.out.ap()[tok0:tok0 + 128, :], x.ap, c.Dk["out"], x)
    c.stage_end()


def make_inputs(cfg, b, inp, q=0):
    inp = {k: np.asarray(v) for k, v in inp.items()}
    f = lambda a: np.ascontiguousarray(a, dtype=np.float32)
    cc = np.stack([inp["c"][b], inp["c_ctx"]]).reshape(32, 128)
    return {
        "x": f(inp["x"][b]), "x_own": f(inp["x"][b, q * cfg.nown:(q + 1) * cfg.nown]), "ctx": f(inp["ctx"][b]), "cc": f(cc),
        "mod_w": f(inp["mod_w"][0]), "mod_b": f(inp["mod_b"][0].reshape(96, 128)),
        "norm1_g": f(inp["norm1_g"][0].reshape(16, 128)), "w_in": f(inp["w_in"][0]),
        "rw_mu": f(np.pad(inp["rw_mu"][0], ((0, 0), (0, 28 * 128 - RWC))).reshape(4 * 28, 128)),
        "s5_are": f(inp["s5_a_re"][0].reshape(64, 128)), "s5_aim": f(inp["s5_a_im"][0].reshape(64, 128)),
        "s5_ldt": f(np.repeat(inp["s5_log_dt"][0].reshape(2, 32, 2, 1), 64, axis=3).reshape(64, 128)),
        "s5_d": f(inp["s5_d"][0].reshape(8, 128)), "s5_glub": f(inp["s5_glu_b"][0].reshape(8, 128)),
        "s5_bre": f(inp["s5_b_re"][0].reshape(-1, 16)), "s5_bim": f(inp["s5_b_im"][0].reshape(-1, 16)),
        "s5_cre": f(inp["s5_c_re"][0].reshape(-1, 64)), "s5_cim": f(inp["s5_c_im"][0].reshape(-1, 64)),
        "s5_gluw": f(inp["s5_glu_w"][0].reshape(1024, 16)),
        "rw_w0": f(inp["rw_w0"][0].reshape(16, 128)), "rw_a0": f(inp["rw_a0"][0].reshape(16, 128)),
        "rw_w2": f(inp["rw_w2"][0].reshape(128, 1024)), "rw_a2": f(inp["rw_a2"][0].reshape(128, 1024)),
        "rw_g2": f(inp["rw_g2"][0]), "rw_kk": f(inp["rw_k_k"][0].reshape(8, 128)), "rw_ka": f(inp["rw_k_a"][0].reshape(8, 128)),
        "rw_rk": f(inp["rw_r_k"][0].reshape(8, 128)), "rw_lnw": f(inp["rw_ln_w"][0].reshape(1, 1024)),
        "rw_lnb": f(inp["rw_ln_b"][0].reshape(1, 1024)),
        "w_out": f(inp["w_out"][0]), "norm2_g": f(inp["norm2_g"][0].reshape(16, 128)),
        "router_w": f(inp["router_w"][0]), "router_b": f(inp["router_b"][0].reshape(1, 32)),
        "final_g": f(inp["final_g"].reshape(16, 128)),
        "exp_wgu": f(inp["exp_w_gu"][0]), "exp_bgu": f(inp["exp_b_gu"][0].reshape(-1, 128)),
        "exp_wdn": f(inp["exp_w_dn"][0]), "exp_bdn": f(inp["exp_b_dn"][0]),
    }


_NC_CACHE = {}


def kernel(**inp):
    cfg = Cfg()
    if "nc" not in _NC_CACHE:
        _NC_CACHE["nc"] = build(cfg)
    nc = _NC_CACHE["nc"]
    nq = cfg.nq
    shared = {}
    in_maps = []
    for i in range(2 * nq):
        b, q = i // nq, i % nq
        m = make_inputs(cfg, b, inp, q)
        for k, v in m.items():
            if k in shared and shared[k].shape == v.shape and (shared[k] is v or np.shares_memory(shared[k], v)):
                m[k] = shared[k]
            else:
                shared.setdefault(k, v)
        in_maps.append(m)
    res = run_bass_kernel_spmd(nc, in_maps, core_ids=list(range(2 * nq)))
    out = np.empty((2, cfg.seq, D), np.float32)
    for i in range(2 * nq):
        b, q = i // nq, i % nq
        out[b, q * cfg.nown:(q + 1) * cfg.nown] = res.results[i]["out"]
    return out
```

```python
import math
from contextlib import ExitStack

import numpy as np
import concourse.bass as bass
import concourse.mybir as mybir
from concourse.bass_utils import run_bass_kernel_spmd

F32 = mybir.dt.float32
BF16 = mybir.dt.bfloat16
ALU = mybir.AluOpType
AF = mybir.ActivationFunctionType
AX = mybir.AxisListType

D = 2048
KC = D // 128
S5W = 1024
RWW = 1024
RWC = 3 * RWW + 128 + 128 + 160
INC = S5W + RWC
NEXP = 32
CS = 64
NSUB = 128 // CS
ARENA = 52800


class Cfg:
    def __init__(self, nrow=64, ctx=256, dexp=2048, stages=99, nq=4):
        self.nrow = nrow
        self.seq = nrow * 64
        self.ctx = ctx
        self.ntok = self.seq + ctx
        self.npos = self.seq + 2 * ctx
        self.dexp = dexp
        self.stages = stages
        self.nq = nq
        self.nown = self.seq // nq


ENGS = ("sp", "act", "dve", "pool", "pe")
NDSEM = 6
SELF_SYNC = {"pool", "dve", "act"}


class Op:
    __slots__ = ("eng", "fn", "deps", "sig", "sem", "val", "dma", "prev_dma")

    def __init__(self, eng, fn, dma):
        self.eng, self.fn, self.dma = eng, fn, dma
        self.deps = []
        self.sig = dma
        self.sem = None
        self.val = 0
        self.prev_dma = None


class Prog:
    def __init__(self, nc, es):
        self.nc = nc
        self.ops = {e: [] for e in ENGS}
        self.tab = {}
        self.csem = {e: es.enter_context(nc.semaphore("c_" + e)) for e in ENGS}
        self.dsem = {e: [es.enter_context(nc.semaphore(f"d_{e}{i}")) for i in range(NDSEM)]
                     for e in ("sp", "act", "pool")}
        self.dcnt = {e: [0] * NDSEM for e in self.dsem}
        self.dlast = {e: [None] * NDSEM for e in self.dsem}
        self.drr = {e: 0 for e in self.dsem}

    def _entries(self, key):
        name, sub = key
        t = self.tab.setdefault(name, {})
        if sub is None:
            return list(t.values())
        out = []
        if sub in t:
            out.append(t[sub])
        if None in t:
            out.append(t[None])
        return out

    def add(self, eng, fn, reads=(), writes=(), dma=False):
        op = Op(eng, fn, dma)
        deps = []
        for k in reads:
            for ent in self._entries(k):
                if ent[0] is not None:
                    deps.append(ent[0])
        for k in writes:
            for ent in self._entries(k):
                if ent[0] is not None:
                    deps.append(ent[0])
                deps.extend(ent[1].values())
        if dma:
            i = self.drr[eng]
            self.drr[eng] = (i + 1) % NDSEM
            op.sem = self.dsem[eng][i]
            self.dcnt[eng][i] += 1
            op.val = 16 * self.dcnt[eng][i]
            op.prev_dma = self.dlast[eng][i]
            self.dlast[eng][i] = op
            if op.prev_dma is not None:
                deps.append(op.prev_dma)
        seen = set()
        for d in deps:
            if d is op or id(d) in seen:
                continue
            seen.add(id(d))
            if (not d.dma) and d.eng == eng and eng not in SELF_SYNC:
                continue
            d.sig = True
            op.deps.append(d)
        for k in writes:
            name, sub = k
            t = self.tab.setdefault(name, {})
            if sub is None:
                t.clear()
            t[sub] = [op, {}]
        for k in reads:
            name, sub = k
            t = self.tab.setdefault(name, {})
            if sub not in t:
                t[sub] = [None, {}]
            t[sub][1][eng if not dma else (eng, id(op))] = op
        self.ops[eng].append(op)
        return op

    def barrier(self):
        lasts = []
        for e in ENGS:
            for o in reversed(self.ops[e]):
                if o.fn is not None and not o.dma:
                    lasts.append(o)
                    break
        for e in self.dsem:
            for d in self.dlast[e]:
                if d is not None:
                    lasts.append(d)
        for e in ENGS:
            op = Op(e, None, False)
            for d in lasts:
                if (not d.dma) and d.eng == e:
                    continue
                d.sig = True
                op.deps.append(d)
            self.ops[e].append(op)
        self.tab = {}

    def emit(self):
        nc = self.nc
        for e in ENGS:
            n = 0
            for op in self.ops[e]:
                if not op.dma and op.sig:
                    n += 1
                    op.sem = self.csem[e]
                    op.val = n
        with nc.Block() as block:
            def run(e, eng):
                seen = {}
                for op in self.ops[e]:
                    need = {}
                    for d in op.deps:
                        k = id(d.sem)
                        if d.val > seen.get(k, 0) and d.val > need.get(k, (None, 0))[1]:
                            need[k] = (d.sem, d.val)
                    for k, (s, v) in need.items():
                        eng.wait_ge(s, v)
                        seen[k] = v
                    if op.fn is not None:
                        ins = op.fn(eng)
                        if op.sig:
                            ins.then_inc(op.sem, 16 if op.dma else 1)

            @block.sync
            def _(eng):
                run("sp", eng)

            @block.scalar
            def _(eng):
                run("act", eng)

            @block.vector
            def _(eng):
                run("dve", eng)

            @block.gpsimd
            def _(eng):
                run("pool", eng)

            @block.tensor
            def _(eng):
                run("pe", eng)


class B:
    __slots__ = ("ap", "key")

    def __init__(self, ap, key):
        self.ap, self.key = ap, key


class Ctx:
    def __init__(self, nc, es, cfg):
        self.nc, self.cfg = nc, cfg
        self.p = Prog(nc, es)
        self.arena = es.enter_context(nc.sbuf_tensor("arena", [128, ARENA], F32))
        self.psum = es.enter_context(nc.psum_tensor("psum", [128, 4096], F32))
        self.top = 0
        self.uid = 0
        self.pers = 0

    def alloc(self, name, n, dt=F32):
        words = n if dt == F32 else (n + 1) // 2
        words = (words + 7) // 8 * 8
        assert self.top + words <= ARENA, (name, self.top, words)
        ap = self.arena[:, self.top:self.top + words]
        if dt != F32:
            ap = ap.bitcast(dt)[:, 0:n]
        else:
            ap = ap[:, 0:n]
        self.top += words
        self.uid += 1
        return B(ap, (f"{name}#{self.uid}", None))

    def bank(self, i, name=None):
        return B(self.psum[:, i * 512:(i + 1) * 512], (f"bank{i}", None))

    def stage_end(self):
        self.p.barrier()
        self.top = self.pers

    def op(self, eng, fn, reads, writes, dma=False):
        return self.p.add(eng, fn, [b.key for b in reads], [b.key for b in writes], dma)

    def dma(self, q, out, in_, outb, inb, **kw):
        return self.op(q, lambda e: e.dma_start(out=out, in_=in_, **kw), [inb], [outb], dma=True)

    def mm(self, out, lhsT, rhs, outb, inbs, start=True, stop=True):
        return self.op("pe", lambda e: e.matmul(out, lhsT, rhs, start=start, stop=stop), inbs,
                       [outb] if start else [outb])

    def tr(self, out, in_, ident, outb, inbs):
        return self.op("pe", lambda e: e.transpose(out, in_, ident), inbs, [outb])

    def act(self, out, in_, func, outb, inbs, bias=None, scale=None, accum=None, accb=None, eng="act"):
        kw = {}
        if bias is not None:
            kw["bias"] = bias
        if scale is not None:
            kw["scale"] = scale
        if accum is not None:
            kw["accum_out"] = accum
        return self.op(eng, lambda e: e.activation(out=out, in_=in_, func=func, **kw), inbs,
                       [outb] + ([accb] if accb is not None else []))

    def ts(self, eng, out, in0, s1, s2, op0, op1, outb, inbs):
        if op1 is None:
            return self.op(eng, lambda e: e.tensor_scalar(out=out, in0=in0, scalar1=s1, scalar2=None, op0=op0),
                           inbs, [outb])
        return self.op(eng, lambda e: e.tensor_scalar(out=out, in0=in0, scalar1=s1, scalar2=s2, op0=op0, op1=op1),
                       inbs, [outb])

    def tt(self, eng, out, in0, in1, op, outb, inbs):
        return self.op(eng, lambda e: e.tensor_tensor(out=out, in0=in0, in1=in1, op=op), inbs, [outb])

    def stt(self, eng, out, in0, scalar, in1, op0, op1, outb, inbs):
        return self.op(eng, lambda e: e.scalar_tensor_tensor(out=out, in0=in0, scalar=scalar, in1=in1,
                                                             op0=op0, op1=op1), inbs, [outb])

    def copy(self, eng, out, in_, outb, inbs):
        if eng == "act":
            return self.op(eng, lambda e: e.copy(out=out, in_=in_), inbs, [outb])
        return self.op(eng, lambda e: e.tensor_copy(out=out, in_=in_), inbs, [outb])

    def memset(self, eng, ap, val, outb):
        return self.op(eng, lambda e: e.memset(ap, val), [], [outb])


def dram_in(nc, name, shape):
    return nc.dram_tensor(name, list(shape), F32, kind="ExternalInput")


def build(cfg):
    nc = bass.Bass("TRN2", target_bir_lowering=False)
    es = ExitStack()
    T = {}
    shapes = input_shapes(cfg)
    for k, s in shapes.items():
        T[k] = dram_in(nc, k, s)
    out = nc.dram_tensor("out", [cfg.nown, D], F32, kind="ExternalOutput")
    with es:
        c = Ctx(nc, es, cfg)
        c.T = T
        c.out = out
        c.Dk = {k: B(None, ("dram_" + k, None)) for k in T}
        c.Dk["out"] = B(None, ("dram_out", None))
        c.P = nc.dram_tensor("P_s", [INC, cfg.npos], F32, kind="Internal")
        c.Pk = B(None, ("P_s", None))
        stage_consts(c)
        stage_adaln(c)
        nch = (RWC + 127) // 128
        c.ZS = nc.dram_tensor("ZS_s", [nch * 128, cfg.npos], F32, kind="Internal")
        c.ZSk = B(None, ("ZS_s", None))
        c.YM = nc.dram_tensor("YM_s", [D, cfg.seq], BF16, kind="Internal")
        c.YMk = B(None, ("YM_s", None))
        if cfg.stages >= 1:
            stage_inproj(c)
        if cfg.stages >= 2:
            stage_shift(c)
        if cfg.stages >= 3:
            stage_s5(c)
        c.LO = nc.dram_tensor("LO_s", [5, 8, 128, cfg.npos], F32, kind="Internal")
        c.LOk = B(None, ("LO_s", None))
        c.RW = nc.dram_tensor("RW_s", [2, 8, 128, 6, cfg.npos], F32, kind="Internal")
        c.RWk = B(None, ("RW_s", None))
        c.YD = nc.dram_tensor("YD_s", [2, cfg.seq, 1024], F32, kind="Internal")
        c.YDk = B(None, ("YD_s", None))
        if cfg.stages >= 4:
            stage_rw_lora(c)
        if cfg.stages >= 5:
            stage_rw(c)
        c.qv = nc.partition_id() % cfg.nq
        c.X1 = nc.dram_tensor("X1_s", [cfg.nown, D], F32, kind="Internal")
        c.X1k = B(None, ("X1_s", None))
        c.FT = nc.dram_tensor("FT_s", [D, cfg.nown], BF16, kind="Internal")
        c.FTk = B(None, ("FT_s", None))
        c.GT = nc.dram_tensor("GT_s", [32, cfg.nown], F32, kind="Internal")
        c.GTk = B(None, ("GT_s", None))
        if cfg.stages >= 6:
            stage_outproj(c)
        if cfg.stages >= 7:
            stage_moe(c)
        c.p.barrier()
        c.p.emit()
    return nc


def input_shapes(cfg):
    return {
        "x": (cfg.seq, D), "x_own": (cfg.nown, D), "ctx": (cfg.ctx, D), "cc": (32, 128),
        "mod_w": (D, 6 * D), "mod_b": (96, 128), "norm1_g": (16, 128), "w_in": (D, INC),
        "rw_mu": (4 * 28, 128),
        "s5_are": (64, 128), "s5_aim": (64, 128), "s5_ldt": (64, 128), "s5_d": (8, 128), "s5_glub": (8, 128),
        "s5_bre": (2 * 64 * 64, 16), "s5_bim": (2 * 64 * 64, 16), "s5_cre": (2 * 64 * 16, 64), "s5_cim": (2 * 64 * 16, 64),
        "s5_gluw": (1024, 16),
        "rw_w0": (16, 128), "rw_a0": (16, 128), "rw_w2": (128, 1024), "rw_a2": (128, 1024), "rw_g2": (160, 1024),
        "rw_kk": (8, 128), "rw_ka": (8, 128), "rw_rk": (8, 128), "rw_lnw": (1, 1024), "rw_lnb": (1, 1024),
        "w_out": (D, D), "norm2_g": (16, 128), "router_w": (D, 32), "router_b": (1, 32), "final_g": (16, 128),
        "exp_wgu": (NEXP, D, 2 * cfg.dexp), "exp_bgu": (NEXP * 2 * cfg.dexp // 128, 128),
        "exp_wdn": (NEXP, cfg.dexp, D), "exp_bdn": (NEXP, D),
    }


def stage_consts(c):
    nc = c.nc
    ident = c.alloc("ident", 128)
    c.ident = ident
    c.memset("pool", ident.ap, 1.0, ident)
    c.op("pool", lambda e: e.affine_select(out=ident.ap, in_=ident.ap, pattern=[[-1, 128]],
                                           compare_op=ALU.is_equal, fill=0.0, base=0, channel_multiplier=1),
         [ident], [ident])
    onesrow = c.alloc("onesrow", 128)
    c.memset("pool", onesrow.ap, 1.0, onesrow)
    c.onesrow = onesrow
    ones128 = c.alloc("ones128", 128)
    c.memset("pool", ones128.ap, 1.0, ones128)
    c.ones128 = ones128
    c.pers = c.top


def load_colvec(c, src_ap, srcb, nrows, name, bank=7):
    tmp = c.alloc(name + "_r", 128)
    res = c.alloc(name, nrows)
    c.dma("sp", tmp.ap[0:nrows, :], src_ap, tmp, srcb)
    pb = c.bank(bank)
    c.tr(pb.ap[:, 0:nrows], tmp.ap[0:nrows, :], c.ident.ap[0:nrows, 0:nrows], pb, [tmp, c.ident])
    c.copy("dve", res.ap, pb.ap[:, 0:nrows], res, [pb])
    return res


def stage_adaln(c):
    T = c.T
    mT = c.alloc("mT", 96 * 2)
    g1 = load_colvec(c, T["norm1_g"].ap(), c.Dk["norm1_g"], 16, "g1T")
    c.mT, c.g1 = mT, g1
    scl1 = c.alloc("scl1", 32)
    c.scl1 = scl1
    c.fgT = load_colvec(c, T["final_g"].ap(), c.Dk["final_g"], 16, "fgT")
    c.pers = c.top
    bT = load_colvec(c, T["mod_b"].ap(), c.Dk["mod_b"], 96, "modbT")
    craw = c.alloc("craw", 128)
    c.dma("sp", craw.ap[0:32, :], T["cc"].ap(), craw, c.Dk["cc"])
    c.act(craw.ap[0:32, :], craw.ap[0:32, :], AF.Silu, craw, [craw])
    pb = c.bank(6)
    c.tr(pb.ap[:, 0:32], craw.ap[0:32, :], c.ident.ap[0:32, 0:32], pb, [craw, c.ident])
    sT = c.alloc("siluT", 32)
    c.copy("dve", sT.ap, pb.ap[:, 0:32], sT, [pb])
    sTv = sT.ap.rearrange("p (r j) -> p j r", r=2)
    mTv = mT.ap.rearrange("p (o r) -> p o r", r=2)
    wbuf = [c.alloc(f"modw{i}", 16 * 512) for i in range(2)]
    mw = T["mod_w"].ap().rearrange("(kc p) n -> p kc n", p=128)
    for blk in range(24):
        wb = wbuf[blk % 2]
        wv = wb.ap.rearrange("p (kc n) -> p kc n", kc=16)
        c.dma("sp", wv, mw[:, :, blk * 512:(blk + 1) * 512], wb, c.Dk["mod_w"])
        ps = c.bank(blk % 2)
        for o in range(4):
            for kc in range(16):
                c.mm(ps.ap[:, o * 2:o * 2 + 2], wv[:, kc, o * 128:(o + 1) * 128], sTv[:, kc, :], ps, [wb, sT],
                     start=(kc == 0), stop=(kc == 15))
        o0 = blk * 4
        c.tt("dve", mTv[:, o0:o0 + 4, :], ps.ap[:, 0:8].rearrange("p (o r) -> p o r", r=2),
             bT.ap[:, o0:o0 + 4].unsqueeze(2).to_broadcast([128, 4, 2]), ALU.add, mT, [ps, bT])
    for r in range(2):
        c.stt("dve", scl1.ap[:, r * 16:(r + 1) * 16], mTv[:, 16:32, r], 1.0, g1.ap, ALU.add, ALU.mult,
              scl1, [mT, g1])
    c.stage_end()


def stage_inproj(c):
    cfg, T = c.cfg, c.T
    mTv = c.mT.ap.rearrange("p (o r) -> p o r", r=2)
    ntile = cfg.ntok // 128
    nct = cfg.ctx // 128
    xt = [c.alloc(f"xt{i}", D) for i in range(2)]
    junk = c.alloc("junk", D, BF16)
    ssq = c.alloc("ssq", 8)
    xnT = c.alloc("xnT", 16 * 512, BF16)
    xnv = xnT.ap.rearrange("p (kc n) -> p kc n", kc=16)
    wgrp = [c.alloc(f"wgrp{i}", 16 * 512, BF16) for i in range(2)]
    pst = [c.alloc(f"pst{i}", 512) for i in range(2)]
    win = T["w_in"].ap().rearrange("(kc p) n -> p kc n", p=128)
    P = c.P.ap()
    nblk = (ntile + 3) // 4
    gi = 0
    ci = 0
    for blk in range(nblk):
        t0 = blk * 4
        nt = min(4, ntile - t0)
        for ti in range(nt):
            g = t0 + ti
            isctx = g < nct
            r = 1 if isctx else 0
            src = T["ctx"].ap()[g * 128:(g + 1) * 128, :] if isctx else \
                T["x"].ap()[(g - nct) * 128:(g - nct + 1) * 128, :]
            xb = xt[gi % 2]
            gi += 1
            c.dma("sp", xb.ap, src, xb, c.Dk["ctx" if isctx else "x"])
            sq = ssq.ap[:, (g % 8):(g % 8) + 1]
            c.act(junk.ap, xb.ap, AF.Square, junk, [xb], accum=sq, accb=ssq)
            c.op("dve", lambda e, sq=sq: e.tensor_scalar(out=sq, in0=sq, scalar1=1.0 / D, scalar2=1e-5,
                                                         op0=ALU.mult, op1=ALU.add), [junk, ssq], [ssq])
            c.act(sq, sq, AF.Sqrt, ssq, [ssq])
            c.op("dve", lambda e, sq=sq: e.reciprocal(out=sq, in_=sq), [ssq], [ssq])
            c.op("dve", lambda e, sq=sq, xb=xb: e.tensor_scalar(out=xb.ap, in0=xb.ap, scalar1=sq, scalar2=None,
                                                                op0=ALU.mult), [ssq, xb], [xb])
            for q in range(4):
                ps = c.bank(q + 4 * (g % 2))
                for jj in range(4):
                    j = q * 4 + jj
                    c.tr(ps.ap[:, jj * 128:(jj + 1) * 128], xb.ap[:, j * 128:(j + 1) * 128], c.ident.ap,
                         ps, [xb, c.ident])
                for jj in range(4):
                    j = q * 4 + jj
                    c.act(xnv[:, j, ti * 128:(ti + 1) * 128], ps.ap[:, jj * 128:(jj + 1) * 128], AF.Identity,
                          xnT, [ps, c.scl1, c.mT], bias=mTv[:, j, r:r + 1], scale=c.scl1.ap[:, r * 16 + j:r * 16 + j + 1])
        n = nt * 128
        p0 = t0 * 128
        for cc in range((INC + 127) // 128):
            ncol = min(128, INC - cc * 128)
            po = pst[ci % 2]
            ci += 1
            if cc % 4 == 0:
                wg_ = wgrp[(cc // 4 + blk) % 2]
                wgv = wg_.ap.rearrange("p (kc n) -> p kc n", kc=16)
                gcols = min(512, INC - cc * 128)
                c.dma("pool", wgv[:, :, 0:gcols], win[:, :, cc * 128:cc * 128 + gcols], wg_, c.Dk["w_in"])
            cl = (cc % 4) * 128
            ps = c.bank(ci % 2)
            for kc in range(16):
                c.mm(ps.ap[0:ncol, 0:n], wgv[:, kc, cl:cl + ncol], xnv[:, kc, 0:n], ps, [wg_, xnT],
                     start=(kc == 0), stop=(kc == 15))
            c.copy("dve", po.ap[0:ncol, 0:n], ps.ap[0:ncol, 0:n], po, [ps])
            segs = []
            a, b_ = p0, p0 + n
            if a < cfg.ctx:
                e = min(b_, cfg.ctx)
                segs.append((a, e, a))
                segs.append((a, e, cfg.ctx + cfg.seq + a))
                a = e
            if a < b_:
                segs.append((a, b_, a))
            for (sa, sb, dpos) in segs:
                c.dma("pool", P[cc * 128:cc * 128 + ncol, dpos:dpos + (sb - sa)],
                      po.ap[0:ncol, sa - p0:sb - p0], c.Pk, po)
    c.stage_end()


def regions(cfg):
    return [(0, cfg.ctx, 1, cfg.ctx), (cfg.ctx, cfg.seq, cfg.nrow, 64), (cfg.ctx + cfg.seq, cfg.ctx, 1, cfg.ctx)]


def stage_shift(c):
    cfg, T = c.cfg, c.T
    nch = (RWC + 127) // 128
    muT = load_colvec(c, T["rw_mu"].ap(), c.Dk["rw_mu"], 4 * nch, "muT")
    P, ZS = c.P.ap(), c.ZS.ap()
    zb = [c.alloc(f"z{i}", cfg.npos) for i in range(2)]
    ob = [c.alloc(f"zo{i}", cfg.npos) for i in range(2)]
    tmp = c.alloc("ztmp", cfg.npos)
    for cc in range(nch):
        ncol = min(128, RWC - cc * 128)
        z, o = zb[cc % 2], ob[cc % 2]
        c.dma("sp", z.ap[0:ncol, :], P[S5W + cc * 128:S5W + cc * 128 + ncol, :], z, c.Pk)
        c.copy("act", o.ap[0:ncol, :], z.ap[0:ncol, :], o, [z])
        for (a, ln, rows, w) in regions(cfg):
            zv = z.ap[0:ncol, a:a + ln].rearrange("p (r w) -> p r w", w=w)
            ov = o.ap[0:ncol, a:a + ln].rearrange("p (r w) -> p r w", w=w)
            tv = tmp.ap[0:ncol, a:a + ln].rearrange("p (r w) -> p r w", w=w)
            pairs = [(0, (slice(None), slice(1, w)), (slice(None), slice(0, w - 1))),
                     (1, (slice(None), slice(0, w - 1)), (slice(None), slice(1, w)))]
            if rows > 1:
                pairs += [(2, (slice(1, rows), slice(None)), (slice(0, rows - 1), slice(None))),
                          (3, (slice(0, rows - 1), slice(None)), (slice(1, rows), slice(None)))]
            for (j, dst, src) in pairs:
                mu = muT.ap[0:ncol, j * nch + cc:j * nch + cc + 1]
                c.tt("dve", tv[:, dst[0], dst[1]], zv[:, src[0], src[1]], zv[:, dst[0], dst[1]], ALU.subtract,
                     tmp, [z])
                c.stt("dve", ov[:, dst[0], dst[1]], tv[:, dst[0], dst[1]], mu, ov[:, dst[0], dst[1]],
                      ALU.mult, ALU.add, o, [tmp, muT, o])
        c.dma("pool", ZS[cc * 128:cc * 128 + ncol, :], o.ap[0:ncol, :], c.ZSk, o)
    c.stage_end()


def stage_s5(c):
    cfg, T = c.cfg, c.T
    L = cfg.ntok
    P = c.P.ap()
    CH = next(x for x in (1088, 512, 384, 256, 128) if L % x == 0)
    if getattr(cfg, "s5_ch", None):
        CH = cfg.s5_ch
    NCH = L // CH
    NLV = max(1, int(math.ceil(math.log2(CH))))
    are = load_colvec(c, T["s5_are"].ap(), c.Dk["s5_are"], 64, "are")
    aim = load_colvec(c, T["s5_aim"].ap(), c.Dk["s5_aim"], 64, "aim")
    ldt = load_colvec(c, T["s5_ldt"].ap(), c.Dk["s5_ldt"], 64, "ldt")
    dT = load_colvec(c, T["s5_d"].ap(), c.Dk["s5_d"], 8, "s5dT")
    gbT = load_colvec(c, T["s5_glub"].ap(), c.Dk["s5_glub"], 8, "s5gbT")
    xr = c.alloc("xr", 64); xi = c.alloc("xi", 64); rho = c.alloc("rho", 64)
    t1 = c.alloc("t1", 64); t2 = c.alloc("t2", 64); tq = c.alloc("lb_t", 64)
    ur = c.alloc("u_re", (NLV + 1) * 64); ui = c.alloc("u_im", (NLV + 1) * 64); un = c.alloc("u_nim", (NLV + 1) * 64)
    wr = c.alloc("w_re", 64); wi = c.alloc("w_im", 64); wn = c.alloc("w_nim", 64)
    lbr = c.alloc("lb_re", 64); lbi = c.alloc("lb_im", 64)
    cr = c.alloc("coef_re", 64); ci = c.alloc("coef_im", 64); nci = c.alloc("coef_nim", 64)
    c.act(ldt.ap, ldt.ap, AF.Exp, ldt, [ldt])
    c.tt("dve", xr.ap, are.ap, ldt.ap, ALU.mult, xr, [are, ldt])
    c.tt("dve", xi.ap, aim.ap, ldt.ap, ALU.mult, xi, [aim, ldt])
    c.act(rho.ap, xr.ap, AF.Exp, rho, [xr])
    c.ts("dve", t1.ap, xi.ap, 1.0 / 16, None, ALU.mult, None, t1, [xi])
    c.ts("dve", t2.ap, xi.ap, 1.0 / 16, math.pi / 2, ALU.mult, ALU.add, t2, [xi])
    br_ = c.alloc("lb_r", 64); bi_ = c.alloc("lb_i", 64)
    c.act(bi_.ap, t1.ap, AF.Sin, bi_, [t1])
    c.act(br_.ap, t2.ap, AF.Sin, br_, [t2])

    def csq(dr_, di_, drb, dib, sr_, si_, srb, sib):
        c.tt("dve", t1.ap, sr_, sr_, ALU.mult, t1, [srb])
        c.tt("dve", t2.ap, si_, si_, ALU.mult, t2, [sib])
        c.tt("dve", tq.ap, sr_, si_, ALU.mult, tq, [srb, sib])
        c.tt("dve", dr_, t1.ap, t2.ap, ALU.subtract, drb, [t1, t2])
        c.ts("dve", di_, tq.ap, 2.0, None, ALU.mult, None, dib, [tq])
    for q in range(4):
        if q == 3:
            csq(ur.ap[:, 0:64], ui.ap[:, 0:64], ur, ui, br_.ap, bi_.ap, br_, bi_)
        else:
            csq(br_.ap, bi_.ap, br_, bi_, br_.ap, bi_.ap, br_, bi_)
    for k in range(1, NLV + 1):
        csq(ur.ap[:, k * 64:(k + 1) * 64], ui.ap[:, k * 64:(k + 1) * 64], ur, ui,
            ur.ap[:, (k - 1) * 64:k * 64], ui.ap[:, (k - 1) * 64:k * 64], ur, ui)
    c.ts("dve", un.ap, ui.ap, -1.0, None, ALU.mult, None, un, [ui])
    bits = [k for k in range(NLV + 1) if (CH >> k) & 1]
    c.copy("dve", wr.ap, ur.ap[:, bits[0] * 64:(bits[0] + 1) * 64], wr, [ur])
    c.copy("dve", wi.ap, ui.ap[:, bits[0] * 64:(bits[0] + 1) * 64], wi, [ui])
    for k in bits[1:]:
        a_, b_ = ur.ap[:, k * 64:(k + 1) * 64], ui.ap[:, k * 64:(k + 1) * 64]
        c.tt("dve", t1.ap, wr.ap, a_, ALU.mult, t1, [wr, ur])
        c.tt("dve", t2.ap, wi.ap, b_, ALU.mult, t2, [wi, ui])
        c.tt("dve", tq.ap, wr.ap, b_, ALU.mult, tq, [wr, ui])
        c.tt("dve", wi.ap, wi.ap, a_, ALU.mult, wi, [wi, ur])
        c.tt("dve", wi.ap, wi.ap, tq.ap, ALU.add, wi, [wi, tq])
        c.tt("dve", wr.ap, t1.ap, t2.ap, ALU.subtract, wr, [t1, t2])
    c.ts("dve", wn.ap, wi.ap, -1.0, None, ALU.mult, None, wn, [wi])
    c.tt("dve", lbr.ap, rho.ap, ur.ap[:, 0:64], ALU.mult, lbr, [rho, ur])
    c.tt("dve", lbi.ap, rho.ap, ui.ap[:, 0:64], ALU.mult, lbi, [rho, ui])
    c.ts("dve", t1.ap, lbr.ap, -1.0, None, ALU.add, None, t1, [lbr])
    c.tt("dve", t2.ap, are.ap, are.ap, ALU.mult, t2, [are])
    c.tt("dve", xr.ap, aim.ap, aim.ap, ALU.mult, xr, [aim])
    c.tt("dve", t2.ap, t2.ap, xr.ap, ALU.add, t2, [t2, xr])
    c.op("dve", lambda e: e.reciprocal(out=t2.ap, in_=t2.ap), [t2], [t2])
    c.tt("dve", cr.ap, t1.ap, are.ap, ALU.mult, cr, [t1, are])
    c.tt("dve", xr.ap, lbi.ap, aim.ap, ALU.mult, xr, [lbi, aim])
    c.tt("dve", cr.ap, cr.ap, xr.ap, ALU.add, cr, [cr, xr])
    c.tt("dve", cr.ap, cr.ap, t2.ap, ALU.mult, cr, [cr, t2])
    c.tt("dve", ci.ap, lbi.ap, are.ap, ALU.mult, ci, [lbi, are])
    c.tt("dve", xr.ap, t1.ap, aim.ap, ALU.mult, xr, [t1, aim])
    c.tt("dve", ci.ap, ci.ap, xr.ap, ALU.subtract, ci, [ci, xr])
    c.tt("dve", ci.ap, ci.ap, t2.ap, ALU.mult, ci, [ci, t2])
    c.ts("dve", nci.ap, ci.ap, -1.0, None, ALU.mult, None, nci, [ci])
    u = c.alloc("s5u", cfg.npos)
    yacc = c.alloc("s5y", cfg.seq)
    bb = [c.alloc(f"s5b{k}", L) for k in range(2)]
    mb = [c.alloc(f"s5m{k}", L) for k in range(2)]
    tmpL = c.alloc("s5tmp", L)
    Ec = c.alloc("s5Ec", CH); Es = c.alloc("s5Es", CH)
    rt = c.alloc("s5rt", CH)
    onesC = c.alloc("s5ones", CH)
    c.memset("pool", onesC.ap, 1.0, onesC)
    car = [c.alloc(f"s5car{i}", 4) for i in range(2)]
    braw = [c.alloc(f"braw{i}", 16) for i in range(2)]
    bbar = [c.alloc(f"bbar{i}", 16) for i in range(2)]
    xpad = [c.alloc(f"xpad{i}", 128) for i in range(2)]
    lB = [c.alloc(f"lB{i}", 128) for i in range(2)]
    craw = [c.alloc(f"craw{i}", 128) for i in range(2)]
    lC = [c.alloc(f"lC{i}", 128) for i in range(2)]
    lG = c.alloc("lG", 128)
    gt = c.alloc("s5g", cfg.seq)
    yo = c.alloc("s5o", cfg.seq, BF16)
    nblk_l = (L + 511) // 512
    nblk_s = (cfg.seq + 511) // 512
    bre, bim = T["s5_bre"].ap(), T["s5_bim"].ap()
    cre, cim = T["s5_cre"].ap(), T["s5_cim"].ap()
    gw = T["s5_gluw"].ap()
    YM = c.YM.ap()

    def rev(ap_, lo, n, base):
        hi = base + L - 1 - lo
        end = hi - n
        return ap_[:, hi:(end if end >= 0 else None):-1]

    for ch in range(8):
        c.dma("sp", u.ap, P[ch * 128:(ch + 1) * 128, :], u, c.Pk)
        first = True
        for d in range(2):
            off = 0 if d == 0 else cfg.ctx
            for gpl in range(4):
                gp = ch * 4 + gpl
                col = d * 32 + gp
                rowb = (d * 64 + gp * 2) * 64
                c.dma("sp", braw[0].ap, bre[rowb:rowb + 128, :], braw[0], c.Dk["s5_bre"])
                c.dma("sp", braw[1].ap, bim[rowb:rowb + 128, :], braw[1], c.Dk["s5_bim"])
                crc, cic, ncic = cr.ap[:, col:col + 1], ci.ap[:, col:col + 1], nci.ap[:, col:col + 1]
                c.ts("dve", bbar[0].ap, braw[0].ap, crc, None, ALU.mult, None, bbar[0], [braw[0], cr])
                c.stt("dve", bbar[0].ap, braw[1].ap, ncic, bbar[0].ap, ALU.mult, ALU.add, bbar[0], [braw[1], nci, bbar[0]])
                c.ts("dve", bbar[1].ap, braw[1].ap, crc, None, ALU.mult, None, bbar[1], [braw[1], cr])
                c.stt("dve", bbar[1].ap, braw[0].ap, cic, bbar[1].ap, ALU.mult, ALU.add, bbar[1], [braw[0], ci, bbar[1]])
                for k in range(2):
                    c.memset("pool", xpad[k].ap, 0.0, xpad[k])
                    c.copy("pool", xpad[k].ap[0:64, 32 * gpl:32 * gpl + 16], bbar[k].ap[0:64, :], xpad[k], [bbar[k]])
                    c.copy("pool", xpad[k].ap[64:128, 32 * gpl + 16:32 * gpl + 32], bbar[k].ap[64:128, :], xpad[k], [bbar[k]])
                    ps = c.bank(6 + k)
                    c.tr(ps.ap[:, 0:128], xpad[k].ap, c.ident.ap, ps, [xpad[k], c.ident])
                    c.copy("act", lB[k].ap, ps.ap[:, 0:128], lB[k], [ps])
                rowc = (d * 64 + gp * 2) * 16
                for k, src in ((0, cre), (1, cim)):
                    c.memset("pool", craw[k].ap[0:32, :], 0.0, craw[k])
                    c.dma("sp", craw[k].ap[0:16, 0:64], src[rowc:rowc + 16, :], craw[k], c.Dk["s5_cre" if k == 0 else "s5_cim"])
                    c.dma("sp", craw[k].ap[16:32, 64:128], src[rowc + 16:rowc + 32, :], craw[k], c.Dk["s5_cre" if k == 0 else "s5_cim"])
                    ps = c.bank(6 + k)
                    c.tr(ps.ap[:, 0:32], craw[k].ap[0:32, :], c.ident.ap[0:32, 0:32], ps, [craw[k], c.ident])
                    c.memset("pool", lC[k].ap, 0.0, lC[k])
                    if k == 0:
                        c.copy("act", lC[k].ap[:, 32 * gpl:32 * gpl + 32], ps.ap[:, 0:32], lC[k], [ps])
                    else:
                        c.op("act", lambda e, o=lC[k].ap[:, 32 * gpl:32 * gpl + 32], i=ps.ap[:, 0:32]: e.mul(out=o, in_=i, mul=-1.0),
                             [ps], [lC[k]])
                c.memset("pool", Ec.ap[:, 0:1], 1.0, Ec)
                c.memset("pool", Es.ap[:, 0:1], 0.0, Es)
                for k in range(NLV):
                    n_ = 1 << k
                    cnt = min(n_, CH - n_)
                    if cnt <= 0:
                        break
                    a_ = ur.ap[:, k * 64 + col:k * 64 + col + 1]
                    b_ = ui.ap[:, k * 64 + col:k * 64 + col + 1]
                    nb_ = un.ap[:, k * 64 + col:k * 64 + col + 1]
                    c.ts("dve", Ec.ap[:, n_:n_ + cnt], Ec.ap[:, 0:cnt], a_, None, ALU.mult, None, Ec, [Ec, ur])
                    c.stt("dve", Ec.ap[:, n_:n_ + cnt], Es.ap[:, 0:cnt], nb_, Ec.ap[:, n_:n_ + cnt], ALU.mult, ALU.add, Ec, [Es, un, Ec])
                    c.ts("dve", Es.ap[:, n_:n_ + cnt], Es.ap[:, 0:cnt], a_, None, ALU.mult, None, Es, [Es, ur])
                    c.stt("dve", Es.ap[:, n_:n_ + cnt], Ec.ap[:, 0:cnt], b_, Es.ap[:, n_:n_ + cnt], ALU.mult, ALU.add, Es, [Ec, ui, Es])
                c.ts("dve", rt.ap, onesC.ap, rho.ap[:, col:col + 1], None, ALU.mult, None, rt, [onesC, rho])
                for blk in range(nblk_l):
                    n = min(512, L - blk * 512)
                    for k in range(2):
                        ps = c.bank((blk * 2 + k) % 4)
                        rhs = u.ap[:, off + blk * 512:off + blk * 512 + n] if d == 0 else rev(u.ap, blk * 512, n, off)
                        c.mm(ps.ap[:, 0:n], lB[k].ap, rhs, ps, [lB[k], u])
                        c.copy("act", bb[k].ap[:, blk * 512:blk * 512 + n], ps.ap[:, 0:n], bb[k], [ps])
                v3 = lambda t_: t_.ap.rearrange("p (c j) -> p c j", j=CH)
                Ecb = Ec.ap.unsqueeze(1).to_broadcast([128, NCH, CH])
                Esb = Es.ap.unsqueeze(1).to_broadcast([128, NCH, CH])
                c.tt("dve", v3(tmpL), v3(bb[1]), Esb, ALU.mult, tmpL, [bb[1], Es])
                c.tt("dve", v3(mb[0]), v3(bb[0]), Ecb, ALU.mult, mb[0], [bb[0], Ec])
                c.tt("dve", mb[0].ap, mb[0].ap, tmpL.ap, ALU.add, mb[0], [mb[0], tmpL])
                c.tt("dve", v3(tmpL), v3(bb[0]), Esb, ALU.mult, tmpL, [bb[0], Es])
                c.tt("dve", v3(mb[1]), v3(bb[1]), Ecb, ALU.mult, mb[1], [bb[1], Ec])
                c.tt("dve", mb[1].ap, mb[1].ap, tmpL.ap, ALU.subtract, mb[1], [mb[1], tmpL])
                wrc, wic, wnc = wr.ap[:, col:col + 1], wi.ap[:, col:col + 1], wn.ap[:, col:col + 1]
                for cc in range(NCH):
                    sl = slice(cc * CH, (cc + 1) * CH)
                    cb = car[cc % 2]
                    for k in range(2):
                        init = 0.0 if cc == 0 else cb.ap[:, k:k + 1]
                        c.op("dve", lambda e, o=bb[k].ap[:, sl], i=mb[k].ap[:, sl], init=init: e.tensor_tensor_scan(
                            out=o, data0=rt.ap, data1=i, initial=init, op0=ALU.mult, op1=ALU.add),
                            [mb[k], rt] + ([cb] if cc > 0 else []), [bb[k]])
                    if cc < NCH - 1:
                        nb_ = car[(cc + 1) % 2]
                        lr, li = bb[0].ap[:, (cc + 1) * CH - 1:(cc + 1) * CH], bb[1].ap[:, (cc + 1) * CH - 1:(cc + 1) * CH]
                        c.ts("dve", nb_.ap[:, 2:3], li, wnc, None, ALU.mult, None, nb_, [bb[1], wn])
                        c.stt("dve", nb_.ap[:, 0:1], lr, wrc, nb_.ap[:, 2:3], ALU.mult, ALU.add, nb_, [bb[0], wr, nb_])
                        c.ts("dve", nb_.ap[:, 3:4], lr, wic, None, ALU.mult, None, nb_, [bb[0], wi])
                        c.stt("dve", nb_.ap[:, 1:2], li, wrc, nb_.ap[:, 3:4], ALU.mult, ALU.add, nb_, [bb[1], wr, nb_])
                c.tt("dve", v3(tmpL), v3(bb[1]), Esb, ALU.mult, tmpL, [bb[1], Es])
                c.tt("dve", v3(mb[0]), v3(bb[0]), Ecb, ALU.mult, mb[0], [bb[0], Ec])
                c.tt("dve", mb[0].ap, mb[0].ap, tmpL.ap, ALU.subtract, mb[0], [mb[0], tmpL])
                c.tt("dve", v3(tmpL), v3(bb[0]), Esb, ALU.mult, tmpL, [bb[0], Es])
                c.tt("dve", v3(mb[1]), v3(bb[1]), Ecb, ALU.mult, mb[1], [bb[1], Ec])
                c.tt("dve", mb[1].ap, mb[1].ap, tmpL.ap, ALU.add, mb[1], [mb[1], tmpL])
                for blk in range(nblk_s):
                    n = min(512, cfg.seq - blk * 512)
                    ps = c.bank(4 + blk % 2)
                    for k in range(2):
                        if d == 0:
                            rhs = mb[k].ap[:, cfg.ctx + blk * 512:cfg.ctx + blk * 512 + n]
                        else:
                            hi = L - 1 - blk * 512
                            end = hi - n
                            rhs = mb[k].ap[:, hi:(end if end >= 0 else None):-1]
                        c.mm(ps.ap[:, 0:n], lC[k].ap, rhs, ps, [lC[k], mb[k]], start=(k == 0), stop=(k == 1))
                    ysl = yacc.ap[:, blk * 512:blk * 512 + n]
                    if first:
                        c.copy("dve", ysl, ps.ap[:, 0:n], yacc, [ps])
                    else:
                        c.tt("dve", ysl, ysl, ps.ap[:, 0:n], ALU.add, yacc, [ps, yacc])
                first = False
        ul = u.ap[:, cfg.ctx:cfg.ctx + cfg.seq]
        c.stt("dve", yacc.ap, ul, dT.ap[:, ch:ch + 1], yacc.ap, ALU.mult, ALU.add, yacc, [u, dT, yacc])
        c.tt("dve", gt.ap, yacc.ap, yacc.ap, ALU.mult, gt, [yacc])
        c.ts("dve", gt.ap, gt.ap, 0.044715, 1.0, ALU.mult, ALU.add, gt, [gt])
        c.tt("dve", gt.ap, gt.ap, yacc.ap, ALU.mult, gt, [gt, yacc])
        c.act(gt.ap, gt.ap, AF.Tanh, gt, [gt], scale=0.7978845608028654)
        c.ts("dve", gt.ap, gt.ap, 1.0, 0.5, ALU.add, ALU.mult, gt, [gt])
        c.tt("dve", yacc.ap, yacc.ap, gt.ap, ALU.mult, yacc, [yacc, gt])
        c.memset("pool", lG.ap, 0.0, lG)
        for g in range(8):
            c.dma("sp", lG.ap[16 * g:16 * g + 16, 16 * g:16 * g + 16], gw[(ch * 8 + g) * 16:(ch * 8 + g) * 16 + 16, :],
                  lG, c.Dk["s5_gluw"])
        for blk in range(nblk_s):
            n = min(512, cfg.seq - blk * 512)
            ps = c.bank(4 + blk % 2)
            c.mm(ps.ap[:, 0:n], lG.ap, yacc.ap[:, blk * 512:blk * 512 + n], ps, [lG, yacc])
            c.act(gt.ap[:, blk * 512:blk * 512 + n], ps.ap[:, 0:n], AF.Sigmoid, gt, [ps, gbT], bias=gbT.ap[:, ch:ch + 1])
        c.tt("dve", yo.ap, yacc.ap, gt.ap, ALU.mult, yo, [yacc, gt])
        c.dma("pool", YM[ch * 128:(ch + 1) * 128, :], yo.ap, c.YMk, yo)
    c.stage_end()


def stage_rw_lora(c):
    cfg, T = c.cfg, c.T
    ZS, LO = c.ZS.ap(), c.LO.ap()
    NP_ = cfg.npos
    w0T = load_colvec(c, T["rw_w0"].ap(), c.Dk["rw_w0"], 16, "w0T")
    a0T = load_colvec(c, T["rw_a0"].ap(), c.Dk["rw_a0"], 16, "a0T")
    txw = c.alloc("txw", NP_); xa = c.alloc("xa", NP_)
    sg0 = c.alloc("sg0", cfg.seq); sg1 = c.alloc("sg1", cfg.seq)
    w2 = c.alloc("w2", 1024); a2 = c.alloc("a2", 1024); g2a = c.alloc("g2a", 1024); g2b = c.alloc("g2b", 1024)
    c.dma("sp", txw.ap, ZS[3072:3200, :], txw, c.ZSk)
    c.dma("sp", xa.ap, ZS[3200:3328, :], xa, c.ZSk)
    c.dma("sp", sg0.ap, ZS[3328:3456, cfg.ctx:cfg.ctx + cfg.seq], sg0, c.ZSk)
    c.dma("sp", sg1.ap[0:32, :], ZS[3456:3488, cfg.ctx:cfg.ctx + cfg.seq], sg1, c.ZSk)
    c.dma("sp", w2.ap, T["rw_w2"].ap(), w2, c.Dk["rw_w2"])
    c.dma("sp", a2.ap, T["rw_a2"].ap(), a2, c.Dk["rw_a2"])
    c.dma("sp", g2a.ap, T["rw_g2"].ap()[0:128, :], g2a, c.Dk["rw_g2"])
    c.dma("sp", g2b.ap[0:32, :], T["rw_g2"].ap()[128:160, :], g2b, c.Dk["rw_g2"])
    c.act(txw.ap, txw.ap, AF.Tanh, txw, [txw])
    c.act(sg0.ap, sg0.ap, AF.Sigmoid, sg0, [sg0])
    c.act(sg1.ap[0:32, :], sg1.ap[0:32, :], AF.Sigmoid, sg1, [sg1])
    ob = [c.alloc(f"lo{i}", NP_) for i in range(2)]
    oi = 0
    nb = (NP_ + 511) // 512
    for hp in range(8):
        cs = slice(hp * 128, (hp + 1) * 128)
        for kind in range(5):
            o = ob[oi % 2]; oi += 1
            if kind < 4:
                d = kind % 2
                wt, src, bT = (w2, txw, w0T) if kind < 2 else (a2, xa, a0T)
                for blk in range(nb):
                    n = min(512, NP_ - blk * 512)
                    ps = c.bank(blk % 4)
                    c.mm(ps.ap[:, 0:n], wt.ap[d * 64:(d + 1) * 64, cs], src.ap[d * 64:(d + 1) * 64, blk * 512:blk * 512 + n],
                         ps, [wt, src])
                    c.act(o.ap[:, blk * 512:blk * 512 + n], ps.ap[:, 0:n], AF.Sigmoid, o, [ps, bT],
                          bias=bT.ap[:, d * 8 + hp:d * 8 + hp + 1])
                c.dma("pool", LO[kind, hp], o.ap, c.LOk, o)
            else:
                for blk in range((cfg.seq + 511) // 512):
                    n = min(512, cfg.seq - blk * 512)
                    ps = c.bank(blk % 4)
                    c.mm(ps.ap[:, 0:n], g2a.ap[:, cs], sg0.ap[:, blk * 512:blk * 512 + n], ps, [g2a, sg0], start=True, stop=False)
                    c.mm(ps.ap[:, 0:n], g2b.ap[0:32, cs], sg1.ap[0:32, blk * 512:blk * 512 + n], ps, [g2b, sg1], start=False, stop=True)
                    c.copy("act", o.ap[:, blk * 512:blk * 512 + n], ps.ap[:, 0:n], o, [ps])
                c.dma("pool", LO[4, hp, :, 0:cfg.seq], o.ap[:, 0:cfg.seq], c.LOk, o)
    c.stage_end()


def rw_consts(c):
    m = {}
    for name, pat, base, cm, cmp_ in (("SU", 1, 0, -1, ALU.is_gt), ("SL", -1, 0, 1, ALU.is_gt),
                                       ("IU", 1, 0, -1, ALU.is_ge), ("IL", -1, 0, 1, ALU.is_ge)):
        t = c.alloc("mask" + name, 128)
        c.memset("pool", t.ap, 1.0, t)
        c.op("pool", lambda e, t=t, pat=pat, base=base, cm=cm, cmp_=cmp_: e.affine_select(
            out=t.ap, in_=t.ap, pattern=[[pat, 128]], compare_op=cmp_, fill=0.0, base=base, channel_multiplier=cm), [t], [t])
        for j in range(NSUB):
            if j > 0:
                c.memset("pool", t.ap[j * CS:(j + 1) * CS, 0:j * CS], 0.0, t)
            if j < NSUB - 1:
                c.memset("pool", t.ap[j * CS:(j + 1) * CS, (j + 1) * CS:128], 0.0, t)
        m[name] = t
    bo = c.alloc("blockones", 128)
    c.memset("pool", bo.ap, 0.0, bo)
    c.memset("pool", bo.ap[0:64, 0:64], 1.0, bo)
    c.memset("pool", bo.ap[64:128, 64:128], 1.0, bo)
    hs = c.alloc("headsel", 2)
    c.memset("pool", hs.ap, 0.0, hs)
    c.memset("pool", hs.ap[0:64, 0:1], 1.0, hs)
    c.memset("pool", hs.ap[64:128, 1:2], 1.0, hs)
    ones1 = c.alloc("ones1", 128)
    c.memset("pool", ones1.ap[0:1, :], 1.0, ones1)
    m["BO"], m["HS"], m["ONES"] = bo, hs, ones1
    return m


def stage_rw(c):
    cfg, T = c.cfg, c.T
    ZS, LO, RW, YD, YM = c.ZS.ap(), c.LO.ap(), c.RW.ap(), c.YD.ap(), c.YM.ap()
    NP_, L = cfg.npos, cfg.ntok
    nblk = L // 128
    nlat = cfg.seq // 128
    nctx = cfg.ctx // 128
    M = rw_consts(c)
    kkT = load_colvec(c, T["rw_kk"].ap(), c.Dk["rw_kk"], 8, "kkT")
    kaT = load_colvec(c, T["rw_ka"].ap(), c.Dk["rw_ka"], 8, "kaT")
    rkT = load_colvec(c, T["rw_rk"].ap(), c.Dk["rw_rk"], 8, "rkT")
    lnrow = c.alloc("lnrow", 2048)
    c.dma("sp", lnrow.ap[0:1, 0:1024], T["rw_lnw"].ap(), lnrow, c.Dk["rw_lnw"])
    c.dma("sp", lnrow.ap[0:1, 1024:2048], T["rw_lnb"].ap(), lnrow, c.Dk["rw_lnb"])
    lnbc = c.alloc("lnbc", 2048)
    for q in range(4):
        ps = c.bank(q)
        c.mm(ps.ap[:, 0:512], M["ONES"].ap[0:1, :], lnrow.ap[0:1, q * 512:(q + 1) * 512], ps, [M["ONES"], lnrow])
        c.copy("act", lnbc.ap[:, q * 512:(q + 1) * 512], ps.ap[:, 0:512], lnbc, [ps])
    bonus_all = c.alloc("bonus", 8 * nlat * 2)
    gCt_all = c.alloc("gCt", 8 * 2 * (NP_ // CS))
    SEG = 1536 if NP_ % 1536 == 0 else NP_
    rmask = c.alloc("rmask", SEG)
    c.memset("pool", rmask.ap, 1.0, rmask)
    c.memset("pool", rmask.ap[:, 0:SEG:CS], 0.0, rmask)
    c.pers2 = c.top
    nseg = NP_ // SEG
    names = ["r", "k", "kh", "kka", "sg", "a", "cs", "lg", "kt", "E", "q"]
    S = {n_: c.alloc("f_" + n_, SEG) for n_ in names}
    O = [c.alloc(f"f_o{i}", SEG) for i in range(3)]
    oi = 0
    ngc = 2 * (NP_ // CS)
    bon = [B(bonus_all.ap[:, hp * nlat * 2:(hp + 1) * nlat * 2], (bonus_all.key[0], hp)) for hp in range(8)]
    gcs_ = [B(gCt_all.ap[:, hp * ngc:(hp + 1) * ngc], (gCt_all.key[0], hp)) for hp in range(8)]
    for hp in range(8):
        bonus, gCt = bon[hp], gcs_[hp]
        for sg_i in range(nseg):
            p0 = sg_i * SEG
            psl = slice(p0, p0 + SEG)
            nch = SEG // CS
            c.dma("sp", S["r"].ap, ZS[hp * 128:(hp + 1) * 128, psl], S["r"], c.ZSk)
            c.dma("sp", S["k"].ap, ZS[1024 + hp * 128:1024 + (hp + 1) * 128, psl], S["k"], c.ZSk)
            kkc, kac, rkc = kkT.ap[:, hp:hp + 1], kaT.ap[:, hp:hp + 1], rkT.ap[:, hp:hp + 1]
            c.ts("dve", S["kh"].ap, S["k"].ap, kkc, None, ALU.mult, None, S["kh"], [S["k"], kkT])
            c.tt("dve", S["E"].ap, S["kh"].ap, S["kh"].ap, ALU.mult, S["E"], [S["kh"]])
            for blk in range((SEG + 511) // 512):
                n = min(512, SEG - blk * 512)
                ps = c.bank(blk % 4)
                c.mm(ps.ap[:, 0:n], M["BO"].ap, S["E"].ap[:, blk * 512:blk * 512 + n], ps, [M["BO"], S["E"]])
                c.act(S["cs"].ap[:, blk * 512:blk * 512 + n], ps.ap[:, 0:n], AF.Sqrt, S["cs"], [ps])
            c.ts("dve", S["cs"].ap, S["cs"].ap, 1e-12, None, ALU.max, None, S["cs"], [S["cs"]])
            c.op("dve", lambda e, o=S["cs"].ap: e.reciprocal(out=o, in_=o), [S["cs"]], [S["cs"]])
            c.tt("dve", S["kh"].ap, S["kh"].ap, S["cs"].ap, ALU.mult, S["kh"], [S["kh"], S["cs"]])
            c.ts("dve", S["kka"].ap, S["k"].ap, kac, None, ALU.mult, None, S["kka"], [S["k"], kaT])
            for d in range(2):
                c.dma("sp", S["sg"].ap, LO[d, hp, :, psl], S["sg"], c.LOk)
                c.dma("sp", S["a"].ap, LO[2 + d, hp, :, psl], S["a"], c.LOk)
                c.ts("dve", S["sg"].ap, S["sg"].ap, -math.exp(-0.5), None, ALU.mult, None, S["sg"], [S["sg"]])
                csv = S["cs"].ap.rearrange("p (c t) -> p c t", t=CS)
                sgv = S["sg"].ap.rearrange("p (c t) -> p c t", t=CS)
                lgv = S["lg"].ap.rearrange("p (c t) -> p c t", t=CS)
                c.op("dve", lambda e, o=S["cs"].ap, i=S["sg"].ap: e.tensor_tensor_scan(
                    out=o, data0=rmask.ap, data1=i, initial=0.0, op0=ALU.mult, op1=ALU.add),
                    [S["sg"], rmask], [S["cs"]])
                ctot = csv[:, :, CS - 1:CS]
                if d == 0:
                    c.copy("act", S["lg"].ap, S["cs"].ap, S["lg"], [S["cs"]])
                else:
                    c.tt("dve", lgv, sgv, csv, ALU.subtract, S["lg"], [S["sg"], S["cs"]])
                    c.tt("dve", lgv, lgv, ctot.to_broadcast([128, nch, CS]), ALU.add, S["lg"], [S["lg"], S["cs"]])
                gcs = gCt.ap[:, d * (NP_ // CS) + p0 // CS:d * (NP_ // CS) + p0 // CS + nch]
                c.act(gcs, S["cs"].ap[:, CS - 1:SEG:CS], AF.Exp, gCt, [S["cs"]])
                c.stt("dve", S["kt"].ap, S["a"].ap, -1.0, S["kka"].ap, ALU.add, ALU.mult, S["kt"], [S["a"], S["kka"]])
                c.tt("dve", S["kt"].ap, S["kt"].ap, S["k"].ap, ALU.add, S["kt"], [S["kt"], S["k"]])
                c.tt("dve", S["a"].ap, S["a"].ap, S["kh"].ap, ALU.mult, S["a"], [S["a"], S["kh"]])
                if d == 0:
                    c.stt("dve", S["q"].ap, S["r"].ap, rkc, S["kt"].ap, ALU.mult, ALU.mult, S["q"], [S["r"], rkT, S["kt"]])
                else:
                    c.stt("dve", S["E"].ap, S["r"].ap, rkc, S["kt"].ap, ALU.mult, ALU.mult, S["E"], [S["r"], rkT, S["kt"]])
                    c.tt("dve", S["q"].ap, S["q"].ap, S["E"].ap, ALU.add, S["q"], [S["q"], S["E"]])

                def emit_out(idx, base, ex, neg):
                    nonlocal oi
                    o = O[oi % 3]; oi += 1
                    c.tt("dve", o.ap, base.ap, ex.ap, ALU.mult, o, [base, ex])
                    if neg:
                        c.op("act", lambda e, o=o: e.mul(out=o.ap, in_=o.ap, mul=-1.0), [o], [o])
                    c.dma("pool", RW[d, hp, :, idx, psl], o.ap, c.RWk, o)
                c.act(S["E"].ap, S["lg"].ap, AF.Exp, S["E"], [S["lg"]], scale=-1.0)
                emit_out(0, S["a"], S["E"], True)
                emit_out(2, S["kt"], S["E"], False)
                c.act(S["E"].ap, S["lg"].ap, AF.Exp, S["E"], [S["lg"]])
                emit_out(3, S["r"], S["E"], False)
                c.tt("dve", S["E"].ap, S["lg"].ap, S["sg"].ap, ALU.subtract, S["E"], [S["lg"], S["sg"]])
                c.act(S["E"].ap, S["E"].ap, AF.Exp, S["E"], [S["E"]])
                emit_out(1, S["kh"], S["E"], False)
                Ev = S["E"].ap.rearrange("p (c t) -> p c t", t=CS)
                c.tt("dve", Ev, ctot.to_broadcast([128, nch, CS]), lgv, ALU.subtract, S["E"], [S["cs"], S["lg"]])
                c.act(S["E"].ap, S["E"].ap, AF.Exp, S["E"], [S["E"]])
                emit_out(4, S["a"], S["E"], True)
                emit_out(5, S["kt"], S["E"], False)
            for bl in range(SEG // 128):
                gpos = p0 + bl * 128
                if gpos < cfg.ctx or gpos >= cfg.ctx + cfg.seq:
                    continue
                lb = (gpos - cfg.ctx) // 128
                ps = c.bank(4 + bl % 2)
                c.mm(ps.ap[:, 0:2], S["q"].ap[:, bl * 128:(bl + 1) * 128], M["HS"].ap, ps, [S["q"], M["HS"]])
                c.copy("act", bonus.ap[:, lb * 2:lb * 2 + 2], ps.ap[:, 0:2], bonus, [ps])
    c.p.barrier()
    c.top = c.pers2
    bufs2 = rw_chain_alloc(c)
    bufs3 = rw_readout_alloc(c)
    inter = None
    for hp in range(8):
        rw_chain(c, hp, M, gcs_[hp], bufs2, inter)
        if inter is not None:
            for _ in inter:
                pass
        inter = rw_readout(c, hp, M, bon[hp], lnbc, bufs3)
    for _ in inter:
        pass
    c.stage_end()


RW_NG = 1
RW_NSET = 2


def rw_chain_alloc(c):
    cfg = c.cfg
    NP_ = cfg.npos
    NG, nset = RW_NG, RW_NSET
    H = [[c.alloc(f"H{d}{hl}", 64) for hl in range(2)] for d in range(2)]
    gC1 = c.alloc("gC1", 2 * (NP_ // CS))

    def mk(name, n):
        return [[[c.alloc(f"{name}{s_}{g}{d}", n) for d in range(2)] for g in range(NG)] for s_ in range(nset)]
    OP = [[[[c.alloc(f"op{s_}{g}{d}{hl}", 6 * 128) for hl in range(2)] for d in range(2)] for g in range(NG)] for s_ in range(nset)]
    VF = mk("vf", 128)
    VT = mk("vt", 128)
    TM = [[[[c.alloc(f"tm{s_}{g}{d}{hl}", 192) for hl in range(2)] for d in range(2)] for g in range(NG)] for s_ in range(nset)]
    RES = [[[[c.alloc(f"res{s_}{g}{d}{hl}", 2 * NSUB * 64 + 192) for hl in range(2)] for d in range(2)] for g in range(NG)] for s_ in range(nset)]
    TMP = [[[[[c.alloc(f"t{s_}{g}{d}{hl}{i}", 128) for i in range(9)] for hl in range(2)] for d in range(2)] for g in range(NG)] for s_ in range(nset)]
    YO = [[c.alloc(f"yo{s_}{d}", 128) for d in range(2)] for s_ in range(4)]
    return dict(H=H, gC1=gC1, OP=OP, VF=VF, VT=VT, TM=TM, RES=RES, TMP=TMP, YO=YO)


def rw_chain(c, hp, M, gCt, bufs, inter):
    cfg = c.cfg
    ZS, RW, YD = c.ZS.ap(), c.RW.ap(), c.YD.ap()
    NP_, L = cfg.npos, cfg.ntok
    nblk = L // 128
    NG, nset = RW_NG, RW_NSET
    ident = c.ident
    H, gC1, OP, VF, VT, TM, RES, TMP, YO = (bufs[k] for k in ("H", "gC1", "OP", "VF", "VT", "TM", "RES", "TMP", "YO"))
    YDk = B(None, ("YD_s", hp))
    for d in range(2):
        for hl in range(2):
            c.memset("pool", H[d][hl].ap[0:64, :], 0.0, H[d][hl])
    c.dma("sp", gC1.ap[0:64, :], gCt.ap[64:128, :], gC1, gCt)
    gC = [gCt, gC1]

    def unit(s_, g, d, hl, blk_pos, islat, slotbase, gci0, gcb):
        op = OP[s_][g][d][hl]
        opv = op.ap.rearrange("p (i t) -> p i t", i=6)
        At, Bt, Kt, Rt, Ab, Kb = (opv[0:64, i, :] for i in range(6))
        tm = TM[s_][g][d][hl]
        Btm, Abm, Kbm = tm.ap[:, 0:64], tm.ap[:, 64:128], tm.ap[:, 128:192]
        Vtm = VT[s_][g][d].ap[:, hl * 64:(hl + 1) * 64]
        vtb = VT[s_][g][d]
        res = RES[s_][g][d][hl]
        PhiT, Psi = res.ap[0:64, 0:NSUB * 64], res.ap[0:64, NSUB * 64:2 * NSUB * 64]
        PT, Y2 = res.ap[0:64, 2 * NSUB * 64:2 * NSUB * 64 + 128], res.ap[:, 2 * NSUB * 64 + 128:2 * NSUB * 64 + 192]
        t = TMP[s_][g][d][hl]
        strict = M["SU"] if d == 0 else M["SL"]
        strictT = M["SL"] if d == 0 else M["SU"]
        incl = M["IU"] if d == 0 else M["IL"]

        def slot(i):
            bk = c.bank(slotbase)
            return B(bk.ap[:, i * 128:(i + 1) * 128], bk.key)
        s0, s1 = slot(0), slot(1)
        for src, dst in ((Bt, Btm), (Ab, Abm), (Kb, Kbm)):
            c.tr(s0.ap[:, 0:64], src, ident.ap[0:64, 0:64], s0, [op, ident])
            c.copy("act", dst, s0.ap[:, 0:64], tm, [s0])
            yield
        LAT, LA, LKT, MAT, MKT = t[0], t[1], t[4], t[5], t[6]
        c.mm(s0.ap, At, Bt, s0, [op]); c.tt("dve", LAT.ap, s0.ap, strict.ap, ALU.mult, LAT, [s0, strict])
        c.mm(s1.ap, Bt, At, s1, [op]); c.tt("dve", LA.ap, s1.ap, strictT.ap, ALU.mult, LA, [s1, strictT])
        yield
        c.mm(s0.ap, Kt, Bt, s0, [op]); c.tt("dve", LKT.ap, s0.ap, strict.ap, ALU.mult, LKT, [s0, strict])
        if islat:
            c.mm(s1.ap, At, Rt, s1, [op]); c.tt("dve", MAT.ap, s1.ap, incl.ap, ALU.mult, MAT, [s1, incl])
        yield
        if islat:
            c.mm(s0.ap, Kt, Rt, s0, [op]); c.tt("dve", MKT.ap, s0.ap, incl.ap, ALU.mult, MKT, [s0, incl])
        Z = [t[7], t[8]]
        c.mm(s1.ap[:, 0:64], LKT.ap, Vtm, s1, [LKT, vtb])
        c.copy("act", Z[0].ap[:, 64:128], s1.ap[:, 0:64], Z[0], [s1])
        c.copy("act", Z[0].ap[:, 0:64], Btm, Z[0], [tm])
        yield
        X, XT = LA, LAT
        alt = [(t[2], t[3]), (t[1], t[0])]
        zc = 0
        NL = int(math.log2(CS))
        for i in range(NL):
            c.mm(s0.ap, XT.ap, Z[zc].ap, s0, [XT, Z[zc]])
            c.tt("dve", Z[1 - zc].ap, Z[zc].ap, s0.ap, ALU.add, Z[1 - zc], [Z[zc], s0])
            zc = 1 - zc
            if i < NL - 1:
                nX, nXT = alt[i % 2]
                c.mm(s1.ap, X.ap, XT.ap, s1, [X, XT])
                c.copy("act", nXT.ap, s1.ap, nXT, [s1])
                if i < NL - 2:
                    yield
                    c.mm(s1.ap, XT.ap, X.ap, s1, [X, XT])
                    c.copy("act", nX.ap, s1.ap, nX, [s1])
                X, XT = nX, nXT
            yield
        Zf = Z[zc]
        W1, U2 = Zf.ap[:, 0:64], Zf.ap[:, 64:128]
        for j in range(NSUB):
            js = slice(j * CS, (j + 1) * CS)
            c.mm(s0.ap[0:64, 0:64], Zf.ap[js, 0:64], tm.ap[js, 64:128], s0, [Zf, tm])
            c.stt("dve", PhiT[:, j * 64:(j + 1) * 64], c.ident.ap[0:64, 0:64], gcb.ap[0:64, gci0 + j:gci0 + j + 1],
                  s0.ap[0:64, 0:64], ALU.mult, ALU.add, res, [c.ident, gcb, s0])
            c.mm(s1.ap[0:64, 0:64], tm.ap[js, 64:128], Zf.ap[js, 64:128], s1, [Zf, tm], start=True, stop=False)
            c.mm(s1.ap[0:64, 0:64], tm.ap[js, 128:192], VT[s_][g][d].ap[js, hl * 64:(hl + 1) * 64], s1, [tm, vtb], start=False, stop=True)
            c.copy("act", Psi[:, j * 64:(j + 1) * 64], s1.ap[0:64, 0:64], res, [s1])
            yield
        if islat:
            c.mm(s0.ap[:, 0:64], MAT.ap, U2, s0, [MAT, Zf], start=True, stop=False)
            c.mm(s0.ap[:, 0:64], MKT.ap, Vtm, s0, [MKT, vtb], start=False, stop=True)
            c.copy("act", Y2, s0.ap[:, 0:64], res, [s0])
            c.mm(s1.ap[0:64, :], W1, MAT.ap, s1, [Zf, MAT])
            c.tt("dve", PT, s1.ap[0:64, :], Rt, ALU.add, res, [s1, op])
        yield

    nsteps = nblk
    step = 0
    gi = 0
    yo_i = 0
    while step < nsteps:
        s_ = gi % nset
        gi += 1
        ng = min(NG, nsteps - step)
        gens = []
        info = []
        for g in range(ng):
            for d in range(2):
                bi = step + g
                blk = bi if d == 0 else (nblk - 1 - bi)
                pos = blk * 128 + (0 if d == 0 else cfg.ctx)
                islat = cfg.ctx <= pos < cfg.ctx + cfg.seq
                c.dma("sp", VF[s_][g][d].ap, ZS[2048 + hp * 128:2048 + (hp + 1) * 128, pos:pos + 128], VF[s_][g][d], c.ZSk)
                pb = c.bank(4 + d)
                c.tr(pb.ap[:, 0:128], VF[s_][g][d].ap, c.ident.ap, pb, [VF[s_][g][d], c.ident])
                c.copy("act", VT[s_][g][d].ap, pb.ap[:, 0:128], VT[s_][g][d], [pb])
                for hl in range(2):
                    op = OP[s_][g][d][hl]
                    c.dma("sp", op.ap[0:64, :].rearrange("p (i t) -> p i t", i=6),
                          RW[d, hp, hl * 64:(hl + 1) * 64, :, pos:pos + 128], op, c.RWk)
                    u_idx = (g * 2 + d) * 2 + hl
                    gci = d * (NP_ // CS) + pos // CS
                    gcb = gC[hl]
                    gens.append(unit(s_, g, d, hl, pos, islat, u_idx, gci, gcb))
                    info.append((g, d, hl, pos, islat))
        results = [None] * len(gens)
        live = list(range(len(gens)))
        while live:
            nxt = []
            for ui in live:
                try:
                    r = next(gens[ui])
                    if r is not None:
                        results[ui] = r
                    nxt.append(ui)
                except StopIteration:
                    pass
            live = nxt
        for g in range(ng):
            for d in range(2):
                bi = step + g
                blk = bi if d == 0 else (nblk - 1 - bi)
                pos = blk * 128 + (0 if d == 0 else cfg.ctx)
                islat = cfg.ctx <= pos < cfg.ctx + cfg.seq
                gcol_i = d * (NP_ // 128) + pos // 128
                yo = YO[yo_i % 4][d]
                for hl in range(2):
                    res = RES[s_][g][d][hl]
                    Hh = H[d][hl]
                    o2 = 2 * NSUB * 64
                    PT, Y2 = res.ap[0:64, o2:o2 + 128], res.ap[:, o2 + 128:o2 + 192]
                    pb = c.bank(6 + hl)
                    order = range(NSUB) if d == 0 else range(NSUB - 1, -1, -1)
                    for j in order:
                        js = slice(j * CS, (j + 1) * CS)
                        PhiT, Psi = res.ap[0:64, j * 64:(j + 1) * 64], res.ap[0:64, NSUB * 64 + j * 64:NSUB * 64 + (j + 1) * 64]
                        if islat:
                            pbs = B(pb.ap[js, 0:64], pb.key)
                            c.mm(pbs.ap, PT[:, js], Hh.ap[0:64, :], pbs, [res, Hh])
                            c.tt("dve", yo.ap[js, hl * 64:(hl + 1) * 64], pbs.ap, Y2[js, :], ALU.add, yo, [pbs, res])
                        pbs2 = B(pb.ap[0:64, 128:192], pb.key)
                        c.mm(pbs2.ap, PhiT, Hh.ap[0:64, :], pbs2, [res, Hh])
                        c.tt("dve", Hh.ap[0:64, :], pbs2.ap, Psi, ALU.add, Hh, [pbs2, res])
                if islat:
                    lpos = pos - cfg.ctx
                    c.dma("pool", YD[d, lpos:lpos + 128, hp * 128:(hp + 1) * 128], yo.ap, YDk, yo)
            yo_i += 1
        step += ng
        if inter is not None:
            next(inter, None)


def rw_readout_alloc(c):
    yo = c.alloc("rd_out", c.cfg.seq, BF16)
    bufs = [[c.alloc(f"rd{n_}{i}", 128) for i in range(2)] for n_ in ("yf", "yb", "vf", "g", "t", "sq")]
    st = [c.alloc(f"rdst{i}", 8) for i in range(2)]
    return yo, bufs, st


def rw_readout(c, hp, M, bonus, lnbc, bufs3):
    cfg = c.cfg
    ZS, LO, YD, YM = c.ZS.ap(), c.LO.ap(), c.YD.ap(), c.YM.ap()
    nlat = cfg.seq // 128
    yo, bufs, st = bufs3
    YDk = B(None, ("YD_s", hp))
    for lb in range(nlat):
        yf, yb, vf, gf, tt_, sq = (bufs[i][lb % 2] for i in range(6))
        s_ = st[lb % 2]
        pos = cfg.ctx + lb * 128
        c.dma("sp", yf.ap, YD[0, lb * 128:(lb + 1) * 128, hp * 128:(hp + 1) * 128], yf, YDk)
        c.dma("sp", yb.ap, YD[1, lb * 128:(lb + 1) * 128, hp * 128:(hp + 1) * 128], yb, YDk)
        c.dma("sp", vf.ap, ZS[2048 + hp * 128:2048 + (hp + 1) * 128, pos:pos + 128], vf, c.ZSk)
        c.dma("sp", gf.ap, LO[4, hp, :, lb * 128:(lb + 1) * 128], gf, c.LOk)
        c.tt("dve", yf.ap, yf.ap, yb.ap, ALU.add, yf, [yf, yb])
        yv = yf.ap.rearrange("p (h v) -> p h v", h=2)
        c.op("dve", lambda e, o=s_.ap[:, 0:2], i=yv: e.tensor_reduce(out=o, in_=i, axis=AX.X, op=ALU.add), [yf], [s_])
        c.ts("dve", s_.ap[:, 0:2], s_.ap[:, 0:2], 1.0 / 64, None, ALU.mult, None, s_, [s_])
        c.tt("dve", yv, yv, s_.ap[:, 0:2].unsqueeze(2).to_broadcast([128, 2, 64]), ALU.subtract, yf, [yf, s_])
        c.tt("dve", sq.ap, yf.ap, yf.ap, ALU.mult, sq, [yf])
        c.op("dve", lambda e, o=s_.ap[:, 2:4], i=sq.ap.rearrange("p (h v) -> p h v", h=2): e.tensor_reduce(
            out=o, in_=i, axis=AX.X, op=ALU.add), [sq], [s_])
        c.ts("dve", s_.ap[:, 2:4], s_.ap[:, 2:4], 1.0 / 64, 64e-5, ALU.mult, ALU.add, s_, [s_])
        c.act(s_.ap[:, 2:4], s_.ap[:, 2:4], AF.Sqrt, s_, [s_])
        c.op("dve", lambda e, o=s_.ap[:, 2:4]: e.reciprocal(out=o, in_=o), [s_], [s_])
        c.tt("dve", yv, yv, s_.ap[:, 2:4].unsqueeze(2).to_broadcast([128, 2, 64]), ALU.mult, yf, [yf, s_])
        c.tt("dve", yf.ap, yf.ap, lnbc.ap[:, hp * 128:(hp + 1) * 128], ALU.mult, yf, [yf, lnbc])
        c.tt("dve", yf.ap, yf.ap, lnbc.ap[:, 1024 + hp * 128:1024 + (hp + 1) * 128], ALU.add, yf, [yf, lnbc])
        pb = c.bank(4 + lb % 2)
        c.tr(pb.ap[:, 0:128], vf.ap, c.ident.ap, pb, [vf, c.ident])
        tv = tt_.ap.rearrange("p (h v) -> p h v", h=2)
        c.tt("dve", tv, pb.ap[:, 0:128].rearrange("p (h v) -> p h v", h=2),
             bonus.ap[:, lb * 2:lb * 2 + 2].unsqueeze(2).to_broadcast([128, 2, 64]), ALU.mult, tt_, [pb, bonus])
        c.tt("dve", yf.ap, yf.ap, tt_.ap, ALU.add, yf, [yf, tt_])
        pb2 = c.bank(6 + lb % 2)
        c.tr(pb2.ap[:, 0:128], yf.ap, c.ident.ap, pb2, [yf, c.ident])
        c.tt("dve", yo.ap[:, lb * 128:(lb + 1) * 128], pb2.ap[:, 0:128], gf.ap, ALU.mult, yo, [pb2, gf])
        yield
    c.dma("pool", YM[1024 + hp * 128:1024 + (hp + 1) * 128, :], yo.ap, c.YMk, yo)


def bcast_rows(c, colT, name, bank0=0, res=None, dg=None):
    if res is None:
        res = c.alloc(name, D)
    if dg is None:
        dg = c.alloc(name + "_dg", D)
    for j in range(16):
        c.ts("dve", dg.ap[:, j * 128:(j + 1) * 128], c.ident.ap, colT[:, j:j + 1], None, ALU.mult, None, dg, [c.ident, c.mT, c.fgT])
    for q in range(4):
        ps = c.bank(bank0 + q)
        c.mm(ps.ap, c.ones128.ap, dg.ap[:, q * 512:(q + 1) * 512], ps, [c.ones128, dg])
        c.copy("act", res.ap[:, q * 512:(q + 1) * 512], ps.ap, res, [ps])
    return res


def stage_outproj(c):
    cfg, T = c.cfg, c.T
    mTv = c.mT.ap.rearrange("p (o r) -> p o r", r=2)
    YM, X1, FT, GT = c.YM.ap(), c.X1.ap(), c.FT.ap(), c.GT.ap()
    g2 = load_colvec(c, T["norm2_g"].ap(), c.Dk["norm2_g"], 16, "g2T")
    scl2 = c.alloc("scl2", 16)
    c.stt("dve", scl2.ap, mTv[:, 64:80, 0], 1.0, g2.ap, ALU.add, ALU.mult, scl2, [c.mT, g2])
    gt1bc = bcast_rows(c, mTv[:, 32:48, 0], "gt1bc")
    wout = c.alloc("wout", 16 * D, BF16)
    wov = wout.ap.rearrange("p (jc n) -> p jc n", jc=16)
    wsrc = T["w_out"].ap().rearrange("(jc p) n -> p jc n", p=128)
    for jc in range(16):
        c.dma("pool", wov[:, jc, :], wsrc[:, jc, :], wout, c.Dk["w_out"])
    rw = c.alloc("routerw", 16 * 32)
    rwv = rw.ap.rearrange("p (kc e) -> p kc e", kc=16)
    c.dma("sp", rwv, T["router_w"].ap().rearrange("(kc p) e -> p kc e", p=128), rw, c.Dk["router_w"])
    rb = c.alloc("routerb", 32)
    c.dma("sp", rb.ap[0:1, :], T["router_b"].ap(), rb, c.Dk["router_b"])
    TBK = min(512, cfg.nown)
    ymb = [c.alloc(f"ymb{i}", 16 * TBK, BF16) for i in range(2)]
    ftb = [c.alloc(f"ftb{i}", 16 * TBK, BF16) for i in range(2)]
    gtb = [c.alloc(f"gtb{i}", TBK) for i in range(2)]
    xt = [c.alloc(f"ox{i}", D) for i in range(2)]
    xs = c.alloc("oxs", D)
    tmp = c.alloc("otmp", 512)
    junk = c.alloc("ojunk", D, BF16)
    f32 = c.alloc("of32", 16 * 128)
    f32v = f32.ap.rearrange("p (j t) -> p j t", j=16)
    st = c.alloc("ost", 8)
    lg = c.alloc("olg", 32); mx = c.alloc("omx", 8); ex = c.alloc("oex", 32); mk = c.alloc("omk", 32)
    ymsrc = YM.rearrange("(jc p) t -> p jc t", p=128)
    ftdst = FT.rearrange("(jc p) t -> p jc t", p=128)
    for blk in range(cfg.nown // TBK):
        yb, fb, gb = ymb[blk % 2], ftb[blk % 2], gtb[blk % 2]
        ybv = yb.ap.rearrange("p (jc t) -> p jc t", jc=16)
        fbv = fb.ap.rearrange("p (jc t) -> p jc t", jc=16)
        c.dma("sp", ybv, ymsrc[:, :, bass.ts(c.qv * (cfg.nown // TBK) + blk, TBK)], yb, c.YMk)
        for ti in range(TBK // 128):
            tok0 = blk * TBK + ti * 128
            x = xt[ti % 2]
            c.dma("sp", x.ap, T["x_own"].ap()[tok0:tok0 + 128, :], x, c.Dk["x_own"])
            for cb in range(4):
                ps = c.bank(cb)
                for jc in range(16):
                    c.mm(ps.ap, ybv[:, jc, ti * 128:(ti + 1) * 128], wov[:, jc, cb * 512:(cb + 1) * 512], ps, [yb, wout],
                         start=(jc == 0), stop=(jc == 15))
                c.tt("dve", tmp.ap, ps.ap, gt1bc.ap[:, cb * 512:(cb + 1) * 512], ALU.mult, tmp, [ps, gt1bc])
                c.tt("dve", x.ap[:, cb * 512:(cb + 1) * 512], x.ap[:, cb * 512:(cb + 1) * 512], tmp.ap, ALU.add, x, [x, tmp])
            c.dma("act", X1[tok0:tok0 + 128, :], x.ap, c.X1k, x)
            c.act(junk.ap, x.ap, AF.Square, junk, [x], accum=st.ap[:, 0:1], accb=st)
            c.ts("dve", st.ap[:, 0:1], st.ap[:, 0:1], 1.0 / D, 1e-5, ALU.mult, ALU.add, st, [st, junk])
            c.act(st.ap[:, 0:1], st.ap[:, 0:1], AF.Sqrt, st, [st])
            c.op("dve", lambda e: e.reciprocal(out=st.ap[:, 0:1], in_=st.ap[:, 0:1]), [st], [st])
            c.ts("dve", xs.ap, x.ap, st.ap[:, 0:1], None, ALU.mult, None, xs, [x, st])
            for q in range(4):
                ps = c.bank(4 + q % 2)
                for jj in range(4):
                    j = q * 4 + jj
                    c.tr(ps.ap[:, jj * 128:(jj + 1) * 128], xs.ap[:, j * 128:(j + 1) * 128], c.ident.ap, ps, [xs, c.ident])
                for jj in range(4):
                    j = q * 4 + jj
                    c.act(f32v[:, j, :], ps.ap[:, jj * 128:(jj + 1) * 128], AF.Identity, f32, [ps, scl2, c.mT],
                          bias=mTv[:, 48 + j, 0:1], scale=scl2.ap[:, j:j + 1])
            c.copy("pool", fbv[:, :, ti * 128:(ti + 1) * 128], f32v, fb, [f32])
            ps = c.bank(6)
            for j in range(16):
                c.mm(ps.ap[:, 0:32], f32v[:, j, :], rwv[:, j, :], ps, [f32, rw], start=(j == 0), stop=False)
            c.mm(ps.ap[:, 0:32], c.onesrow.ap[0:1, :], rb.ap[0:1, :], ps, [c.onesrow, rb], start=False, stop=True)
            c.copy("act", lg.ap, ps.ap[:, 0:32], lg, [ps])
            c.op("dve", lambda e: e.max(out=mx.ap, in_=lg.ap), [lg], [mx])
            c.ts("dve", mk.ap, lg.ap, mx.ap[:, 3:4], None, ALU.is_ge, None, mk, [lg, mx])
            c.ts("dve", mx.ap[:, 4:5], mx.ap[:, 0:1], -1.0, None, ALU.mult, None, mx, [mx])
            c.act(ex.ap, lg.ap, AF.Exp, ex, [lg, mx], bias=mx.ap[:, 4:5])
            c.tt("dve", ex.ap, ex.ap, mk.ap, ALU.mult, ex, [ex, mk])
            c.op("dve", lambda e: e.tensor_reduce(out=mx.ap[:, 5:6], in_=ex.ap, axis=AX.X, op=ALU.add), [ex], [mx])
            c.op("dve", lambda e: e.reciprocal(out=mx.ap[:, 5:6], in_=mx.ap[:, 5:6]), [mx], [mx])
            c.ts("dve", ex.ap, ex.ap, mx.ap[:, 5:6], None, ALU.mult, None, ex, [ex, mx])
            ps2 = c.bank(7)
            c.tr(ps2.ap[0:32, 0:128], ex.ap, c.ident.ap, ps2, [ex, c.ident])
            c.copy("act", gb.ap[0:32, ti * 128:(ti + 1) * 128], ps2.ap[0:32, 0:128], gb, [ps2])
        c.dma("act", ftdst[:, :, blk * TBK:(blk + 1) * TBK], fbv, c.FTk, fb)
        c.dma("act", GT[:, blk * TBK:(blk + 1) * TBK], gb.ap[0:32, :], c.GTk, gb)
    c.stage_end()


def stage_moe(c):
    cfg, T = c.cfg, c.T
    mTv = c.mT.ap.rearrange("p (o r) -> p o r", r=2)
    X1, FT, GT = c.X1.ap(), c.FT.ap(), c.GT.ap()
    NJ = cfg.dexp // 128
    TB = min(1024, cfg.nown)
    NH = (TB + 511) // 512
    HW = TB // NH
    nrow_b = NEXP * 2 * NJ
    bgu = c.alloc("bguT", nrow_b)
    bdn = c.alloc("bdn", D)
    gt2bc = c.alloc("gt2bc", D)
    fgbc = c.alloc("fgbc", D)
    base = c.top
    bsrc = T["exp_bgu"].ap()
    for r0 in range(0, nrow_b, 128):
        n = min(128, nrow_b - r0)
        c.top = base
        tmpc = load_colvec(c, bsrc[r0:r0 + n, :], c.Dk["exp_bgu"], n, f"bgu{r0}")
        c.copy("dve", bgu.ap[:, r0:r0 + n], tmpc.ap, bgu, [tmpc])
        c.p.barrier()
    c.top = base
    c.dma("sp", bdn.ap[0:32, :], T["exp_bdn"].ap(), bdn, c.Dk["exp_bdn"])
    dg = c.alloc("dgscr", D)
    bcast_rows(c, mTv[:, 80:96, 0], "gt2bc", res=gt2bc, dg=dg)
    bcast_rows(c, c.fgT.ap, "fgbc", bank0=4, res=fgbc, dg=dg)
    c.p.barrier()
    c.top = base
    for blk in range(cfg.nown // TB):
        c.p.barrier()
        c.top = base
        t0 = blk * TB
        ft = c.alloc("m_ft", 16 * TB, BF16)
        ftv = ft.ap.rearrange("p (kc t) -> p kc t", kc=16)
        yacc = c.alloc("m_yacc", 16 * TB)
        yav = yacc.ap.rearrange("p (cc t) -> p cc t", cc=16)
        base2 = c.top
        gts = c.alloc("m_gt", TB)
        actT = c.alloc("m_act", NJ * TB, BF16)
        acv = actT.ap.rearrange("p (j t) -> p j t", j=NJ)
        gbc = c.alloc("m_gbc", TB)
        gl = c.alloc("m_gl", HW); ln = c.alloc("m_ln", HW); sg = c.alloc("m_sg", HW)
        wg = [[c.alloc(f"m_wg{k}{i}", 16 * 128, BF16) for i in range(2)] for k in range(2)]
        wd = [c.alloc(f"m_wd{i}", D, BF16) for i in range(2)]
        c.dma("sp", ftv, FT.rearrange("(kc p) t -> p kc t", p=128)[:, :, t0:t0 + TB], ft, c.FTk)
        c.dma("sp", gts.ap[0:32, :], GT[:, t0:t0 + TB], gts, c.GTk)
        for cc in range(16):
            for h in range(NH):
                ps = c.bank((cc * NH + h) % 4)
                c.mm(ps.ap[:, 0:HW], bdn.ap[0:32, cc * 128:(cc + 1) * 128], gts.ap[0:32, h * HW:(h + 1) * HW], ps, [bdn, gts])
                c.copy("act", yav[:, cc, h * HW:(h + 1) * HW], ps.ap[:, 0:HW], yacc, [ps])
        wi = 0
        di = 0
        for e in range(NEXP):
            for h in range(NH):
                ps = c.bank(4 + h)
                c.mm(ps.ap[:, 0:HW], c.ident.ap[0:32, e:e + 1].to_broadcast([32, 128]), gts.ap[0:32, h * HW:(h + 1) * HW], ps,
                     [c.ident, gts])
                c.copy("act", gbc.ap[:, h * HW:(h + 1) * HW], ps.ap[:, 0:HW], gbc, [ps])
            wsrc = T["exp_wgu"].ap()[e].rearrange("(kc p) n -> p kc n", p=128)
            for j in range(NJ):
                wt = []
                for kind in range(2):
                    w = wg[kind][wi % 2]
                    wv = w.ap.rearrange("p (kc n) -> p kc n", kc=16)
                    c.dma("pool", wv, wsrc[:, :, kind * cfg.dexp + j * 128:kind * cfg.dexp + (j + 1) * 128], w, c.Dk["exp_wgu"])
                    wt.append((w, wv))
                wi += 1
                bcol = e * 2 * NJ + j
                for h in range(NH):
                    hs = slice(h * HW, (h + 1) * HW)
                    pg, pl = c.bank(2 * h), c.bank(2 * h + 1)
                    for kind, ps in ((0, pg), (1, pl)):
                        w, wv = wt[kind]
                        for kc in range(16):
                            c.mm(ps.ap[:, 0:HW], wv[:, kc, :], ftv[:, kc, hs], ps, [w, ft], start=(kc == 0), stop=(kc == 15))
                    c.ts("dve", gl.ap, pg.ap[:, 0:HW], bgu.ap[:, bcol:bcol + 1], 7.0, ALU.add, ALU.min, gl, [pg, bgu])
                    c.act(sg.ap, gl.ap, AF.Sigmoid, sg, [gl], scale=1.702)
                    c.ts("dve", ln.ap, pl.ap[:, 0:HW], bgu.ap[:, bcol + NJ:bcol + NJ + 1], 7.0, ALU.add, ALU.min, ln, [pl, bgu])
                    c.ts("dve", ln.ap, ln.ap, -7.0, 1.0, ALU.max, ALU.add, ln, [ln])
                    c.tt("dve", gl.ap, gl.ap, sg.ap, ALU.mult, gl, [gl, sg])
                    c.tt("dve", gl.ap, gl.ap, ln.ap, ALU.mult, gl, [gl, ln])
                    c.tt("dve", acv[:, j, hs], gl.ap, gbc.ap[:, hs], ALU.mult, actT, [gl, gbc])
            dsrc = T["exp_wdn"].ap()[e].rearrange("(jc p) n -> p jc n", p=128)
            for cc in range(16):
                w = wd[di % 2]
                di += 1
                wv = w.ap.rearrange("p (jc n) -> p jc n", jc=16)
                c.dma("pool", wv[:, 0:NJ, :], dsrc[:, :, cc * 128:(cc + 1) * 128], w, c.Dk["exp_wdn"])
                for h in range(NH):
                    ps = c.bank(4 + (cc * NH + h) % 4)
                    for j in range(NJ):
                        c.mm(ps.ap[:, 0:HW], wv[:, j, :], acv[:, j, h * HW:(h + 1) * HW], ps, [w, actT],
                             start=(j == 0), stop=(j == NJ - 1))
                    c.tt("dve", yav[:, cc, h * HW:(h + 1) * HW], yav[:, cc, h * HW:(h + 1) * HW], ps.ap[:, 0:HW], ALU.add, yacc, [yacc, ps])
        c.p.barrier()
        c.top = base2
        x1t = [c.alloc(f"fx{i}", D) for i in range(2)]
        tmp = c.alloc("ftmp", 512)
        junk = c.alloc("fjunk", D, BF16)
        st = c.alloc("fst", 8)
        for ti in range(TB // 128):
            tok0 = t0 + ti * 128
            x = x1t[ti % 2]
            c.dma("sp", x.ap, X1[tok0:tok0 + 128, :], x, c.X1k)
            for q in range(4):
                ps = c.bank(q)
                for jj in range(4):
                    cc = q * 4 + jj
                    c.tr(ps.ap[:, jj * 128:(jj + 1) * 128], yav[:, cc, ti * 128:(ti + 1) * 128], c.ident.ap, ps, [yacc, c.ident])
                c.tt("dve", tmp.ap, ps.ap, gt2bc.ap[:, q * 512:(q + 1) * 512], ALU.mult, tmp, [ps, gt2bc])
                c.tt("dve", x.ap[:, q * 512:(q + 1) * 512], x.ap[:, q * 512:(q + 1) * 512], tmp.ap, ALU.add, x, [x, tmp])
            c.act(junk.ap, x.ap, AF.Square, junk, [x], accum=st.ap[:, 0:1], accb=st)
            c.ts("dve", st.ap[:, 0:1], st.ap[:, 0:1], 1.0 / D, 1e-5, ALU.mult, ALU.add, st, [st, junk])
            c.act(st.ap[:, 0:1], st.ap[:, 0:1], AF.Sqrt, st, [st])
            c.op("dve", lambda e: e.reciprocal(out=st.ap[:, 0:1], in_=st.ap[:, 0:1]), [st], [st])
            c.stt("dve", x.ap, x.ap, st.ap[:, 0:1], fgbc.ap, ALU.mult, ALU.mult, x, [x, st, fgbc])
            c.dma("act", c.out.ap()[tok0:tok0 + 128, :], x.ap, c.Dk["out"], x)
    c.stage_end()


def make_inputs(cfg, b, inp, q=0):
    inp = {k: np.asarray(v) for k, v in inp.items()}
    f = lambda a: np.ascontiguousarray(a, dtype=np.float32)
    cc = np.stack([inp["c"][b], inp["c_ctx"]]).reshape(32, 128)
    return {
        "x": f(inp["x"][b]), "x_own": f(inp["x"][b, q * cfg.nown:(q + 1) * cfg.nown]), "ctx": f(inp["ctx"][b]), "cc": f(cc),
        "mod_w": f(inp["mod_w"][0]), "mod_b": f(inp["mod_b"][0].reshape(96, 128)),
        "norm1_g": f(inp["norm1_g"][0].reshape(16, 128)), "w_in": f(inp["w_in"][0]),
        "rw_mu": f(np.pad(inp["rw_mu"][0], ((0, 0), (0, 28 * 128 - RWC))).reshape(4 * 28, 128)),
        "s5_are": f(inp["s5_a_re"][0].reshape(64, 128)), "s5_aim": f(inp["s5_a_im"][0].reshape(64, 128)),
        "s5_ldt": f(np.repeat(inp["s5_log_dt"][0].reshape(2, 32, 2, 1), 64, axis=3).reshape(64, 128)),
        "s5_d": f(inp["s5_d"][0].reshape(8, 128)), "s5_glub": f(inp["s5_glu_b"][0].reshape(8, 128)),
        "s5_bre": f(inp["s5_b_re"][0].reshape(-1, 16)), "s5_bim": f(inp["s5_b_im"][0].reshape(-1, 16)),
        "s5_cre": f(inp["s5_c_re"][0].reshape(-1, 64)), "s5_cim": f(inp["s5_c_im"][0].reshape(-1, 64)),
        "s5_gluw": f(inp["s5_glu_w"][0].reshape(1024, 16)),
        "rw_w0": f(inp["rw_w0"][0].reshape(16, 128)), "rw_a0": f(inp["rw_a0"][0].reshape(16, 128)),
        "rw_w2": f(inp["rw_w2"][0].reshape(128, 1024)), "rw_a2": f(inp["rw_a2"][0].reshape(128, 1024)),
        "rw_g2": f(inp["rw_g2"][0]), "rw_kk": f(inp["rw_k_k"][0].reshape(8, 128)), "rw_ka": f(inp["rw_k_a"][0].reshape(8, 128)),
        "rw_rk": f(inp["rw_r_k"][0].reshape(8, 128)), "rw_lnw": f(inp["rw_ln_w"][0].reshape(1, 1024)),
        "rw_lnb": f(inp["rw_ln_b"][0].reshape(1, 1024)),
        "w_out": f(inp["w_out"][0]), "norm2_g": f(inp["norm2_g"][0].reshape(16, 128)),
        "router_w": f(inp["router_w"][0]), "router_b": f(inp["router_b"][0].reshape(1, 32)),
        "final_g": f(inp["final_g"].reshape(16, 128)),
        "exp_wgu": f(inp["exp_w_gu"][0]), "exp_bgu": f(inp["exp_b_gu"][0].reshape(-1, 128)),
        "exp_wdn": f(inp["exp_w_dn"][0]), "exp_bdn": f(inp["exp_b_dn"][0]),
    }


_NC_CACHE = {}


def kernel(**inp):
    cfg = Cfg()
    if "nc" not in _NC_CACHE:
        _NC_CACHE["nc"] = build(cfg)
    nc = _NC_CACHE["nc"]
    nq = cfg.nq
    shared = {}
    in_maps = []
    for i in range(2 * nq):
        b, q = i // nq, i % nq
        m = make_inputs(cfg, b, inp, q)
        for k, v in m.items():
            if k in shared and shared[k].shape == v.shape and (shared[k] is v or np.shares_memory(shared[k], v)):
                m[k] = shared[k]
            else:
                shared.setdefault(k, v)
        in_maps.append(m)
    res = run_bass_kernel_spmd(nc, in_maps, core_ids=list(range(2 * nq)))
    out = np.empty((2, cfg.seq, D), np.float32)
    for i in range(2 * nq):
        b, q = i // nq, i % nq
        out[b, q * cfg.nown:(q + 1) * cfg.nown] = res.results[i]["out"]
    return out
```

```python
import math
from contextlib import ExitStack

import numpy as np
import concourse.bass as bass
import concourse.mybir as mybir
from concourse.bass_utils import run_bass_kernel_spmd

F32 = mybir.dt.float32
BF16 = mybir.dt.bfloat16
ALU = mybir.AluOpType
AF = mybir.ActivationFunctionType
AX = mybir.AxisListType

D = 2048
KC = D // 128
S5W = 1024
RWW = 1024
RWC = 3 * RWW + 128 + 128 + 160
INC = S5W + RWC
NEXP = 32
CS = 64
NSUB = 128 // CS
ARENA = 52800


class Cfg:
    def __init__(self, nrow=64, ctx=256, dexp=2048, stages=99, nq=4):
        self.nrow = nrow
        self.seq = nrow * 64
        self.ctx = ctx
        self.ntok = self.seq + ctx
        self.npos = self.seq + 2 * ctx
        self.dexp = dexp
        self.stages = stages
        self.nq = nq
        self.nown = self.seq // nq


ENGS = ("sp", "act", "dve", "pool", "pe")
NDSEM = 6
SELF_SYNC = {"pool", "dve", "act"}


class Op:
    __slots__ = ("eng", "fn", "deps", "sig", "sem", "val", "dma", "prev_dma")

    def __init__(self, eng, fn, dma):
        self.eng, self.fn, self.dma = eng, fn, dma
        self.deps = []
        self.sig = dma
        self.sem = None
        self.val = 0
        self.prev_dma = None


class Prog:
    def __init__(self, nc, es):
        self.nc = nc
        self.ops = {e: [] for e in ENGS}
        self.tab = {}
        self.csem = {e: es.enter_context(nc.semaphore("c_" + e)) for e in ENGS}
        self.dsem = {e: [es.enter_context(nc.semaphore(f"d_{e}{i}")) for i in range(NDSEM)]
                     for e in ("sp", "act", "pool")}
        self.dcnt = {e: [0] * NDSEM for e in self.dsem}
        self.dlast = {e: [None] * NDSEM for e in self.dsem}
        self.drr = {e: 0 for e in self.dsem}

    def _entries(self, key):
        name, sub = key
        t = self.tab.setdefault(name, {})
        if sub is None:
            return list(t.values())
        out = []
        if sub in t:
            out.append(t[sub])
        if None in t:
            out.append(t[None])
        return out

    def add(self, eng, fn, reads=(), writes=(), dma=False):
        op = Op(eng, fn, dma)
        deps = []
        for k in reads:
            for ent in self._entries(k):
                if ent[0] is not None:
                    deps.append(ent[0])
        for k in writes:
            for ent in self._entries(k):
                if ent[0] is not None:
                    deps.append(ent[0])
                deps.extend(ent[1].values())
        if dma:
            i = self.drr[eng]
            self.drr[eng] = (i + 1) % NDSEM
            op.sem = self.dsem[eng][i]
            self.dcnt[eng][i] += 1
            op.val = 16 * self.dcnt[eng][i]
            op.prev_dma = self.dlast[eng][i]
            self.dlast[eng][i] = op
            if op.prev_dma is not None:
                deps.append(op.prev_dma)
        seen = set()
        for d in deps:
            if d is op or id(d) in seen:
                continue
            seen.add(id(d))
            if (not d.dma) and d.eng == eng and eng not in SELF_SYNC:
                continue
            d.sig = True
            op.deps.append(d)
        for k in writes:
            name, sub = k
            t = self.tab.setdefault(name, {})
            if sub is None:
                t.clear()
            t[sub] = [op, {}]
        for k in reads:
            name, sub = k
            t = self.tab.setdefault(name, {})
            if sub not in t:
                t[sub] = [None, {}]
            t[sub][1][eng if not dma else (eng, id(op))] = op
        self.ops[eng].append(op)
        return op

    def barrier(self):
        lasts = []
        for e in ENGS:
            for o in reversed(self.ops[e]):
                if o.fn is not None and not o.dma:
                    lasts.append(o)
                    break
        for e in self.dsem:
            for d in self.dlast[e]:
                if d is not None:
                    lasts.append(d)
        for e in ENGS:
            op = Op(e, None, False)
            for d in lasts:
                if (not d.dma) and d.eng == e:
                    continue
                d.sig = True
                op.deps.append(d)
            self.ops[e].append(op)
        self.tab = {}

    def emit(self):
        nc = self.nc
        for e in ENGS:
            n = 0
            for op in self.ops[e]:
                if not op.dma and op.sig:
                    n += 1
                    op.sem = self.csem[e]
                    op.val = n
        with nc.Block() as block:
            def run(e, eng):
                seen = {}
                for op in self.ops[e]:
                    need = {}
                    for d in op.deps:
                        k = id(d.sem)
                        if d.val > seen.get(k, 0) and d.val > need.get(k, (None, 0))[1]:
                            need[k] = (d.sem, d.val)
                    for k, (s, v) in need.items():
                        eng.wait_ge(s, v)
                        seen[k] = v
                    if op.fn is not None:
                        ins = op.fn(eng)
                        if op.sig:
                            ins.then_inc(op.sem, 16 if op.dma else 1)

            @block.sync
            def _(eng):
                run("sp", eng)

            @block.scalar
            def _(eng):
                run("act", eng)

            @block.vector
            def _(eng):
                run("dve", eng)

            @block.gpsimd
            def _(eng):
                run("pool", eng)

            @block.tensor
            def _(eng):
                run("pe", eng)


class B:
    __slots__ = ("ap", "key")

    def __init__(self, ap, key):
        self.ap, self.key = ap, key


class Ctx:
    def __init__(self, nc, es, cfg):
        self.nc, self.cfg = nc, cfg
        self.p = Prog(nc, es)
        self.arena = es.enter_context(nc.sbuf_tensor("arena", [128, ARENA], F32))
        self.psum = es.enter_context(nc.psum_tensor("psum", [128, 4096], F32))
        self.top = 0
        self.uid = 0
        self.pers = 0

    def alloc(self, name, n, dt=F32):
        words = n if dt == F32 else (n + 1) // 2
        words = (words + 7) // 8 * 8
        assert self.top + words <= ARENA, (name, self.top, words)
        ap = self.arena[:, self.top:self.top + words]
        if dt != F32:
            ap = ap.bitcast(dt)[:, 0:n]
        else:
            ap = ap[:, 0:n]
        self.top += words
        self.uid += 1
        return B(ap, (f"{name}#{self.uid}", None))

    def bank(self, i, name=None):
        return B(self.psum[:, i * 512:(i + 1) * 512], (f"bank{i}", None))

    def stage_end(self):
        self.p.barrier()
        self.top = self.pers

    def op(self, eng, fn, reads, writes, dma=False):
        return self.p.add(eng, fn, [b.key for b in reads], [b.key for b in writes], dma)

    def dma(self, q, out, in_, outb, inb, **kw):
        return self.op(q, lambda e: e.dma_start(out=out, in_=in_, **kw), [inb], [outb], dma=True)

    def mm(self, out, lhsT, rhs, outb, inbs, start=True, stop=True):
        return self.op("pe", lambda e: e.matmul(out, lhsT, rhs, start=start, stop=stop), inbs,
                       [outb] if start else [outb])

    def tr(self, out, in_, ident, outb, inbs):
        return self.op("pe", lambda e: e.transpose(out, in_, ident), inbs, [outb])

    def act(self, out, in_, func, outb, inbs, bias=None, scale=None, accum=None, accb=None, eng="act"):
        kw = {}
        if bias is not None:
            kw["bias"] = bias
        if scale is not None:
            kw["scale"] = scale
        if accum is not None:
            kw["accum_out"] = accum
        return self.op(eng, lambda e: e.activation(out=out, in_=in_, func=func, **kw), inbs,
                       [outb] + ([accb] if accb is not None else []))

    def ts(self, eng, out, in0, s1, s2, op0, op1, outb, inbs):
        if op1 is None:
            return self.op(eng, lambda e: e.tensor_scalar(out=out, in0=in0, scalar1=s1, scalar2=None, op0=op0),
                           inbs, [outb])
        return self.op(eng, lambda e: e.tensor_scalar(out=out, in0=in0, scalar1=s1, scalar2=s2, op0=op0, op1=op1),
                       inbs, [outb])

    def tt(self, eng, out, in0, in1, op, outb, inbs):
        return self.op(eng, lambda e: e.tensor_tensor(out=out, in0=in0, in1=in1, op=op), inbs, [outb])

    def stt(self, eng, out, in0, scalar, in1, op0, op1, outb, inbs):
        return self.op(eng, lambda e: e.scalar_tensor_tensor(out=out, in0=in0, scalar=scalar, in1=in1,
                                                             op0=op0, op1=op1), inbs, [outb])

    def copy(self, eng, out, in_, outb, inbs):
        if eng == "act":
            return self.op(eng, lambda e: e.copy(out=out, in_=in_), inbs, [outb])
        return self.op(eng, lambda e: e.tensor_copy(out=out, in_=in_), inbs, [outb])

    def memset(self, eng, ap, val, outb):
        return self.op(eng, lambda e: e.memset(ap, val), [], [outb])


def dram_in(nc, name, shape):
    return nc.dram_tensor(name, list(shape), F32, kind="ExternalInput")


def build(cfg):
    nc = bass.Bass("TRN2", target_bir_lowering=False)
    es = ExitStack()
    T = {}
    shapes = input_shapes(cfg)
    for k, s in shapes.items():
        T[k] = dram_in(nc, k, s)
    out = nc.dram_tensor("out", [cfg.nown, D], F32, kind="ExternalOutput")
    with es:
        c = Ctx(nc, es, cfg)
        c.T = T
        c.out = out
        c.Dk = {k: B(None, ("dram_" + k, None)) for k in T}
        c.Dk["out"] = B(None, ("dram_out", None))
        c.P = nc.dram_tensor("P_s", [INC, cfg.npos], F32, kind="Internal")
        c.Pk = B(None, ("P_s", None))
        stage_consts(c)
        stage_adaln(c)
        nch = (RWC + 127) // 128
        c.ZS = nc.dram_tensor("ZS_s", [nch * 128, cfg.npos], F32, kind="Internal")
        c.ZSk = B(None, ("ZS_s", None))
        c.YM = nc.dram_tensor("YM_s", [D, cfg.seq], BF16, kind="Internal")
        c.YMk = B(None, ("YM_s", None))
        if cfg.stages >= 1:
            stage_inproj(c)
        if cfg.stages >= 2:
            stage_shift(c)
        if cfg.stages >= 3:
            stage_s5(c)
        c.LO = nc.dram_tensor("LO_s", [5, 8, 128, cfg.npos], F32, kind="Internal")
        c.LOk = B(None, ("LO_s", None))
        c.RW = nc.dram_tensor("RW_s", [2, 8, 128, 6, cfg.npos], F32, kind="Internal")
        c.RWk = B(None, ("RW_s", None))
        c.YD = nc.dram_tensor("YD_s", [2, cfg.seq, 1024], F32, kind="Internal")
        c.YDk = B(None, ("YD_s", None))
        if cfg.stages >= 4:
            stage_rw_lora(c)
        if cfg.stages >= 5:
            stage_rw(c)
        c.qv = nc.partition_id() % cfg.nq
        c.X1 = nc.dram_tensor("X1_s", [cfg.nown, D], F32, kind="Internal")
        c.X1k = B(None, ("X1_s", None))
        c.FT = nc.dram_tensor("FT_s", [D, cfg.nown], BF16, kind="Internal")
        c.FTk = B(None, ("FT_s", None))
        c.GT = nc.dram_tensor("GT_s", [32, cfg.nown], F32, kind="Internal")
        c.GTk = B(None, ("GT_s", None))
        if cfg.stages >= 6:
            stage_outproj(c)
        if cfg.stages >= 7:
            stage_moe(c)
        c.p.barrier()
        c.p.emit()
    return nc


def input_shapes(cfg):
    return {
        "x": (cfg.seq, D), "x_own": (cfg.nown, D), "ctx": (cfg.ctx, D), "cc": (32, 128),
        "mod_w": (D, 6 * D), "mod_b": (96, 128), "norm1_g": (16, 128), "w_in": (D, INC),
        "rw_mu": (4 * 28, 128),
        "s5_are": (64, 128), "s5_aim": (64, 128), "s5_ldt": (64, 128), "s5_d": (8, 128), "s5_glub": (8, 128),
        "s5_bre": (2 * 64 * 64, 16), "s5_bim": (2 * 64 * 64, 16), "s5_cre": (2 * 64 * 16, 64), "s5_cim": (2 * 64 * 16, 64),
        "s5_gluw": (1024, 16),
        "rw_w0": (16, 128), "rw_a0": (16, 128), "rw_w2": (128, 1024), "rw_a2": (128, 1024), "rw_g2": (160, 1024),
        "rw_kk": (8, 128), "rw_ka": (8, 128), "rw_rk": (8, 128), "rw_lnw": (1, 1024), "rw_lnb": (1, 1024),
        "w_out": (D, D), "norm2_g": (16, 128), "router_w": (D, 32), "router_b": (1, 32), "final_g": (16, 128),
        "exp_wgu": (NEXP, D, 2 * cfg.dexp), "exp_bgu": (NEXP * 2 * cfg.dexp // 128, 128),
        "exp_wdn": (NEXP, cfg.dexp, D), "exp_bdn": (NEXP, D),
    }


def stage_consts(c):
    nc = c.nc
    ident = c.alloc("ident", 128)
    c.ident = ident
    c.memset("pool", ident.ap, 1.0, ident)
    c.op("pool", lambda e: e.affine_select(out=ident.ap, in_=ident.ap, pattern=[[-1, 128]],
                                           compare_op=ALU.is_equal, fill=0.0, base=0, channel_multiplier=1),
         [ident], [ident])
    onesrow = c.alloc("onesrow", 128)
    c.memset("pool", onesrow.ap, 1.0, onesrow)
    c.onesrow = onesrow
    ones128 = c.alloc("ones128", 128)
    c.memset("pool", ones128.ap, 1.0, ones128)
    c.ones128 = ones128
    c.pers = c.top


def load_colvec(c, src_ap, srcb, nrows, name, bank=7):
    tmp = c.alloc(name + "_r", 128)
    res = c.alloc(name, nrows)
    c.dma("sp", tmp.ap[0:nrows, :], src_ap, tmp, srcb)
    pb = c.bank(bank)
    c.tr(pb.ap[:, 0:nrows], tmp.ap[0:nrows, :], c.ident.ap[0:nrows, 0:nrows], pb, [tmp, c.ident])
    c.copy("dve", res.ap, pb.ap[:, 0:nrows], res, [pb])
    return res


def stage_adaln(c):
    T = c.T
    mT = c.alloc("mT", 96 * 2)
    g1 = load_colvec(c, T["norm1_g"].ap(), c.Dk["norm1_g"], 16, "g1T")
    c.mT, c.g1 = mT, g1
    scl1 = c.alloc("scl1", 32)
    c.scl1 = scl1
    c.fgT = load_colvec(c, T["final_g"].ap(), c.Dk["final_g"], 16, "fgT")
    c.pers = c.top
    bT = load_colvec(c, T["mod_b"].ap(), c.Dk["mod_b"], 96, "modbT")
    craw = c.alloc("craw", 128)
    c.dma("sp", craw.ap[0:32, :], T["cc"].ap(), craw, c.Dk["cc"])
    c.act(craw.ap[0:32, :], craw.ap[0:32, :], AF.Silu, craw, [craw])
    pb = c.bank(6)
    c.tr(pb.ap[:, 0:32], craw.ap[0:32, :], c.ident.ap[0:32, 0:32], pb, [craw, c.ident])
    sT = c.alloc("siluT", 32)
    c.copy("dve", sT.ap, pb.ap[:, 0:32], sT, [pb])
    sTv = sT.ap.rearrange("p (r j) -> p j r", r=2)
    mTv = mT.ap.rearrange("p (o r) -> p o r", r=2)
    wbuf = [c.alloc(f"modw{i}", 16 * 512) for i in range(2)]
    mw = T["mod_w"].ap().rearrange("(kc p) n -> p kc n", p=128)
    for blk in range(24):
        wb = wbuf[blk % 2]
        wv = wb.ap.rearrange("p (kc n) -> p kc n", kc=16)
        c.dma("sp", wv, mw[:, :, blk * 512:(blk + 1) * 512], wb, c.Dk["mod_w"])
        ps = c.bank(blk % 2)
        for o in range(4):
            for kc in range(16):
                c.mm(ps.ap[:, o * 2:o * 2 + 2], wv[:, kc, o * 128:(o + 1) * 128], sTv[:, kc, :], ps, [wb, sT],
                     start=(kc == 0), stop=(kc == 15))
        o0 = blk * 4
        c.tt("dve", mTv[:, o0:o0 + 4, :], ps.ap[:, 0:8].rearrange("p (o r) -> p o r", r=2),
             bT.ap[:, o0:o0 + 4].unsqueeze(2).to_broadcast([128, 4, 2]), ALU.add, mT, [ps, bT])
    for r in range(2):
        c.stt("dve", scl1.ap[:, r * 16:(r + 1) * 16], mTv[:, 16:32, r], 1.0, g1.ap, ALU.add, ALU.mult,
              scl1, [mT, g1])
    c.stage_end()


def stage_inproj(c):
    cfg, T = c.cfg, c.T
    mTv = c.mT.ap.rearrange("p (o r) -> p o r", r=2)
    ntile = cfg.ntok // 128
    nct = cfg.ctx // 128
    xt = [c.alloc(f"xt{i}", D) for i in range(2)]
    junk = c.alloc("junk", D, BF16)
    ssq = c.alloc("ssq", 8)
    TBA = 1024
    xnT = c.alloc("xnT", 16 * TBA, BF16)
    xnv = xnT.ap.rearrange("p (kc n) -> p kc n", kc=16)
    wgrp = [c.alloc(f"wgrp{i}", 16 * 512, BF16) for i in range(2)]
    pst = [c.alloc(f"pst{i}", TBA) for i in range(2)]
    win = T["w_in"].ap().rearrange("(kc p) n -> p kc n", p=128)
    P = c.P.ap()
    TPB = TBA // 128
    nblk = (ntile + TPB - 1) // TPB
    gi = 0
    ci = 0
    for blk in range(nblk):
        t0 = blk * TPB
        nt = min(TPB, ntile - t0)
        for ti in range(nt):
            g = t0 + ti
            isctx = g < nct
            r = 1 if isctx else 0
            src = T["ctx"].ap()[g * 128:(g + 1) * 128, :] if isctx else \
                T["x"].ap()[(g - nct) * 128:(g - nct + 1) * 128, :]
            xb = xt[gi % 2]
            gi += 1
            c.dma("sp", xb.ap, src, xb, c.Dk["ctx" if isctx else "x"])
            sq = ssq.ap[:, (g % 8):(g % 8) + 1]
            c.act(junk.ap, xb.ap, AF.Square, junk, [xb], accum=sq, accb=ssq)
            c.op("dve", lambda e, sq=sq: e.tensor_scalar(out=sq, in0=sq, scalar1=1.0 / D, scalar2=1e-5,
                                                         op0=ALU.mult, op1=ALU.add), [junk, ssq], [ssq])
            c.act(sq, sq, AF.Sqrt, ssq, [ssq])
            c.op("dve", lambda e, sq=sq: e.reciprocal(out=sq, in_=sq), [ssq], [ssq])
            c.op("dve", lambda e, sq=sq, xb=xb: e.tensor_scalar(out=xb.ap, in0=xb.ap, scalar1=sq, scalar2=None,
                                                                op0=ALU.mult), [ssq, xb], [xb])
            for q in range(4):
                ps = c.bank(q + 4 * (g % 2))
                for jj in range(4):
                    j = q * 4 + jj
                    c.tr(ps.ap[:, jj * 128:(jj + 1) * 128], xb.ap[:, j * 128:(j + 1) * 128], c.ident.ap,
                         ps, [xb, c.ident])
                for jj in range(4):
                    j = q * 4 + jj
                    c.act(xnv[:, j, ti * 128:(ti + 1) * 128], ps.ap[:, jj * 128:(jj + 1) * 128], AF.Identity,
                          xnT, [ps, c.scl1, c.mT], bias=mTv[:, j, r:r + 1], scale=c.scl1.ap[:, r * 16 + j:r * 16 + j + 1])
        n = nt * 128
        p0 = t0 * 128
        for cc in range((INC + 127) // 128):
            ncol = min(128, INC - cc * 128)
            po = pst[ci % 2]
            ci += 1
            if cc % 4 == 0:
                wg_ = wgrp[(cc // 4 + blk) % 2]
                wgv = wg_.ap.rearrange("p (kc n) -> p kc n", kc=16)
                gcols = min(512, INC - cc * 128)
                c.dma("pool", wgv[:, :, 0:gcols], win[:, :, cc * 128:cc * 128 + gcols], wg_, c.Dk["w_in"])
            cl = (cc % 4) * 128
            for hf in range((n + 511) // 512):
                h0 = hf * 512
                hn = min(512, n - h0)
                ps = c.bank(2 * (ci % 2) + hf)
                for kc in range(16):
                    c.mm(ps.ap[0:ncol, 0:hn], wgv[:, kc, cl:cl + ncol], xnv[:, kc, h0:h0 + hn], ps, [wg_, xnT],
                         start=(kc == 0), stop=(kc == 15))
                c.copy("dve", po.ap[0:ncol, h0:h0 + hn], ps.ap[0:ncol, 0:hn], po, [ps])
            segs = []
            a, b_ = p0, p0 + n
            if a < cfg.ctx:
                e = min(b_, cfg.ctx)
                segs.append((a, e, a))
                segs.append((a, e, cfg.ctx + cfg.seq + a))
                a = e
            if a < b_:
                segs.append((a, b_, a))
            for (sa, sb, dpos) in segs:
                c.dma("sp", P[cc * 128:cc * 128 + ncol, dpos:dpos + (sb - sa)],
                      po.ap[0:ncol, sa - p0:sb - p0], c.Pk, po)
    c.stage_end()


def regions(cfg):
    return [(0, cfg.ctx, 1, cfg.ctx), (cfg.ctx, cfg.seq, cfg.nrow, 64), (cfg.ctx + cfg.seq, cfg.ctx, 1, cfg.ctx)]


def stage_shift(c):
    cfg, T = c.cfg, c.T
    nch = (RWC + 127) // 128
    muT = load_colvec(c, T["rw_mu"].ap(), c.Dk["rw_mu"], 4 * nch, "muT")
    P, ZS = c.P.ap(), c.ZS.ap()
    zb = [c.alloc(f"z{i}", cfg.npos) for i in range(2)]
    ob = [c.alloc(f"zo{i}", cfg.npos) for i in range(2)]
    tmp = c.alloc("ztmp", cfg.npos)
    for cc in range(nch):
        ncol = min(128, RWC - cc * 128)
        z, o = zb[cc % 2], ob[cc % 2]
        c.dma("sp", z.ap[0:ncol, :], P[S5W + cc * 128:S5W + cc * 128 + ncol, :], z, c.Pk)
        c.copy("act", o.ap[0:ncol, :], z.ap[0:ncol, :], o, [z])
        for (a, ln, rows, w) in regions(cfg):
            zv = z.ap[0:ncol, a:a + ln].rearrange("p (r w) -> p r w", w=w)
            ov = o.ap[0:ncol, a:a + ln].rearrange("p (r w) -> p r w", w=w)
            tv = tmp.ap[0:ncol, a:a + ln].rearrange("p (r w) -> p r w", w=w)
            pairs = [(0, (slice(None), slice(1, w)), (slice(None), slice(0, w - 1))),
                     (1, (slice(None), slice(0, w - 1)), (slice(None), slice(1, w)))]
            if rows > 1:
                pairs += [(2, (slice(1, rows), slice(None)), (slice(0, rows - 1), slice(None))),
                          (3, (slice(0, rows - 1), slice(None)), (slice(1, rows), slice(None)))]
            for (j, dst, src) in pairs:
                mu = muT.ap[0:ncol, j * nch + cc:j * nch + cc + 1]
                c.tt("dve", tv[:, dst[0], dst[1]], zv[:, src[0], src[1]], zv[:, dst[0], dst[1]], ALU.subtract,
                     tmp, [z])
                c.stt("dve", ov[:, dst[0], dst[1]], tv[:, dst[0], dst[1]], mu, ov[:, dst[0], dst[1]],
                      ALU.mult, ALU.add, o, [tmp, muT, o])
        c.dma("pool", ZS[cc * 128:cc * 128 + ncol, :], o.ap[0:ncol, :], c.ZSk, o)
    c.stage_end()


def stage_s5(c):
    cfg, T = c.cfg, c.T
    L = cfg.ntok
    P = c.P.ap()
    CH = next(x for x in (1088, 512, 384, 256, 128) if L % x == 0)
    if getattr(cfg, "s5_ch", None):
        CH = cfg.s5_ch
    NCH = L // CH
    NLV = max(1, int(math.ceil(math.log2(CH))))
    are = load_colvec(c, T["s5_are"].ap(), c.Dk["s5_are"], 64, "are")
    aim = load_colvec(c, T["s5_aim"].ap(), c.Dk["s5_aim"], 64, "aim")
    ldt = load_colvec(c, T["s5_ldt"].ap(), c.Dk["s5_ldt"], 64, "ldt")
    dT = load_colvec(c, T["s5_d"].ap(), c.Dk["s5_d"], 8, "s5dT")
    gbT = load_colvec(c, T["s5_glub"].ap(), c.Dk["s5_glub"], 8, "s5gbT")
    xr = c.alloc("xr", 64); xi = c.alloc("xi", 64); rho = c.alloc("rho", 64)
    t1 = c.alloc("t1", 64); t2 = c.alloc("t2", 64); tq = c.alloc("lb_t", 64)
    ur = c.alloc("u_re", (NLV + 1) * 64); ui = c.alloc("u_im", (NLV + 1) * 64); un = c.alloc("u_nim", (NLV + 1) * 64)
    wr = c.alloc("w_re", 64); wi = c.alloc("w_im", 64); wn = c.alloc("w_nim", 64)
    lbr = c.alloc("lb_re", 64); lbi = c.alloc("lb_im", 64)
    cr = c.alloc("coef_re", 64); ci = c.alloc("coef_im", 64); nci = c.alloc("coef_nim", 64)
    c.act(ldt.ap, ldt.ap, AF.Exp, ldt, [ldt])
    c.tt("dve", xr.ap, are.ap, ldt.ap, ALU.mult, xr, [are, ldt])
    c.tt("dve", xi.ap, aim.ap, ldt.ap, ALU.mult, xi, [aim, ldt])
    c.act(rho.ap, xr.ap, AF.Exp, rho, [xr])
    c.ts("dve", t1.ap, xi.ap, 1.0 / 16, None, ALU.mult, None, t1, [xi])
    c.ts("dve", t2.ap, xi.ap, 1.0 / 16, math.pi / 2, ALU.mult, ALU.add, t2, [xi])
    br_ = c.alloc("lb_r", 64); bi_ = c.alloc("lb_i", 64)
    c.act(bi_.ap, t1.ap, AF.Sin, bi_, [t1])
    c.act(br_.ap, t2.ap, AF.Sin, br_, [t2])

    def csq(dr_, di_, drb, dib, sr_, si_, srb, sib):
        c.tt("dve", t1.ap, sr_, sr_, ALU.mult, t1, [srb])
        c.tt("dve", t2.ap, si_, si_, ALU.mult, t2, [sib])
        c.tt("dve", tq.ap, sr_, si_, ALU.mult, tq, [srb, sib])
        c.tt("dve", dr_, t1.ap, t2.ap, ALU.subtract, drb, [t1, t2])
        c.ts("dve", di_, tq.ap, 2.0, None, ALU.mult, None, dib, [tq])
    for q in range(4):
        if q == 3:
            csq(ur.ap[:, 0:64], ui.ap[:, 0:64], ur, ui, br_.ap, bi_.ap, br_, bi_)
        else:
            csq(br_.ap, bi_.ap, br_, bi_, br_.ap, bi_.ap, br_, bi_)
    for k in range(1, NLV + 1):
        csq(ur.ap[:, k * 64:(k + 1) * 64], ui.ap[:, k * 64:(k + 1) * 64], ur, ui,
            ur.ap[:, (k - 1) * 64:k * 64], ui.ap[:, (k - 1) * 64:k * 64], ur, ui)
    c.ts("dve", un.ap, ui.ap, -1.0, None, ALU.mult, None, un, [ui])
    bits = [k for k in range(NLV + 1) if (CH >> k) & 1]
    c.copy("dve", wr.ap, ur.ap[:, bits[0] * 64:(bits[0] + 1) * 64], wr, [ur])
    c.copy("dve", wi.ap, ui.ap[:, bits[0] * 64:(bits[0] + 1) * 64], wi, [ui])
    for k in bits[1:]:
        a_, b_ = ur.ap[:, k * 64:(k + 1) * 64], ui.ap[:, k * 64:(k + 1) * 64]
        c.tt("dve", t1.ap, wr.ap, a_, ALU.mult, t1, [wr, ur])
        c.tt("dve", t2.ap, wi.ap, b_, ALU.mult, t2, [wi, ui])
        c.tt("dve", tq.ap, wr.ap, b_, ALU.mult, tq, [wr, ui])
        c.tt("dve", wi.ap, wi.ap, a_, ALU.mult, wi, [wi, ur])
        c.tt("dve", wi.ap, wi.ap, tq.ap, ALU.add, wi, [wi, tq])
        c.tt("dve", wr.ap, t1.ap, t2.ap, ALU.subtract, wr, [t1, t2])
    c.ts("dve", wn.ap, wi.ap, -1.0, None, ALU.mult, None, wn, [wi])
    c.tt("dve", lbr.ap, rho.ap, ur.ap[:, 0:64], ALU.mult, lbr, [rho, ur])
    c.tt("dve", lbi.ap, rho.ap, ui.ap[:, 0:64], ALU.mult, lbi, [rho, ui])
    c.ts("dve", t1.ap, lbr.ap, -1.0, None, ALU.add, None, t1, [lbr])
    c.tt("dve", t2.ap, are.ap, are.ap, ALU.mult, t2, [are])
    c.tt("dve", xr.ap, aim.ap, aim.ap, ALU.mult, xr, [aim])
    c.tt("dve", t2.ap, t2.ap, xr.ap, ALU.add, t2, [t2, xr])
    c.op("dve", lambda e: e.reciprocal(out=t2.ap, in_=t2.ap), [t2], [t2])
    c.tt("dve", cr.ap, t1.ap, are.ap, ALU.mult, cr, [t1, are])
    c.tt("dve", xr.ap, lbi.ap, aim.ap, ALU.mult, xr, [lbi, aim])
    c.tt("dve", cr.ap, cr.ap, xr.ap, ALU.add, cr, [cr, xr])
    c.tt("dve", cr.ap, cr.ap, t2.ap, ALU.mult, cr, [cr, t2])
    c.tt("dve", ci.ap, lbi.ap, are.ap, ALU.mult, ci, [lbi, are])
    c.tt("dve", xr.ap, t1.ap, aim.ap, ALU.mult, xr, [t1, aim])
    c.tt("dve", ci.ap, ci.ap, xr.ap, ALU.subtract, ci, [ci, xr])
    c.tt("dve", ci.ap, ci.ap, t2.ap, ALU.mult, ci, [ci, t2])
    c.ts("dve", nci.ap, ci.ap, -1.0, None, ALU.mult, None, nci, [ci])
    u = c.alloc("s5u", cfg.npos)
    yacc = c.alloc("s5y", cfg.seq)
    bb = [c.alloc(f"s5b{k}", L) for k in range(2)]
    mb = [c.alloc(f"s5m{k}", L) for k in range(2)]
    tmpL = c.alloc("s5tmp", L)
    Ec = c.alloc("s5Ec", CH); Es = c.alloc("s5Es", CH)
    rt = c.alloc("s5rt", CH)
    onesC = c.alloc("s5ones", CH)
    c.memset("pool", onesC.ap, 1.0, onesC)
    car = [c.alloc(f"s5car{i}", 4) for i in range(2)]
    braw = [c.alloc(f"braw{i}", 16) for i in range(2)]
    bbar = [c.alloc(f"bbar{i}", 16) for i in range(2)]
    xpad = [c.alloc(f"xpad{i}", 128) for i in range(2)]
    lB = [c.alloc(f"lB{i}", 128) for i in range(2)]
    craw = [c.alloc(f"craw{i}", 128) for i in range(2)]
    lC = [c.alloc(f"lC{i}", 128) for i in range(2)]
    lG = c.alloc("lG", 128)
    gt = c.alloc("s5g", cfg.seq)
    yo = c.alloc("s5o", cfg.seq, BF16)
    nblk_l = (L + 511) // 512
    nblk_s = (cfg.seq + 511) // 512
    bre, bim = T["s5_bre"].ap(), T["s5_bim"].ap()
    cre, cim = T["s5_cre"].ap(), T["s5_cim"].ap()
    gw = T["s5_gluw"].ap()
    YM = c.YM.ap()

    def rev(ap_, lo, n, base):
        hi = base + L - 1 - lo
        end = hi - n
        return ap_[:, hi:(end if end >= 0 else None):-1]

    for ch in range(8):
        c.dma("sp", u.ap, P[ch * 128:(ch + 1) * 128, :], u, c.Pk)
        first = True
        for d in range(2):
            off = 0 if d == 0 else cfg.ctx
            for gpl in range(4):
                gp = ch * 4 + gpl
                col = d * 32 + gp
                rowb = (d * 64 + gp * 2) * 64
                c.dma("sp", braw[0].ap, bre[rowb:rowb + 128, :], braw[0], c.Dk["s5_bre"])
                c.dma("sp", braw[1].ap, bim[rowb:rowb + 128, :], braw[1], c.Dk["s5_bim"])
                crc, cic, ncic = cr.ap[:, col:col + 1], ci.ap[:, col:col + 1], nci.ap[:, col:col + 1]
                c.ts("dve", bbar[0].ap, braw[0].ap, crc, None, ALU.mult, None, bbar[0], [braw[0], cr])
                c.stt("dve", bbar[0].ap, braw[1].ap, ncic, bbar[0].ap, ALU.mult, ALU.add, bbar[0], [braw[1], nci, bbar[0]])
                c.ts("dve", bbar[1].ap, braw[1].ap, crc, None, ALU.mult, None, bbar[1], [braw[1], cr])
                c.stt("dve", bbar[1].ap, braw[0].ap, cic, bbar[1].ap, ALU.mult, ALU.add, bbar[1], [braw[0], ci, bbar[1]])
                for k in range(2):
                    c.memset("pool", xpad[k].ap, 0.0, xpad[k])
                    c.copy("pool", xpad[k].ap[0:64, 32 * gpl:32 * gpl + 16], bbar[k].ap[0:64, :], xpad[k], [bbar[k]])
                    c.copy("pool", xpad[k].ap[64:128, 32 * gpl + 16:32 * gpl + 32], bbar[k].ap[64:128, :], xpad[k], [bbar[k]])
                    ps = c.bank(6 + k)
                    c.tr(ps.ap[:, 0:128], xpad[k].ap, c.ident.ap, ps, [xpad[k], c.ident])
                    c.copy("act", lB[k].ap, ps.ap[:, 0:128], lB[k], [ps])
                rowc = (d * 64 + gp * 2) * 16
                for k, src in ((0, cre), (1, cim)):
                    c.memset("pool", craw[k].ap[0:32, :], 0.0, craw[k])
                    c.dma("sp", craw[k].ap[0:16, 0:64], src[rowc:rowc + 16, :], craw[k], c.Dk["s5_cre" if k == 0 else "s5_cim"])
                    c.dma("sp", craw[k].ap[16:32, 64:128], src[rowc + 16:rowc + 32, :], craw[k], c.Dk["s5_cre" if k == 0 else "s5_cim"])
                    ps = c.bank(6 + k)
                    c.tr(ps.ap[:, 0:32], craw[k].ap[0:32, :], c.ident.ap[0:32, 0:32], ps, [craw[k], c.ident])
                    c.memset("pool", lC[k].ap, 0.0, lC[k])
                    if k == 0:
                        c.copy("act", lC[k].ap[:, 32 * gpl:32 * gpl + 32], ps.ap[:, 0:32], lC[k], [ps])
                    else:
                        c.op("act", lambda e, o=lC[k].ap[:, 32 * gpl:32 * gpl + 32], i=ps.ap[:, 0:32]: e.mul(out=o, in_=i, mul=-1.0),
                             [ps], [lC[k]])
                c.memset("pool", Ec.ap[:, 0:1], 1.0, Ec)
                c.memset("pool", Es.ap[:, 0:1], 0.0, Es)
                for k in range(NLV):
                    n_ = 1 << k
                    cnt = min(n_, CH - n_)
                    if cnt <= 0:
                        break
                    a_ = ur.ap[:, k * 64 + col:k * 64 + col + 1]
                    b_ = ui.ap[:, k * 64 + col:k * 64 + col + 1]
                    nb_ = un.ap[:, k * 64 + col:k * 64 + col + 1]
                    c.ts("dve", Ec.ap[:, n_:n_ + cnt], Ec.ap[:, 0:cnt], a_, None, ALU.mult, None, Ec, [Ec, ur])
                    c.stt("dve", Ec.ap[:, n_:n_ + cnt], Es.ap[:, 0:cnt], nb_, Ec.ap[:, n_:n_ + cnt], ALU.mult, ALU.add, Ec, [Es, un, Ec])
                    c.ts("dve", Es.ap[:, n_:n_ + cnt], Es.ap[:, 0:cnt], a_, None, ALU.mult, None, Es, [Es, ur])
                    c.stt("dve", Es.ap[:, n_:n_ + cnt], Ec.ap[:, 0:cnt], b_, Es.ap[:, n_:n_ + cnt], ALU.mult, ALU.add, Es, [Ec, ui, Es])
                c.ts("dve", rt.ap, onesC.ap, rho.ap[:, col:col + 1], None, ALU.mult, None, rt, [onesC, rho])
                for blk in range(nblk_l):
                    n = min(512, L - blk * 512)
                    for k in range(2):
                        ps = c.bank((blk * 2 + k) % 4)
                        rhs = u.ap[:, off + blk * 512:off + blk * 512 + n] if d == 0 else rev(u.ap, blk * 512, n, off)
                        c.mm(ps.ap[:, 0:n], lB[k].ap, rhs, ps, [lB[k], u])
                        c.copy("act", bb[k].ap[:, blk * 512:blk * 512 + n], ps.ap[:, 0:n], bb[k], [ps])
                v3 = lambda t_: t_.ap.rearrange("p (c j) -> p c j", j=CH)
                Ecb = Ec.ap.unsqueeze(1).to_broadcast([128, NCH, CH])
                Esb = Es.ap.unsqueeze(1).to_broadcast([128, NCH, CH])
                c.tt("dve", v3(tmpL), v3(bb[1]), Esb, ALU.mult, tmpL, [bb[1], Es])
                c.tt("dve", v3(mb[0]), v3(bb[0]), Ecb, ALU.mult, mb[0], [bb[0], Ec])
                c.tt("dve", mb[0].ap, mb[0].ap, tmpL.ap, ALU.add, mb[0], [mb[0], tmpL])
                c.tt("dve", v3(tmpL), v3(bb[0]), Esb, ALU.mult, tmpL, [bb[0], Es])
                c.tt("dve", v3(mb[1]), v3(bb[1]), Ecb, ALU.mult, mb[1], [bb[1], Ec])
                c.tt("dve", mb[1].ap, mb[1].ap, tmpL.ap, ALU.subtract, mb[1], [mb[1], tmpL])
                wrc, wic, wnc = wr.ap[:, col:col + 1], wi.ap[:, col:col + 1], wn.ap[:, col:col + 1]
                for cc in range(NCH):
                    sl = slice(cc * CH, (cc + 1) * CH)
                    cb = car[cc % 2]
                    for k in range(2):
                        init = 0.0 if cc == 0 else cb.ap[:, k:k + 1]
                        c.op("dve", lambda e, o=bb[k].ap[:, sl], i=mb[k].ap[:, sl], init=init: e.tensor_tensor_scan(
                            out=o, data0=rt.ap, data1=i, initial=init, op0=ALU.mult, op1=ALU.add),
                            [mb[k], rt] + ([cb] if cc > 0 else []), [bb[k]])
                    if cc < NCH - 1:
                        nb_ = car[(cc + 1) % 2]
                        lr, li = bb[0].ap[:, (cc + 1) * CH - 1:(cc + 1) * CH], bb[1].ap[:, (cc + 1) * CH - 1:(cc + 1) * CH]
                        c.ts("dve", nb_.ap[:, 2:3], li, wnc, None, ALU.mult, None, nb_, [bb[1], wn])
                        c.stt("dve", nb_.ap[:, 0:1], lr, wrc, nb_.ap[:, 2:3], ALU.mult, ALU.add, nb_, [bb[0], wr, nb_])
                        c.ts("dve", nb_.ap[:, 3:4], lr, wic, None, ALU.mult, None, nb_, [bb[0], wi])
                        c.stt("dve", nb_.ap[:, 1:2], li, wrc, nb_.ap[:, 3:4], ALU.mult, ALU.add, nb_, [bb[1], wr, nb_])
                c.tt("dve", v3(tmpL), v3(bb[1]), Esb, ALU.mult, tmpL, [bb[1], Es])
                c.tt("dve", v3(mb[0]), v3(bb[0]), Ecb, ALU.mult, mb[0], [bb[0], Ec])
                c.tt("dve", mb[0].ap, mb[0].ap, tmpL.ap, ALU.subtract, mb[0], [mb[0], tmpL])
                c.tt("dve", v3(tmpL), v3(bb[0]), Esb, ALU.mult, tmpL, [bb[0], Es])
                c.tt("dve", v3(mb[1]), v3(bb[1]), Ecb, ALU.mult, mb[1], [bb[1], Ec])
                c.tt("dve", mb[1].ap, mb[1].ap, tmpL.ap, ALU.add, mb[1], [mb[1], tmpL])
                for blk in range(nblk_s):
                    n = min(512, cfg.seq - blk * 512)
                    ps = c.bank(4 + blk % 2)
                    for k in range(2):
                        if d == 0:
                            rhs = mb[k].ap[:, cfg.ctx + blk * 512:cfg.ctx + blk * 512 + n]
                        else:
                            hi = L - 1 - blk * 512
                            end = hi - n
                            rhs = mb[k].ap[:, hi:(end if end >= 0 else None):-1]
                        c.mm(ps.ap[:, 0:n], lC[k].ap, rhs, ps, [lC[k], mb[k]], start=(k == 0), stop=(k == 1))
                    ysl = yacc.ap[:, blk * 512:blk * 512 + n]
                    if first:
                        c.copy("dve", ysl, ps.ap[:, 0:n], yacc, [ps])
                    else:
                        c.tt("dve", ysl, ysl, ps.ap[:, 0:n], ALU.add, yacc, [ps, yacc])
                first = False
        ul = u.ap[:, cfg.ctx:cfg.ctx + cfg.seq]
        c.stt("dve", yacc.ap, ul, dT.ap[:, ch:ch + 1], yacc.ap, ALU.mult, ALU.add, yacc, [u, dT, yacc])
        c.tt("dve", gt.ap, yacc.ap, yacc.ap, ALU.mult, gt, [yacc])
        c.ts("dve", gt.ap, gt.ap, 0.044715, 1.0, ALU.mult, ALU.add, gt, [gt])
        c.tt("dve", gt.ap, gt.ap, yacc.ap, ALU.mult, gt, [gt, yacc])
        c.act(gt.ap, gt.ap, AF.Tanh, gt, [gt], scale=0.7978845608028654)
        c.ts("dve", gt.ap, gt.ap, 1.0, 0.5, ALU.add, ALU.mult, gt, [gt])
        c.tt("dve", yacc.ap, yacc.ap, gt.ap, ALU.mult, yacc, [yacc, gt])
        c.memset("pool", lG.ap, 0.0, lG)
        for g in range(8):
            c.dma("sp", lG.ap[16 * g:16 * g + 16, 16 * g:16 * g + 16], gw[(ch * 8 + g) * 16:(ch * 8 + g) * 16 + 16, :],
                  lG, c.Dk["s5_gluw"])
        for blk in range(nblk_s):
            n = min(512, cfg.seq - blk * 512)
            ps = c.bank(4 + blk % 2)
            c.mm(ps.ap[:, 0:n], lG.ap, yacc.ap[:, blk * 512:blk * 512 + n], ps, [lG, yacc])
            c.act(gt.ap[:, blk * 512:blk * 512 + n], ps.ap[:, 0:n], AF.Sigmoid, gt, [ps, gbT], bias=gbT.ap[:, ch:ch + 1])
        c.tt("dve", yo.ap, yacc.ap, gt.ap, ALU.mult, yo, [yacc, gt])
        c.dma("pool", YM[ch * 128:(ch + 1) * 128, :], yo.ap, c.YMk, yo)
    c.stage_end()


def stage_rw_lora(c):
    cfg, T = c.cfg, c.T
    ZS, LO = c.ZS.ap(), c.LO.ap()
    NP_ = cfg.npos
    w0T = load_colvec(c, T["rw_w0"].ap(), c.Dk["rw_w0"], 16, "w0T")
    a0T = load_colvec(c, T["rw_a0"].ap(), c.Dk["rw_a0"], 16, "a0T")
    txw = c.alloc("txw", NP_); xa = c.alloc("xa", NP_)
    sg0 = c.alloc("sg0", cfg.seq); sg1 = c.alloc("sg1", cfg.seq)
    w2 = c.alloc("w2", 1024); a2 = c.alloc("a2", 1024); g2a = c.alloc("g2a", 1024); g2b = c.alloc("g2b", 1024)
    c.dma("sp", txw.ap, ZS[3072:3200, :], txw, c.ZSk)
    c.dma("sp", xa.ap, ZS[3200:3328, :], xa, c.ZSk)
    c.dma("sp", sg0.ap, ZS[3328:3456, cfg.ctx:cfg.ctx + cfg.seq], sg0, c.ZSk)
    c.dma("sp", sg1.ap[0:32, :], ZS[3456:3488, cfg.ctx:cfg.ctx + cfg.seq], sg1, c.ZSk)
    c.dma("sp", w2.ap, T["rw_w2"].ap(), w2, c.Dk["rw_w2"])
    c.dma("sp", a2.ap, T["rw_a2"].ap(), a2, c.Dk["rw_a2"])
    c.dma("sp", g2a.ap, T["rw_g2"].ap()[0:128, :], g2a, c.Dk["rw_g2"])
    c.dma("sp", g2b.ap[0:32, :], T["rw_g2"].ap()[128:160, :], g2b, c.Dk["rw_g2"])
    c.act(txw.ap, txw.ap, AF.Tanh, txw, [txw])
    c.act(sg0.ap, sg0.ap, AF.Sigmoid, sg0, [sg0])
    c.act(sg1.ap[0:32, :], sg1.ap[0:32, :], AF.Sigmoid, sg1, [sg1])
    ob = [c.alloc(f"lo{i}", NP_) for i in range(2)]
    oi = 0
    nb = (NP_ + 511) // 512
    for hp in range(8):
        cs = slice(hp * 128, (hp + 1) * 128)
        for kind in range(5):
            o = ob[oi % 2]; oi += 1
            if kind < 4:
                d = kind % 2
                wt, src, bT = (w2, txw, w0T) if kind < 2 else (a2, xa, a0T)
                for blk in range(nb):
                    n = min(512, NP_ - blk * 512)
                    ps = c.bank(blk % 4)
                    c.mm(ps.ap[:, 0:n], wt.ap[d * 64:(d + 1) * 64, cs], src.ap[d * 64:(d + 1) * 64, blk * 512:blk * 512 + n],
                         ps, [wt, src])
                    c.act(o.ap[:, blk * 512:blk * 512 + n], ps.ap[:, 0:n], AF.Sigmoid, o, [ps, bT],
                          bias=bT.ap[:, d * 8 + hp:d * 8 + hp + 1])
                c.dma("pool", LO[kind, hp], o.ap, c.LOk, o)
            else:
                for blk in range((cfg.seq + 511) // 512):
                    n = min(512, cfg.seq - blk * 512)
                    ps = c.bank(blk % 4)
                    c.mm(ps.ap[:, 0:n], g2a.ap[:, cs], sg0.ap[:, blk * 512:blk * 512 + n], ps, [g2a, sg0], start=True, stop=False)
                    c.mm(ps.ap[:, 0:n], g2b.ap[0:32, cs], sg1.ap[0:32, blk * 512:blk * 512 + n], ps, [g2b, sg1], start=False, stop=True)
                    c.copy("act", o.ap[:, blk * 512:blk * 512 + n], ps.ap[:, 0:n], o, [ps])
                c.dma("pool", LO[4, hp, :, 0:cfg.seq], o.ap[:, 0:cfg.seq], c.LOk, o)
    c.stage_end()


def rw_consts(c):
    m = {}
    for name, pat, base, cm, cmp_ in (("SU", 1, 0, -1, ALU.is_gt), ("SL", -1, 0, 1, ALU.is_gt),
                                       ("IU", 1, 0, -1, ALU.is_ge), ("IL", -1, 0, 1, ALU.is_ge)):
        t = c.alloc("mask" + name, 128)
        c.memset("pool", t.ap, 1.0, t)
        c.op("pool", lambda e, t=t, pat=pat, base=base, cm=cm, cmp_=cmp_: e.affine_select(
            out=t.ap, in_=t.ap, pattern=[[pat, 128]], compare_op=cmp_, fill=0.0, base=base, channel_multiplier=cm), [t], [t])
        for j in range(NSUB):
            if j > 0:
                c.memset("pool", t.ap[j * CS:(j + 1) * CS, 0:j * CS], 0.0, t)
            if j < NSUB - 1:
                c.memset("pool", t.ap[j * CS:(j + 1) * CS, (j + 1) * CS:128], 0.0, t)
        m[name] = t
    bo = c.alloc("blockones", 128)
    c.memset("pool", bo.ap, 0.0, bo)
    c.memset("pool", bo.ap[0:64, 0:64], 1.0, bo)
    c.memset("pool", bo.ap[64:128, 64:128], 1.0, bo)
    hs = c.alloc("headsel", 2)
    c.memset("pool", hs.ap, 0.0, hs)
    c.memset("pool", hs.ap[0:64, 0:1], 1.0, hs)
    c.memset("pool", hs.ap[64:128, 1:2], 1.0, hs)
    ones1 = c.alloc("ones1", 128)
    c.memset("pool", ones1.ap[0:1, :], 1.0, ones1)
    m["BO"], m["HS"], m["ONES"] = bo, hs, ones1
    return m


def stage_rw(c):
    cfg, T = c.cfg, c.T
    ZS, LO, RW, YD, YM = c.ZS.ap(), c.LO.ap(), c.RW.ap(), c.YD.ap(), c.YM.ap()
    NP_, L = cfg.npos, cfg.ntok
    nblk = L // 128
    nlat = cfg.seq // 128
    nctx = cfg.ctx // 128
    M = rw_consts(c)
    kkT = load_colvec(c, T["rw_kk"].ap(), c.Dk["rw_kk"], 8, "kkT")
    kaT = load_colvec(c, T["rw_ka"].ap(), c.Dk["rw_ka"], 8, "kaT")
    rkT = load_colvec(c, T["rw_rk"].ap(), c.Dk["rw_rk"], 8, "rkT")
    lnrow = c.alloc("lnrow", 2048)
    c.dma("sp", lnrow.ap[0:1, 0:1024], T["rw_lnw"].ap(), lnrow, c.Dk["rw_lnw"])
    c.dma("sp", lnrow.ap[0:1, 1024:2048], T["rw_lnb"].ap(), lnrow, c.Dk["rw_lnb"])
    lnbc = c.alloc("lnbc", 2048)
    for q in range(4):
        ps = c.bank(q)
        c.mm(ps.ap[:, 0:512], M["ONES"].ap[0:1, :], lnrow.ap[0:1, q * 512:(q + 1) * 512], ps, [M["ONES"], lnrow])
        c.copy("act", lnbc.ap[:, q * 512:(q + 1) * 512], ps.ap[:, 0:512], lnbc, [ps])
    bonus = c.alloc("bonus", nlat * 2)
    gCt = c.alloc("gCt", 2 * (NP_ // CS))
    SEG = 1536 if NP_ % 1536 == 0 else NP_
    rmask = c.alloc("rmask", SEG)
    c.memset("pool", rmask.ap, 1.0, rmask)
    c.memset("pool", rmask.ap[:, 0:SEG:CS], 0.0, rmask)
    c.pers2 = c.top
    nseg = NP_ // SEG
    for hp in range(8):
        c.p.barrier()
        c.top = c.pers2
        names = ["r", "k", "kh", "kka", "sg", "a", "cs", "lg", "kt", "E", "q"]
        S = {n_: c.alloc("f_" + n_, SEG) for n_ in names}
        O = [c.alloc(f"f_o{i}", SEG) for i in range(3)]
        oi = 0
        for sg_i in range(nseg):
            p0 = sg_i * SEG
            psl = slice(p0, p0 + SEG)
            nch = SEG // CS
            c.dma("sp", S["r"].ap, ZS[hp * 128:(hp + 1) * 128, psl], S["r"], c.ZSk)
            c.dma("sp", S["k"].ap, ZS[1024 + hp * 128:1024 + (hp + 1) * 128, psl], S["k"], c.ZSk)
            kkc, kac, rkc = kkT.ap[:, hp:hp + 1], kaT.ap[:, hp:hp + 1], rkT.ap[:, hp:hp + 1]
            c.ts("dve", S["kh"].ap, S["k"].ap, kkc, None, ALU.mult, None, S["kh"], [S["k"], kkT])
            c.tt("dve", S["E"].ap, S["kh"].ap, S["kh"].ap, ALU.mult, S["E"], [S["kh"]])
            for blk in range((SEG + 511) // 512):
                n = min(512, SEG - blk * 512)
                ps = c.bank(blk % 4)
                c.mm(ps.ap[:, 0:n], M["BO"].ap, S["E"].ap[:, blk * 512:blk * 512 + n], ps, [M["BO"], S["E"]])
                c.act(S["cs"].ap[:, blk * 512:blk * 512 + n], ps.ap[:, 0:n], AF.Sqrt, S["cs"], [ps])
            c.ts("dve", S["cs"].ap, S["cs"].ap, 1e-12, None, ALU.max, None, S["cs"], [S["cs"]])
            c.op("dve", lambda e, o=S["cs"].ap: e.reciprocal(out=o, in_=o), [S["cs"]], [S["cs"]])
            c.tt("dve", S["kh"].ap, S["kh"].ap, S["cs"].ap, ALU.mult, S["kh"], [S["kh"], S["cs"]])
            c.ts("dve", S["kka"].ap, S["k"].ap, kac, None, ALU.mult, None, S["kka"], [S["k"], kaT])
            for d in range(2):
                c.dma("sp", S["sg"].ap, LO[d, hp, :, psl], S["sg"], c.LOk)
                c.dma("sp", S["a"].ap, LO[2 + d, hp, :, psl], S["a"], c.LOk)
                c.ts("dve", S["sg"].ap, S["sg"].ap, -math.exp(-0.5), None, ALU.mult, None, S["sg"], [S["sg"]])
                csv = S["cs"].ap.rearrange("p (c t) -> p c t", t=CS)
                sgv = S["sg"].ap.rearrange("p (c t) -> p c t", t=CS)
                lgv = S["lg"].ap.rearrange("p (c t) -> p c t", t=CS)
                c.op("dve", lambda e, o=S["cs"].ap, i=S["sg"].ap: e.tensor_tensor_scan(
                    out=o, data0=rmask.ap, data1=i, initial=0.0, op0=ALU.mult, op1=ALU.add),
                    [S["sg"], rmask], [S["cs"]])
                ctot = csv[:, :, CS - 1:CS]
                if d == 0:
                    c.copy("act", S["lg"].ap, S["cs"].ap, S["lg"], [S["cs"]])
                else:
                    c.tt("dve", lgv, sgv, csv, ALU.subtract, S["lg"], [S["sg"], S["cs"]])
                    c.tt("dve", lgv, lgv, ctot.to_broadcast([128, nch, CS]), ALU.add, S["lg"], [S["lg"], S["cs"]])
                gcs = gCt.ap[:, d * (NP_ // CS) + p0 // CS:d * (NP_ // CS) + p0 // CS + nch]
                c.act(gcs, S["cs"].ap[:, CS - 1:SEG:CS], AF.Exp, gCt, [S["cs"]])
                c.stt("dve", S["kt"].ap, S["a"].ap, -1.0, S["kka"].ap, ALU.add, ALU.mult, S["kt"], [S["a"], S["kka"]])
                c.tt("dve", S["kt"].ap, S["kt"].ap, S["k"].ap, ALU.add, S["kt"], [S["kt"], S["k"]])
                c.tt("dve", S["a"].ap, S["a"].ap, S["kh"].ap, ALU.mult, S["a"], [S["a"], S["kh"]])
                if d == 0:
                    c.stt("dve", S["q"].ap, S["r"].ap, rkc, S["kt"].ap, ALU.mult, ALU.mult, S["q"], [S["r"], rkT, S["kt"]])
                else:
                    c.stt("dve", S["E"].ap, S["r"].ap, rkc, S["kt"].ap, ALU.mult, ALU.mult, S["E"], [S["r"], rkT, S["kt"]])
                    c.tt("dve", S["q"].ap, S["q"].ap, S["E"].ap, ALU.add, S["q"], [S["q"], S["E"]])

                def emit_out(idx, base, ex, neg):
                    nonlocal oi
                    o = O[oi % 3]; oi += 1
                    c.tt("dve", o.ap, base.ap, ex.ap, ALU.mult, o, [base, ex])
                    if neg:
                        c.op("act", lambda e, o=o: e.mul(out=o.ap, in_=o.ap, mul=-1.0), [o], [o])
                    c.dma("pool", RW[d, hp, :, idx, psl], o.ap, c.RWk, o)
                c.act(S["E"].ap, S["lg"].ap, AF.Exp, S["E"], [S["lg"]], scale=-1.0)
                emit_out(0, S["a"], S["E"], True)
                emit_out(2, S["kt"], S["E"], False)
                c.act(S["E"].ap, S["lg"].ap, AF.Exp, S["E"], [S["lg"]])
                emit_out(3, S["r"], S["E"], False)
                c.tt("dve", S["E"].ap, S["lg"].ap, S["sg"].ap, ALU.subtract, S["E"], [S["lg"], S["sg"]])
                c.act(S["E"].ap, S["E"].ap, AF.Exp, S["E"], [S["E"]])
                emit_out(1, S["kh"], S["E"], False)
                Ev = S["E"].ap.rearrange("p (c t) -> p c t", t=CS)
                c.tt("dve", Ev, ctot.to_broadcast([128, nch, CS]), lgv, ALU.subtract, S["E"], [S["cs"], S["lg"]])
                c.act(S["E"].ap, S["E"].ap, AF.Exp, S["E"], [S["E"]])
                emit_out(4, S["a"], S["E"], True)
                emit_out(5, S["kt"], S["E"], False)
            for bl in range(SEG // 128):
                gpos = p0 + bl * 128
                if gpos < cfg.ctx or gpos >= cfg.ctx + cfg.seq:
                    continue
                lb = (gpos - cfg.ctx) // 128
                ps = c.bank(4 + bl % 2)
                c.mm(ps.ap[:, 0:2], S["q"].ap[:, bl * 128:(bl + 1) * 128], M["HS"].ap, ps, [S["q"], M["HS"]])
                c.copy("act", bonus.ap[:, lb * 2:lb * 2 + 2], ps.ap[:, 0:2], bonus, [ps])
        c.p.barrier()
        c.top = c.pers2
        rw_chain(c, hp, M, gCt)
        c.p.barrier()
        c.top = c.pers2
        rw_readout(c, hp, M, bonus, lnbc)
    c.stage_end()


def rw_chain(c, hp, M, gCt):
    cfg = c.cfg
    ZS, RW, YD = c.ZS.ap(), c.RW.ap(), c.YD.ap()
    NP_, L = cfg.npos, cfg.ntok
    nblk = L // 128
    NG = 2
    ident = c.ident
    H = [[c.alloc(f"H{d}{hl}", 64) for hl in range(2)] for d in range(2)]
    for d in range(2):
        for hl in range(2):
            c.memset("pool", H[d][hl].ap[0:64, :], 0.0, H[d][hl])
    nset = 2
    gC1 = c.alloc("gC1", 2 * (NP_ // CS))
    c.dma("sp", gC1.ap[0:64, :], gCt.ap[64:128, :], gC1, gCt)
    gC = [gCt, gC1]
    def mk(name, n):
        return [[[c.alloc(f"{name}{s_}{g}{d}", n) for d in range(2)] for g in range(NG)] for s_ in range(nset)]
    OP = [[[[c.alloc(f"op{s_}{g}{d}{hl}", 6 * 128) for hl in range(2)] for d in range(2)] for g in range(NG)] for s_ in range(nset)]
    VF = mk("vf", 128)
    VT = mk("vt", 128)
    TM = [[[[c.alloc(f"tm{s_}{g}{d}{hl}", 192) for hl in range(2)] for d in range(2)] for g in range(NG)] for s_ in range(nset)]
    RES = [[[[c.alloc(f"res{s_}{g}{d}{hl}", 2 * NSUB * 64 + 192) for hl in range(2)] for d in range(2)] for g in range(NG)] for s_ in range(nset)]
    TMP = [[[[[c.alloc(f"t{s_}{g}{d}{hl}{i}", 128) for i in range(9)] for hl in range(2)] for d in range(2)] for g in range(NG)] for s_ in range(nset)]
    YO = [[c.alloc(f"yo{s_}{d}", 128) for d in range(2)] for s_ in range(4)]
    slot_ctr = [0]

    def unit(s_, g, d, hl, blk_pos, islat, slotbase, gci0, gcb):
        op = OP[s_][g][d][hl]
        opv = op.ap.rearrange("p (i t) -> p i t", i=6)
        At, Bt, Kt, Rt, Ab, Kb = (opv[0:64, i, :] for i in range(6))
        tm = TM[s_][g][d][hl]
        Btm, Abm, Kbm = tm.ap[:, 0:64], tm.ap[:, 64:128], tm.ap[:, 128:192]
        Vtm = VT[s_][g][d].ap[:, hl * 64:(hl + 1) * 64]
        vtb = VT[s_][g][d]
        res = RES[s_][g][d][hl]
        PhiT, Psi = res.ap[0:64, 0:NSUB * 64], res.ap[0:64, NSUB * 64:2 * NSUB * 64]
        PT, Y2 = res.ap[0:64, 2 * NSUB * 64:2 * NSUB * 64 + 128], res.ap[:, 2 * NSUB * 64 + 128:2 * NSUB * 64 + 192]
        t = TMP[s_][g][d][hl]
        strict = M["SU"] if d == 0 else M["SL"]
        strictT = M["SL"] if d == 0 else M["SU"]
        incl = M["IU"] if d == 0 else M["IL"]

        def slot(i):
            bk = c.bank(slotbase)
            return B(bk.ap[:, i * 128:(i + 1) * 128], bk.key)
        s0, s1 = slot(0), slot(1)
        for src, dst in ((Bt, Btm), (Ab, Abm), (Kb, Kbm)):
            c.tr(s0.ap[:, 0:64], src, ident.ap[0:64, 0:64], s0, [op, ident])
            c.copy("act", dst, s0.ap[:, 0:64], tm, [s0])
            yield
        LAT, LA, LKT, MAT, MKT = t[0], t[1], t[4], t[5], t[6]
        c.mm(s0.ap, At, Bt, s0, [op]); c.tt("dve", LAT.ap, s0.ap, strict.ap, ALU.mult, LAT, [s0, strict])
        c.mm(s1.ap, Bt, At, s1, [op]); c.tt("dve", LA.ap, s1.ap, strictT.ap, ALU.mult, LA, [s1, strictT])
        yield
        c.mm(s0.ap, Kt, Bt, s0, [op]); c.tt("dve", LKT.ap, s0.ap, strict.ap, ALU.mult, LKT, [s0, strict])
        if islat:
            c.mm(s1.ap, At, Rt, s1, [op]); c.tt("dve", MAT.ap, s1.ap, incl.ap, ALU.mult, MAT, [s1, incl])
        yield
        if islat:
            c.mm(s0.ap, Kt, Rt, s0, [op]); c.tt("dve", MKT.ap, s0.ap, incl.ap, ALU.mult, MKT, [s0, incl])
        Z = [t[7], t[8]]
        c.mm(s1.ap[:, 0:64], LKT.ap, Vtm, s1, [LKT, vtb])
        c.copy("act", Z[0].ap[:, 64:128], s1.ap[:, 0:64], Z[0], [s1])
        c.copy("act", Z[0].ap[:, 0:64], Btm, Z[0], [tm])
        yield
        X, XT = LA, LAT
        alt = [(t[2], t[3]), (t[1], t[0])]
        zc = 0
        NL = int(math.log2(CS))
        for i in range(NL):
            c.mm(s0.ap, XT.ap, Z[zc].ap, s0, [XT, Z[zc]])
            c.tt("dve", Z[1 - zc].ap, Z[zc].ap, s0.ap, ALU.add, Z[1 - zc], [Z[zc], s0])
            zc = 1 - zc
            if i < NL - 1:
                nX, nXT = alt[i % 2]
                c.mm(s1.ap, X.ap, XT.ap, s1, [X, XT])
                c.copy("act", nXT.ap, s1.ap, nXT, [s1])
                if i < NL - 2:
                    yield
                    c.mm(s1.ap, XT.ap, X.ap, s1, [X, XT])
                    c.copy("act", nX.ap, s1.ap, nX, [s1])
                X, XT = nX, nXT
            yield
        Zf = Z[zc]
        W1, U2 = Zf.ap[:, 0:64], Zf.ap[:, 64:128]
        for j in range(NSUB):
            js = slice(j * CS, (j + 1) * CS)
            c.mm(s0.ap[0:64, 0:64], Zf.ap[js, 0:64], tm.ap[js, 64:128], s0, [Zf, tm])
            c.stt("dve", PhiT[:, j * 64:(j + 1) * 64], c.ident.ap[0:64, 0:64], gcb.ap[0:64, gci0 + j:gci0 + j + 1],
                  s0.ap[0:64, 0:64], ALU.mult, ALU.add, res, [c.ident, gcb, s0])
            c.mm(s1.ap[0:64, 0:64], tm.ap[js, 64:128], Zf.ap[js, 64:128], s1, [Zf, tm], start=True, stop=False)
            c.mm(s1.ap[0:64, 0:64], tm.ap[js, 128:192], VT[s_][g][d].ap[js, hl * 64:(hl + 1) * 64], s1, [tm, vtb], start=False, stop=True)
            c.copy("act", Psi[:, j * 64:(j + 1) * 64], s1.ap[0:64, 0:64], res, [s1])
            yield
        if islat:
            c.mm(s0.ap[:, 0:64], MAT.ap, U2, s0, [MAT, Zf], start=True, stop=False)
            c.mm(s0.ap[:, 0:64], MKT.ap, Vtm, s0, [MKT, vtb], start=False, stop=True)
            c.copy("act", Y2, s0.ap[:, 0:64], res, [s0])
            c.mm(s1.ap[0:64, :], W1, MAT.ap, s1, [Zf, MAT])
            c.tt("dve", PT, s1.ap[0:64, :], Rt, ALU.add, res, [s1, op])
        yield

    nsteps = nblk
    step = 0
    gi = 0
    yo_i = 0
    while step < nsteps:
        s_ = gi % nset
        gi += 1
        ng = min(NG, nsteps - step)
        gens = []
        info = []
        for g in range(ng):
            for d in range(2):
                bi = step + g
                blk = bi if d == 0 else (nblk - 1 - bi)
                pos = blk * 128 + (0 if d == 0 else cfg.ctx)
                islat = cfg.ctx <= pos < cfg.ctx + cfg.seq
                c.dma("sp", VF[s_][g][d].ap, ZS[2048 + hp * 128:2048 + (hp + 1) * 128, pos:pos + 128], VF[s_][g][d], c.ZSk)
                pb = c.bank(4 + d)
                c.tr(pb.ap[:, 0:128], VF[s_][g][d].ap, c.ident.ap, pb, [VF[s_][g][d], c.ident])
                c.copy("act", VT[s_][g][d].ap, pb.ap[:, 0:128], VT[s_][g][d], [pb])
                for hl in range(2):
                    op = OP[s_][g][d][hl]
                    c.dma("sp", op.ap[0:64, :].rearrange("p (i t) -> p i t", i=6),
                          RW[d, hp, hl * 64:(hl + 1) * 64, :, pos:pos + 128], op, c.RWk)
                    u_idx = (g * 2 + d) * 2 + hl
                    gci = d * (NP_ // CS) + pos // CS
                    gcb = gC[hl]
                    gens.append(unit(s_, g, d, hl, pos, islat, u_idx, gci, gcb))
                    info.append((g, d, hl, pos, islat))
        results = [None] * len(gens)
        live = list(range(len(gens)))
        while live:
            nxt = []
            for ui in live:
                try:
                    r = next(gens[ui])
                    if r is not None:
                        results[ui] = r
                    nxt.append(ui)
                except StopIteration:
                    pass
            live = nxt
        for g in range(ng):
            for d in range(2):
                bi = step + g
                blk = bi if d == 0 else (nblk - 1 - bi)
                pos = blk * 128 + (0 if d == 0 else cfg.ctx)
                islat = cfg.ctx <= pos < cfg.ctx + cfg.seq
                gcol_i = d * (NP_ // 128) + pos // 128
                yo = YO[yo_i % 4][d]
                for hl in range(2):
                    res = RES[s_][g][d][hl]
                    Hh = H[d][hl]
                    o2 = 2 * NSUB * 64
                    PT, Y2 = res.ap[0:64, o2:o2 + 128], res.ap[:, o2 + 128:o2 + 192]
                    pb = c.bank(6 + hl)
                    order = range(NSUB) if d == 0 else range(NSUB - 1, -1, -1)
                    for j in order:
                        js = slice(j * CS, (j + 1) * CS)
                        PhiT, Psi = res.ap[0:64, j * 64:(j + 1) * 64], res.ap[0:64, NSUB * 64 + j * 64:NSUB * 64 + (j + 1) * 64]
                        if islat:
                            pbs = B(pb.ap[js, 0:64], pb.key)
                            c.mm(pbs.ap, PT[:, js], Hh.ap[0:64, :], pbs, [res, Hh])
                            c.tt("dve", yo.ap[js, hl * 64:(hl + 1) * 64], pbs.ap, Y2[js, :], ALU.add, yo, [pbs, res])
                        pbs2 = B(pb.ap[0:64, 128:192], pb.key)
                        c.mm(pbs2.ap, PhiT, Hh.ap[0:64, :], pbs2, [res, Hh])
                        c.tt("dve", Hh.ap[0:64, :], pbs2.ap, Psi, ALU.add, Hh, [pbs2, res])
                if islat:
                    lpos = pos - cfg.ctx
                    c.dma("pool", YD[d, lpos:lpos + 128, hp * 128:(hp + 1) * 128], yo.ap, c.YDk, yo)
            yo_i += 1
        step += ng


def rw_readout(c, hp, M, bonus, lnbc):
    cfg = c.cfg
    ZS, LO, YD, YM = c.ZS.ap(), c.LO.ap(), c.YD.ap(), c.YM.ap()
    nlat = cfg.seq // 128
    yo = c.alloc("rd_out", cfg.seq, BF16)
    bufs = [[c.alloc(f"rd{n_}{i}", 128) for i in range(2)] for n_ in ("yf", "yb", "vf", "g", "t", "sq")]
    st = [c.alloc(f"rdst{i}", 8) for i in range(2)]
    for lb in range(nlat):
        yf, yb, vf, gf, tt_, sq = (bufs[i][lb % 2] for i in range(6))
        s_ = st[lb % 2]
        pos = cfg.ctx + lb * 128
        c.dma("sp", yf.ap, YD[0, lb * 128:(lb + 1) * 128, hp * 128:(hp + 1) * 128], yf, c.YDk)
        c.dma("sp", yb.ap, YD[1, lb * 128:(lb + 1) * 128, hp * 128:(hp + 1) * 128], yb, c.YDk)
        c.dma("sp", vf.ap, ZS[2048 + hp * 128:2048 + (hp + 1) * 128, pos:pos + 128], vf, c.ZSk)
        c.dma("sp", gf.ap, LO[4, hp, :, lb * 128:(lb + 1) * 128], gf, c.LOk)
        c.tt("dve", yf.ap, yf.ap, yb.ap, ALU.add, yf, [yf, yb])
        yv = yf.ap.rearrange("p (h v) -> p h v", h=2)
        c.op("dve", lambda e, o=s_.ap[:, 0:2], i=yv: e.tensor_reduce(out=o, in_=i, axis=AX.X, op=ALU.add), [yf], [s_])
        c.ts("dve", s_.ap[:, 0:2], s_.ap[:, 0:2], 1.0 / 64, None, ALU.mult, None, s_, [s_])
        c.tt("dve", yv, yv, s_.ap[:, 0:2].unsqueeze(2).to_broadcast([128, 2, 64]), ALU.subtract, yf, [yf, s_])
        c.tt("dve", sq.ap, yf.ap, yf.ap, ALU.mult, sq, [yf])
        c.op("dve", lambda e, o=s_.ap[:, 2:4], i=sq.ap.rearrange("p (h v) -> p h v", h=2): e.tensor_reduce(
            out=o, in_=i, axis=AX.X, op=ALU.add), [sq], [s_])
        c.ts("dve", s_.ap[:, 2:4], s_.ap[:, 2:4], 1.0 / 64, 64e-5, ALU.mult, ALU.add, s_, [s_])
        c.act(s_.ap[:, 2:4], s_.ap[:, 2:4], AF.Sqrt, s_, [s_])
        c.op("dve", lambda e, o=s_.ap[:, 2:4]: e.reciprocal(out=o, in_=o), [s_], [s_])
        c.tt("dve", yv, yv, s_.ap[:, 2:4].unsqueeze(2).to_broadcast([128, 2, 64]), ALU.mult, yf, [yf, s_])
        c.tt("dve", yf.ap, yf.ap, lnbc.ap[:, hp * 128:(hp + 1) * 128], ALU.mult, yf, [yf, lnbc])
        c.tt("dve", yf.ap, yf.ap, lnbc.ap[:, 1024 + hp * 128:1024 + (hp + 1) * 128], ALU.add, yf, [yf, lnbc])
        pb = c.bank(4 + lb % 2)
        c.tr(pb.ap[:, 0:128], vf.ap, c.ident.ap, pb, [vf, c.ident])
        tv = tt_.ap.rearrange("p (h v) -> p h v", h=2)
        c.tt("dve", tv, pb.ap[:, 0:128].rearrange("p (h v) -> p h v", h=2),
             bonus.ap[:, lb * 2:lb * 2 + 2].unsqueeze(2).to_broadcast([128, 2, 64]), ALU.mult, tt_, [pb, bonus])
        c.tt("dve", yf.ap, yf.ap, tt_.ap, ALU.add, yf, [yf, tt_])
        pb2 = c.bank(6 + lb % 2)
        c.tr(pb2.ap[:, 0:128], yf.ap, c.ident.ap, pb2, [yf, c.ident])
        c.tt("dve", yo.ap[:, lb * 128:(lb + 1) * 128], pb2.ap[:, 0:128], gf.ap, ALU.mult, yo, [pb2, gf])
    c.dma("pool", YM[1024 + hp * 128:1024 + (hp + 1) * 128, :], yo.ap, c.YMk, yo)


def bcast_rows(c, colT, name, bank0=0, res=None, dg=None):
    if res is None:
        res = c.alloc(name, D)
    if dg is None:
        dg = c.alloc(name + "_dg", D)
    for j in range(16):
        c.ts("dve", dg.ap[:, j * 128:(j + 1) * 128], c.ident.ap, colT[:, j:j + 1], None, ALU.mult, None, dg, [c.ident, c.mT, c.fgT])
    for q in range(4):
        ps = c.bank(bank0 + q)
        c.mm(ps.ap, c.ones128.ap, dg.ap[:, q * 512:(q + 1) * 512], ps, [c.ones128, dg])
        c.copy("act", res.ap[:, q * 512:(q + 1) * 512], ps.ap, res, [ps])
    return res


def stage_outproj(c):
    cfg, T = c.cfg, c.T
    mTv = c.mT.ap.rearrange("p (o r) -> p o r", r=2)
    YM, X1, FT, GT = c.YM.ap(), c.X1.ap(), c.FT.ap(), c.GT.ap()
    g2 = load_colvec(c, T["norm2_g"].ap(), c.Dk["norm2_g"], 16, "g2T")
    scl2 = c.alloc("scl2", 16)
    c.stt("dve", scl2.ap, mTv[:, 64:80, 0], 1.0, g2.ap, ALU.add, ALU.mult, scl2, [c.mT, g2])
    gt1bc = bcast_rows(c, mTv[:, 32:48, 0], "gt1bc")
    wout = c.alloc("wout", 16 * D, BF16)
    wov = wout.ap.rearrange("p (jc n) -> p jc n", jc=16)
    wsrc = T["w_out"].ap().rearrange("(jc p) n -> p jc n", p=128)
    for jc in range(16):
        c.dma("pool", wov[:, jc, :], wsrc[:, jc, :], wout, c.Dk["w_out"])
    rw = c.alloc("routerw", 16 * 32)
    rwv = rw.ap.rearrange("p (kc e) -> p kc e", kc=16)
    c.dma("sp", rwv, T["router_w"].ap().rearrange("(kc p) e -> p kc e", p=128), rw, c.Dk["router_w"])
    rb = c.alloc("routerb", 32)
    c.dma("sp", rb.ap[0:1, :], T["router_b"].ap(), rb, c.Dk["router_b"])
    TBK = min(512, cfg.nown)
    ymb = [c.alloc(f"ymb{i}", 16 * TBK, BF16) for i in range(2)]
    ftb = [c.alloc(f"ftb{i}", 16 * TBK, BF16) for i in range(2)]
    gtb = [c.alloc(f"gtb{i}", TBK) for i in range(2)]
    xt = [c.alloc(f"ox{i}", D) for i in range(2)]
    xs = c.alloc("oxs", D)
    tmp = c.alloc("otmp", 512)
    junk = c.alloc("ojunk", D, BF16)
    f32 = c.alloc("of32", 16 * 128)
    f32v = f32.ap.rearrange("p (j t) -> p j t", j=16)
    st = c.alloc("ost", 8)
    lg = c.alloc("olg", 32); mx = c.alloc("omx", 8); ex = c.alloc("oex", 32); mk = c.alloc("omk", 32)
    ymsrc = YM.rearrange("(jc p) t -> p jc t", p=128)
    ftdst = FT.rearrange("(jc p) t -> p jc t", p=128)
    for blk in range(cfg.nown // TBK):
        yb, fb, gb = ymb[blk % 2], ftb[blk % 2], gtb[blk % 2]
        ybv = yb.ap.rearrange("p (jc t) -> p jc t", jc=16)
        fbv = fb.ap.rearrange("p (jc t) -> p jc t", jc=16)
        c.dma("sp", ybv, ymsrc[:, :, bass.ts(c.qv * (cfg.nown // TBK) + blk, TBK)], yb, c.YMk)
        for ti in range(TBK // 128):
            tok0 = blk * TBK + ti * 128
            x = xt[ti % 2]
            c.dma("sp", x.ap, T["x_own"].ap()[tok0:tok0 + 128, :], x, c.Dk["x_own"])
            for cb in range(4):
                ps = c.bank(cb)
                for jc in range(16):
                    c.mm(ps.ap, ybv[:, jc, ti * 128:(ti + 1) * 128], wov[:, jc, cb * 512:(cb + 1) * 512], ps, [yb, wout],
                         start=(jc == 0), stop=(jc == 15))
                c.tt("dve", tmp.ap, ps.ap, gt1bc.ap[:, cb * 512:(cb + 1) * 512], ALU.mult, tmp, [ps, gt1bc])
                c.tt("dve", x.ap[:, cb * 512:(cb + 1) * 512], x.ap[:, cb * 512:(cb + 1) * 512], tmp.ap, ALU.add, x, [x, tmp])
            c.dma("act", X1[tok0:tok0 + 128, :], x.ap, c.X1k, x)
            c.act(junk.ap, x.ap, AF.Square, junk, [x], accum=st.ap[:, 0:1], accb=st)
            c.ts("dve", st.ap[:, 0:1], st.ap[:, 0:1], 1.0 / D, 1e-5, ALU.mult, ALU.add, st, [st, junk])
            c.act(st.ap[:, 0:1], st.ap[:, 0:1], AF.Sqrt, st, [st])
            c.op("dve", lambda e: e.reciprocal(out=st.ap[:, 0:1], in_=st.ap[:, 0:1]), [st], [st])
            c.ts("dve", xs.ap, x.ap, st.ap[:, 0:1], None, ALU.mult, None, xs, [x, st])
            for q in range(4):
                ps = c.bank(4 + q % 2)
                for jj in range(4):
                    j = q * 4 + jj
                    c.tr(ps.ap[:, jj * 128:(jj + 1) * 128], xs.ap[:, j * 128:(j + 1) * 128], c.ident.ap, ps, [xs, c.ident])
                for jj in range(4):
                    j = q * 4 + jj
                    c.act(f32v[:, j, :], ps.ap[:, jj * 128:(jj + 1) * 128], AF.Identity, f32, [ps, scl2, c.mT],
                          bias=mTv[:, 48 + j, 0:1], scale=scl2.ap[:, j:j + 1])
            c.copy("pool", fbv[:, :, ti * 128:(ti + 1) * 128], f32v, fb, [f32])
            ps = c.bank(6)
            for j in range(16):
                c.mm(ps.ap[:, 0:32], f32v[:, j, :], rwv[:, j, :], ps, [f32, rw], start=(j == 0), stop=False)
            c.mm(ps.ap[:, 0:32], c.onesrow.ap[0:1, :], rb.ap[0:1, :], ps, [c.onesrow, rb], start=False, stop=True)
            c.copy("act", lg.ap, ps.ap[:, 0:32], lg, [ps])
            c.op("dve", lambda e: e.max(out=mx.ap, in_=lg.ap), [lg], [mx])
            c.ts("dve", mk.ap, lg.ap, mx.ap[:, 3:4], None, ALU.is_ge, None, mk, [lg, mx])
            c.ts("dve", mx.ap[:, 4:5], mx.ap[:, 0:1], -1.0, None, ALU.mult, None, mx, [mx])
            c.act(ex.ap, lg.ap, AF.Exp, ex, [lg, mx], bias=mx.ap[:, 4:5])
            c.tt("dve", ex.ap, ex.ap, mk.ap, ALU.mult, ex, [ex, mk])
            c.op("dve", lambda e: e.tensor_reduce(out=mx.ap[:, 5:6], in_=ex.ap, axis=AX.X, op=ALU.add), [ex], [mx])
            c.op("dve", lambda e: e.reciprocal(out=mx.ap[:, 5:6], in_=mx.ap[:, 5:6]), [mx], [mx])
            c.ts("dve", ex.ap, ex.ap, mx.ap[:, 5:6], None, ALU.mult, None, ex, [ex, mx])
            ps2 = c.bank(7)
            c.tr(ps2.ap[0:32, 0:128], ex.ap, c.ident.ap, ps2, [ex, c.ident])
            c.copy("act", gb.ap[0:32, ti * 128:(ti + 1) * 128], ps2.ap[0:32, 0:128], gb, [ps2])
        c.dma("act", ftdst[:, :, blk * TBK:(blk + 1) * TBK], fbv, c.FTk, fb)
        c.dma("act", GT[:, blk * TBK:(blk + 1) * TBK], gb.ap[0:32, :], c.GTk, gb)
    c.stage_end()


def stage_moe(c):
    cfg, T = c.cfg, c.T
    mTv = c.mT.ap.rearrange("p (o r) -> p o r", r=2)
    X1, FT, GT = c.X1.ap(), c.FT.ap(), c.GT.ap()
    NJ = cfg.dexp // 128
    TB = min(1024, cfg.nown)
    NH = (TB + 511) // 512
    HW = TB // NH
    nrow_b = NEXP * 2 * NJ
    bgu = c.alloc("bguT", nrow_b)
    bdn = c.alloc("bdn", D)
    gt2bc = c.alloc("gt2bc", D)
    fgbc = c.alloc("fgbc", D)
    base = c.top
    bsrc = T["exp_bgu"].ap()
    for r0 in range(0, nrow_b, 128):
        n = min(128, nrow_b - r0)
        c.top = base
        tmpc = load_colvec(c, bsrc[r0:r0 + n, :], c.Dk["exp_bgu"], n, f"bgu{r0}")
        c.copy("dve", bgu.ap[:, r0:r0 + n], tmpc.ap, bgu, [tmpc])
        c.p.barrier()
    c.top = base
    c.dma("sp", bdn.ap[0:32, :], T["exp_bdn"].ap(), bdn, c.Dk["exp_bdn"])
    dg = c.alloc("dgscr", D)
    bcast_rows(c, mTv[:, 80:96, 0], "gt2bc", res=gt2bc, dg=dg)
    bcast_rows(c, c.fgT.ap, "fgbc", bank0=4, res=fgbc, dg=dg)
    c.p.barrier()
    c.top = base
    for blk in range(cfg.nown // TB):
        c.p.barrier()
        c.top = base
        t0 = blk * TB
        ft = c.alloc("m_ft", 16 * TB, BF16)
        ftv = ft.ap.rearrange("p (kc t) -> p kc t", kc=16)
        yacc = c.alloc("m_yacc", 16 * TB)
        yav = yacc.ap.rearrange("p (cc t) -> p cc t", cc=16)
        base2 = c.top
        gts = c.alloc("m_gt", TB)
        actT = c.alloc("m_act", NJ * TB, BF16)
        acv = actT.ap.rearrange("p (j t) -> p j t", j=NJ)
        gbc = c.alloc("m_gbc", TB)
        gl = c.alloc("m_gl", HW); ln = c.alloc("m_ln", HW); sg = c.alloc("m_sg", HW)
        wg = [[c.alloc(f"m_wg{k}{i}", 16 * 128, BF16) for i in range(2)] for k in range(2)]
        wd = [c.alloc(f"m_wd{i}", D, BF16) for i in range(2)]
        c.dma("sp", ftv, FT.rearrange("(kc p) t -> p kc t", p=128)[:, :, t0:t0 + TB], ft, c.FTk)
        c.dma("sp", gts.ap[0:32, :], GT[:, t0:t0 + TB], gts, c.GTk)
        for cc in range(16):
            for h in range(NH):
                ps = c.bank((cc * NH + h) % 4)
                c.mm(ps.ap[:, 0:HW], bdn.ap[0:32, cc * 128:(cc + 1) * 128], gts.ap[0:32, h * HW:(h + 1) * HW], ps, [bdn, gts])
                c.copy("act", yav[:, cc, h * HW:(h + 1) * HW], ps.ap[:, 0:HW], yacc, [ps])
        wi = 0
        di = 0
        for e in range(NEXP):
            for h in range(NH):
                ps = c.bank(4 + h)
                c.mm(ps.ap[:, 0:HW], c.ident.ap[0:32, e:e + 1].to_broadcast([32, 128]), gts.ap[0:32, h * HW:(h + 1) * HW], ps,
                     [c.ident, gts])
                c.copy("act", gbc.ap[:, h * HW:(h + 1) * HW], ps.ap[:, 0:HW], gbc, [ps])
            wsrc = T["exp_wgu"].ap()[e].rearrange("(kc p) n -> p kc n", p=128)
            for j in range(NJ):
                wt = []
                for kind in range(2):
                    w = wg[kind][wi % 2]
                    wv = w.ap.rearrange("p (kc n) -> p kc n", kc=16)
                    c.dma("pool", wv, wsrc[:, :, kind * cfg.dexp + j * 128:kind * cfg.dexp + (j + 1) * 128], w, c.Dk["exp_wgu"])
                    wt.append((w, wv))
                wi += 1
                bcol = e * 2 * NJ + j
                for h in range(NH):
                    hs = slice(h * HW, (h + 1) * HW)
                    pg, pl = c.bank(2 * h), c.bank(2 * h + 1)
                    for kind, ps in ((0, pg), (1, pl)):
                        w, wv = wt[kind]
                        for kc in range(16):
                            c.mm(ps.ap[:, 0:HW], wv[:, kc, :], ftv[:, kc, hs], ps, [w, ft], start=(kc == 0), stop=(kc == 15))
                    c.ts("dve", gl.ap, pg.ap[:, 0:HW], bgu.ap[:, bcol:bcol + 1], 7.0, ALU.add, ALU.min, gl, [pg, bgu])
                    c.act(sg.ap, gl.ap, AF.Sigmoid, sg, [gl], scale=1.702)
                    c.ts("dve", ln.ap, pl.ap[:, 0:HW], bgu.ap[:, bcol + NJ:bcol + NJ + 1], 7.0, ALU.add, ALU.min, ln, [pl, bgu])
                    c.ts("dve", ln.ap, ln.ap, -7.0, 1.0, ALU.max, ALU.add, ln, [ln])
                    c.tt("dve", gl.ap, gl.ap, sg.ap, ALU.mult, gl, [gl, sg])
                    c.tt("dve", gl.ap, gl.ap, ln.ap, ALU.mult, gl, [gl, ln])
                    c.tt("dve", acv[:, j, hs], gl.ap, gbc.ap[:, hs], ALU.mult, actT, [gl, gbc])
            dsrc = T["exp_wdn"].ap()[e].rearrange("(jc p) n -> p jc n", p=128)
            for cc in range(16):
                w = wd[di % 2]
                di += 1
                wv = w.ap.rearrange("p (jc n) -> p jc n", jc=16)
                c.dma("pool", wv[:, 0:NJ, :], dsrc[:, :, cc * 128:(cc + 1) * 128], w, c.Dk["exp_wdn"])
                for h in range(NH):
                    ps = c.bank(4 + (cc * NH + h) % 4)
                    for j in range(NJ):
                        c.mm(ps.ap[:, 0:HW], wv[:, j, :], acv[:, j, h * HW:(h + 1) * HW], ps, [w, actT],
                             start=(j == 0), stop=(j == NJ - 1))
                    c.tt("dve", yav[:, cc, h * HW:(h + 1) * HW], yav[:, cc, h * HW:(h + 1) * HW], ps.ap[:, 0:HW], ALU.add, yacc, [yacc, ps])
        c.p.barrier()
        c.top = base2
        x1t = [c.alloc(f"fx{i}", D) for i in range(2)]
        tmp = c.alloc("ftmp", 512)
        junk = c.alloc("fjunk", D, BF16)
        st = c.alloc("fst", 8)
        for ti in range(TB // 128):
            tok0 = t0 + ti * 128
            x = x1t[ti % 2]
            c.dma("sp", x.ap, X1[tok0:tok0 + 128, :], x, c.X1k)
            for q in range(4):
                ps = c.bank(q)
                for jj in range(4):
                    cc = q * 4 + jj
                    c.tr(ps.ap[:, jj * 128:(jj + 1) * 128], yav[:, cc, ti * 128:(ti + 1) * 128], c.ident.ap, ps, [yacc, c.ident])
                c.tt("dve", tmp.ap, ps.ap, gt2bc.ap[:, q * 512:(q + 1) * 512], ALU.mult, tmp, [ps, gt2bc])
                c.tt("dve", x.ap[:, q * 512:(q + 1) * 512], x.ap[:, q * 512:(q + 1) * 512], tmp.ap, ALU.add, x, [x, tmp])
            c.act(junk.ap, x.ap, AF.Square, junk, [x], accum=st.ap[:, 0:1], accb=st)
            c.ts("dve", st.ap[:, 0:1], st.ap[:, 0:1], 1.0 / D, 1e-5, ALU.mult, ALU.add, st, [st, junk])
            c.act(st.ap[:, 0:1], st.ap[:, 0:1], AF.Sqrt, st, [st])
            c.op("dve", lambda e: e.reciprocal(out=st.ap[:, 0:1], in_=st.ap[:, 0:1]), [st], [st])
            c.stt("dve", x.ap, x.ap, st.ap[:, 0:1], fgbc.ap, ALU.mult, ALU.mult, x, [x, st, fgbc])
            c.dma("act", c.out.ap()[tok0:tok0 + 128, :], x.ap, c.Dk["out"], x)
    c.stage_end()


def make_inputs(cfg, b, inp, q=0):
    inp = {k: np.asarray(v) for k, v in inp.items()}
    f = lambda a: np.ascontiguousarray(a, dtype=np.float32)
    cc = np.stack([inp["c"][b], inp["c_ctx"]]).reshape(32, 128)
    return {
        "x": f(inp["x"][b]), "x_own": f(inp["x"][b, q * cfg.nown:(q + 1) * cfg.nown]), "ctx": f(inp["ctx"][b]), "cc": f(cc),
        "mod_w": f(inp["mod_w"][0]), "mod_b": f(inp["mod_b"][0].reshape(96, 128)),
        "norm1_g": f(inp["norm1_g"][0].reshape(16, 128)), "w_in": f(inp["w_in"][0]),
        "rw_mu": f(np.pad(inp["rw_mu"][0], ((0, 0), (0, 28 * 128 - RWC))).reshape(4 * 28, 128)),
        "s5_are": f(inp["s5_a_re"][0].reshape(64, 128)), "s5_aim": f(inp["s5_a_im"][0].reshape(64, 128)),
        "s5_ldt": f(np.repeat(inp["s5_log_dt"][0].reshape(2, 32, 2, 1), 64, axis=3).reshape(64, 128)),
        "s5_d": f(inp["s5_d"][0].reshape(8, 128)), "s5_glub": f(inp["s5_glu_b"][0].reshape(8, 128)),
        "s5_bre": f(inp["s5_b_re"][0].reshape(-1, 16)), "s5_bim": f(inp["s5_b_im"][0].reshape(-1, 16)),
        "s5_cre": f(inp["s5_c_re"][0].reshape(-1, 64)), "s5_cim": f(inp["s5_c_im"][0].reshape(-1, 64)),
        "s5_gluw": f(inp["s5_glu_w"][0].reshape(1024, 16)),
        "rw_w0": f(inp["rw_w0"][0].reshape(16, 128)), "rw_a0": f(inp["rw_a0"][0].reshape(16, 128)),
        "rw_w2": f(inp["rw_w2"][0].reshape(128, 1024)), "rw_a2": f(inp["rw_a2"][0].reshape(128, 1024)),
        "rw_g2": f(inp["rw_g2"][0]), "rw_kk": f(inp["rw_k_k"][0].reshape(8, 128)), "rw_ka": f(inp["rw_k_a"][0].reshape(8, 128)),
        "rw_rk": f(inp["rw_r_k"][0].reshape(8, 128)), "rw_lnw": f(inp["rw_ln_w"][0].reshape(1, 1024)),
        "rw_lnb": f(inp["rw_ln_b"][0].reshape(1, 1024)),
        "w_out": f(inp["w_out"][0]), "norm2_g": f(inp["norm2_g"][0].reshape(16, 128)),
        "router_w": f(inp["router_w"][0]), "router_b": f(inp["router_b"][0].reshape(1, 32)),
        "final_g": f(inp["final_g"].reshape(16, 128)),
        "exp_wgu": f(inp["exp_w_gu"][0]), "exp_bgu": f(inp["exp_b_gu"][0].reshape(-1, 128)),
        "exp_wdn": f(inp["exp_w_dn"][0]), "exp_bdn": f(inp["exp_b_dn"][0]),
    }


_NC_CACHE = {}


def kernel(**inp):
    cfg = Cfg()
    if "nc" not in _NC_CACHE:
        _NC_CACHE["nc"] = build(cfg)
    nc = _NC_CACHE["nc"]
    nq = cfg.nq
    shared = {}
    in_maps = []
    for i in range(2 * nq):
        b, q = i // nq, i % nq
        m = make_inputs(cfg, b, inp, q)
        for k, v in m.items():
            if k in shared and shared[k].shape == v.shape and (shared[k] is v or np.shares_memory(shared[k], v)):
                m[k] = shared[k]
            else:
                shared.setdefault(k, v)
        in_maps.append(m)
    res = run_bass_kernel_spmd(nc, in_maps, core_ids=list(range(2 * nq)))
    out = np.empty((2, cfg.seq, D), np.float32)
    for i in range(2 * nq):
        b, q = i // nq, i % nq
        out[b, q * cfg.nown:(q + 1) * cfg.nown] = res.results[i]["out"]
    return out
```
